# Optimizing a Trainium2 kernel written in Bass

```python
import math
import jax
import jax.numpy as jnp
from jax import lax
import numpy as np

D_MODEL = 2048
BATCH = 2
SEQ = 8192
DEPTH = 4

CHUNK = 64
RMS_EPS = 1e-6

A_HEAD_DIM = 64
A_INNER = D_MODEL
A_HEADS = A_INNER // A_HEAD_DIM
A_GROUPS = 4
A_HEADS_PER_GROUP = A_HEADS // A_GROUPS
A_STATE = 128
A_CONV_K = 4
A_CONV_DIM = A_INNER + 2 * A_GROUPS * A_STATE

B_WIDTH = D_MODEL
B_GROUP = 16
B_GROUPS = B_WIDTH // B_GROUP
B_STATE = 64
B_STEP_MIN = 1e-3
B_STEP_MAX = 1e-1

AB_IN = A_INNER + A_CONV_DIM + A_HEADS + B_WIDTH
AB_MIX = A_INNER + B_WIDTH

C_HEAD_DIM = 128
C_WIDTH = D_MODEL
C_HEADS = C_WIDTH // C_HEAD_DIM
C_BLOCK = 16

N_EXPERTS = 32
TOP_K = 4
EXPERT_FF = 3 * D_MODEL // 8
SWIGLU_LIMIT = 7.0
SWIGLU_ALPHA = 1.702

N_AB_LAYERS = (DEPTH + 1) // 2
N_C_LAYERS = DEPTH // 2

kernel_name = "hybrid_ssd_s5_hgrn2_moe_adaln"


def _rmsnorm(x, g, eps=RMS_EPS):
    xf = x.astype(jnp.float32)
    y = xf * lax.rsqrt(jnp.mean(xf * xf, axis=-1, keepdims=True) + eps)
    return (y * g.astype(jnp.float32)).astype(x.dtype)


def _modulate(h, shift, scale):
    return h * (1 + scale[:, None, :]) + shift[:, None, :]


def _causal_dwconv(u, w, b):
    k = w.shape[0]
    y = lax.conv_general_dilated(u, w[:, None, :], window_strides=(1,), padding=[(k - 1, 0)],
                                 dimension_numbers=("NWC", "WIO", "NWC"),
                                 feature_group_count=u.shape[-1])
    return y + b


def _segsum(a):
    t = a.shape[-1]
    cs = jnp.cumsum(a, axis=-1)
    diff = cs[..., :, None] - cs[..., None, :]
    mask = jnp.tril(jnp.ones((t, t), dtype=bool))
    return jnp.where(mask, diff, -jnp.inf)


def _ssd(xs, bm, cm, dt_raw, a_log, dt_bias, d_skip):
    f32 = jnp.float32
    bsz, seq, _ = xs.shape
    nc = seq // CHUNK
    x = xs.astype(f32).reshape(bsz, nc, CHUNK, A_GROUPS, A_HEADS_PER_GROUP, A_HEAD_DIM)
    bc = bm.astype(f32).reshape(bsz, nc, CHUNK, A_GROUPS, A_STATE)
    cc = cm.astype(f32).reshape(bsz, nc, CHUNK, A_GROUPS, A_STATE)
    dt = jax.nn.softplus(dt_raw.astype(f32) + dt_bias.astype(f32))
    dt = dt.reshape(bsz, nc, CHUNK, A_GROUPS, A_HEADS_PER_GROUP)
    a_head = -jnp.exp(a_log.astype(f32)).reshape(A_GROUPS, A_HEADS_PER_GROUP)
    a = jnp.transpose(dt * a_head, (0, 3, 4, 1, 2))
    xdt = x * dt[..., None]
    a_cum = jnp.cumsum(a, axis=-1)
    decay_in = jnp.exp(_segsum(a))
    cb = jnp.einsum("bclgn,bcsgn->bgcls", cc, bc)
    y_diag = jnp.einsum("bgcls,bgjcls,bcsgjp->bclgjp", cb, decay_in, xdt)
    decay_to_end = jnp.exp(a_cum[..., -1:] - a_cum)
    states = jnp.einsum("bclgn,bgjcl,bclgjp->bcgjpn", bc, decay_to_end, xdt)
    states = jnp.concatenate([jnp.zeros_like(states[:, :1]), states], axis=1)
    chunk_tot = jnp.pad(a_cum[..., -1], ((0, 0), (0, 0), (0, 0), (1, 0)))
    chunk_decay = jnp.exp(_segsum(chunk_tot))
    states = jnp.einsum("bgjzc,bcgjpn->bzgjpn", chunk_decay, states)[:, :-1]
    y_off = jnp.einsum("bclgn,bcgjpn,bgjcl->bclgjp", cc, states, jnp.exp(a_cum))
    d = d_skip.astype(f32).reshape(A_GROUPS, A_HEADS_PER_GROUP)[:, :, None]
    y = y_diag + y_off + d * x
    return y.reshape(bsz, seq, A_INNER)


def _gated_group_rmsnorm(y, z, g):
    bsz, seq, w = y.shape
    v = (y * jax.nn.silu(z.astype(jnp.float32))).reshape(bsz, seq, A_GROUPS, w // A_GROUPS)
    v = v * lax.rsqrt(jnp.mean(v * v, axis=-1, keepdims=True) + 1e-5)
    return v.reshape(bsz, seq, w) * g.astype(jnp.float32)


def _s5(u, lam_re, lam_im, log_step, b_re, b_im, c_re, c_im, d_skip, glu_w, glu_b):
    f32 = jnp.float32
    bsz, seq, _ = u.shape
    uf = u.astype(f32)
    ug = uf.reshape(bsz, seq, B_GROUPS, B_GROUP)
    lr = lam_re.astype(f32)
    li = lam_im.astype(f32)
    step = jnp.exp(log_step.astype(f32))
    mag = jnp.exp(lr * step)
    ang = li * step
    lb_re = mag * jnp.cos(ang)
    lb_im = mag * jnp.sin(ang)
    den = lr * lr + li * li
    g_re = ((lb_re - 1) * lr + lb_im * li) / den
    g_im = (lb_im * lr - (lb_re - 1) * li) / den
    br = b_re.astype(f32)
    bi = b_im.astype(f32)
    bb_re = g_re[..., None] * br - g_im[..., None] * bi
    bb_im = g_re[..., None] * bi + g_im[..., None] * br
    bu_re = jnp.einsum("blgh,gph->blgp", ug, bb_re)
    bu_im = jnp.einsum("blgh,gph->blgp", ug, bb_im)
    a_re = jnp.broadcast_to(lb_re[None, None], (1, seq, B_GROUPS, B_STATE))
    a_im = jnp.broadcast_to(lb_im[None, None], (1, seq, B_GROUPS, B_STATE))

    def combine(e1, e2):
        a1r, a1i, b1r, b1i = e1
        a2r, a2i, b2r, b2i = e2
        return (a1r * a2r - a1i * a2i, a1r * a2i + a1i * a2r,
                a2r * b1r - a2i * b1i + b2r, a2r * b1i + a2i * b1r + b2i)

    _, _, s_re, s_im = lax.associative_scan(combine, (a_re, a_im, bu_re, bu_im), axis=1)
    y = (jnp.einsum("gkp,blgp->blgk", c_re.astype(f32), s_re)
         - jnp.einsum("gkp,blgp->blgk", c_im.astype(f32), s_im))
    y = y.reshape(bsz, seq, B_WIDTH) + d_skip.astype(f32) * uf
    gate = jax.nn.gelu(y) @ glu_w.astype(f32) + glu_b.astype(f32)
    return y * jax.nn.sigmoid(gate)


def _ssd_s5_mixer(h, w_in, conv_w, conv_b, a_log, dt_bias, ssd_d, ssd_norm_g,
                  lam_re, lam_im, log_step, b_re, b_im, c_re, c_im, s5_d, glu_w, glu_b, w_out):
    proj = h @ w_in
    z, xbc, dt_raw, u = jnp.split(
        proj, [A_INNER, A_INNER + A_CONV_DIM, A_INNER + A_CONV_DIM + A_HEADS], axis=-1)
    xbc = jax.nn.silu(_causal_dwconv(xbc, conv_w, conv_b))
    xs, bm, cm = jnp.split(xbc, [A_INNER, A_INNER + A_GROUPS * A_STATE], axis=-1)
    y_a = _gated_group_rmsnorm(_ssd(xs, bm, cm, dt_raw, a_log, dt_bias, ssd_d), z, ssd_norm_g)
    y_b = _s5(u, lam_re, lam_im, log_step, b_re, b_im, c_re, c_im, s5_d, glu_w, glu_b)
    return jnp.concatenate([y_a, y_b], axis=-1).astype(h.dtype) @ w_out


def _hgrn2_mixer(h, w_in, lower_bound, norm_g, w_out):
    f32 = jnp.float32
    bsz, seq, _ = h.shape
    nb = seq // C_BLOCK
    q, f, i, og = jnp.split(h @ w_in, 4, axis=-1)
    q = jax.nn.silu(q.astype(f32))
    lb = lower_bound.astype(f32)
    forget = lb + (1 - lb) * jax.nn.sigmoid(f.astype(f32))
    k = 1 - forget
    log_f = jnp.log(forget)

    def to_blocks(t):
        return t.reshape(bsz, nb, C_BLOCK, C_HEADS, C_HEAD_DIM).transpose(1, 0, 3, 2, 4)

    mask = jnp.tril(jnp.ones((C_BLOCK, C_BLOCK), f32))

    def step(state, blk):
        qb, kb, vb, gb = blk
        cum = jnp.cumsum(gb, axis=2)
        q_dec = qb * jnp.exp(cum)
        k_inv = kb * jnp.exp(-cum)
        scores = jnp.einsum("bhtk,bhsk->bhts", q_dec, k_inv) * mask
        o = (jnp.einsum("bhts,bhsv->bhtv", scores, vb)
             + jnp.einsum("bhtk,bhkv->bhtv", q_dec, state))
        last = cum[:, :, -1:, :]
        k_end = kb * jnp.exp(last - cum)
        state = (jnp.exp(last[:, :, 0, :])[..., None] * state
                 + jnp.einsum("bhsk,bhsv->bhkv", k_end, vb))
        return state, o

    s0 = jnp.zeros((bsz, C_HEADS, C_HEAD_DIM, C_HEAD_DIM), f32)
    _, o = lax.scan(step, s0, (to_blocks(q), to_blocks(k), to_blocks(i.astype(f32)), to_blocks(log_f)))
    o = o.transpose(1, 0, 3, 2, 4).reshape(bsz, seq, C_HEADS, C_HEAD_DIM)
    o = o * lax.rsqrt(jnp.mean(o * o, axis=-1, keepdims=True) + RMS_EPS) * norm_g.astype(f32)
    o = o.reshape(bsz, seq, C_WIDTH) * jax.nn.silu(og.astype(f32))
    return o.astype(h.dtype) @ w_out


def _moe(h, router_w, router_b, w1, b1, w2, b2):
    bsz, seq, d = h.shape
    t = h.reshape(bsz * seq, d)
    logits = (t @ router_w + router_b).astype(jnp.float32)
    top_v, top_i = lax.top_k(logits, TOP_K)
    wts = jax.nn.softmax(top_v, axis=-1)
    comb = jnp.sum(jax.nn.one_hot(top_i, N_EXPERTS, dtype=jnp.float32) * wts[..., None], axis=1)
    comb = comb.astype(t.dtype)

    def expert_step(acc, e):
        w1e, b1e, w2e, b2e, ge = e
        gu = t @ w1e + b1e
        gate = jnp.minimum(gu[:, ::2], SWIGLU_LIMIT)
        up = jnp.clip(gu[:, 1::2], -SWIGLU_LIMIT, SWIGLU_LIMIT)
        act = (up + 1) * (gate * jax.nn.sigmoid(SWIGLU_ALPHA * gate))
        return acc + (ge[:, None] * (act @ w2e + b2e)).astype(acc.dtype), None

    out, _ = lax.scan(expert_step, jnp.zeros_like(t), (w1, b1, w2, b2, comb.T))
    return out.reshape(bsz, seq, d)


def setup_inputs(seed: int = 0) -> dict:
    key = jax.random.key(seed)
    keys = iter(jax.random.split(key, 40))
    f32 = jnp.float32

    def nrm(shape, scale):
        return scale * jax.random.normal(next(keys), shape, f32)

    def unif(shape, lo, hi):
        return jax.random.uniform(next(keys), shape, f32, lo, hi)

    D = D_MODEL
    NA, NC = N_AB_LAYERS, N_C_LAYERS
    dt0 = jnp.exp(unif((NA, A_HEADS), math.log(1e-3), math.log(1e-1)))
    return {
        "x": nrm((BATCH, SEQ, D), 1.0),
        "c": nrm((BATCH, D), 1.0),
        "ada_w": nrm((DEPTH, D, 6 * D), 0.5 * D ** -0.5),
        "ada_b": nrm((DEPTH, 6 * D), 0.01),
        "norm1_g": 1.0 + nrm((DEPTH, D), 0.02),
        "norm2_g": 1.0 + nrm((DEPTH, D), 0.02),
        "ab_w_in": nrm((NA, D, AB_IN), D ** -0.5),
        "ab_conv_w": nrm((NA, A_CONV_K, A_CONV_DIM), A_CONV_K ** -0.5),
        "ab_conv_b": nrm((NA, A_CONV_DIM), 0.01),
        "ssd_a_log": jnp.log(unif((NA, A_HEADS), 1.0, 16.0)),
        "ssd_dt_bias": dt0 + jnp.log(-jnp.expm1(-dt0)),
        "ssd_d": 1.0 + nrm((NA, A_HEADS), 0.02),
        "ssd_norm_g": 1.0 + nrm((NA, A_INNER), 0.02),
        "s5_lam_re": -0.5 + nrm((NA, B_GROUPS, B_STATE), 0.01),
        "s5_lam_im": jnp.pi * jnp.arange(B_STATE, dtype=f32) + nrm((NA, B_GROUPS, B_STATE), 0.01),
        "s5_log_step": unif((NA, B_GROUPS, B_STATE), math.log(B_STEP_MIN), math.log(B_STEP_MAX)),
        "s5_b_re": nrm((NA, B_GROUPS, B_STATE, B_GROUP), (2 * B_GROUP) ** -0.5),
        "s5_b_im": nrm((NA, B_GROUPS, B_STATE, B_GROUP), (2 * B_GROUP) ** -0.5),
        "s5_c_re": nrm((NA, B_GROUPS, B_GROUP, B_STATE), B_STATE ** -0.5),
        "s5_c_im": nrm((NA, B_GROUPS, B_GROUP, B_STATE), B_STATE ** -0.5),
        "s5_d": nrm((NA, B_WIDTH), 1.0),
        "s5_glu_w": nrm((NA, B_WIDTH, B_WIDTH), B_WIDTH ** -0.5),
        "s5_glu_b": nrm((NA, B_WIDTH), 0.01),
        "ab_w_out": nrm((NA, AB_MIX, D), AB_MIX ** -0.5),
        "hg_w_in": nrm((NC, D, 4 * C_WIDTH), D ** -0.5),
        "hg_lower_bounds": nrm((DEPTH, C_WIDTH), 0.1),
        "hg_norm_g": 1.0 + nrm((NC, C_HEAD_DIM), 0.02),
        "hg_w_out": nrm((NC, C_WIDTH, D), C_WIDTH ** -0.5),
        "moe_router_w": nrm((DEPTH, D, N_EXPERTS), D ** -0.5),
        "moe_router_b": nrm((DEPTH, N_EXPERTS), 0.01),
        "moe_w1": nrm((DEPTH, N_EXPERTS, D, 2 * EXPERT_FF), D ** -0.5),
        "moe_b1": nrm((DEPTH, N_EXPERTS, 2 * EXPERT_FF), 0.01),
        "moe_w2": nrm((DEPTH, N_EXPERTS, EXPERT_FF, D), EXPERT_FF ** -0.5),
        "moe_b2": nrm((DEPTH, N_EXPERTS, D), 0.01),
        "final_ada_w": nrm((D, 2 * D), 0.5 * D ** -0.5),
        "final_ada_b": nrm((2 * D,), 0.01),
        "final_norm_g": 1.0 + nrm((D,), 0.02),
    }


def reference(x, c, ada_w, ada_b, norm1_g, norm2_g,
              ab_w_in, ab_conv_w, ab_conv_b, ssd_a_log, ssd_dt_bias, ssd_d, ssd_norm_g,
              s5_lam_re, s5_lam_im, s5_log_step, s5_b_re, s5_b_im, s5_c_re, s5_c_im,
              s5_d, s5_glu_w, s5_glu_b, ab_w_out,
              hg_w_in, hg_lower_bounds, hg_norm_g, hg_w_out,
              moe_router_w, moe_router_b, moe_w1, moe_b1, moe_w2, moe_b2,
              final_ada_w, final_ada_b, final_norm_g):
    cs = jax.nn.silu(c)
    lb_soft = jax.nn.softmax(hg_lower_bounds.astype(jnp.float32), axis=0)
    lbs = jnp.cumsum(lb_soft, axis=0) - lb_soft[0]
    h = x
    for l in range(DEPTH):
        mod = cs @ ada_w[l] + ada_b[l]
        sh1, sc1, g1, sh2, sc2, g2 = jnp.split(mod, 6, axis=-1)
        hn = _modulate(_rmsnorm(h, norm1_g[l]), sh1, sc1)
        i = l // 2
        if l % 2 == 0:
            mix = _ssd_s5_mixer(hn, ab_w_in[i], ab_conv_w[i], ab_conv_b[i], ssd_a_log[i],
                                ssd_dt_bias[i], ssd_d[i], ssd_norm_g[i],
                                s5_lam_re[i], s5_lam_im[i], s5_log_step[i], s5_b_re[i], s5_b_im[i],
                                s5_c_re[i], s5_c_im[i], s5_d[i], s5_glu_w[i], s5_glu_b[i],
                                ab_w_out[i])
        else:
            mix = _hgrn2_mixer(hn, hg_w_in[i], lbs[l], hg_norm_g[i], hg_w_out[i])
        h = h + g1[:, None, :] * mix
        hn = _modulate(_rmsnorm(h, norm2_g[l]), sh2, sc2)
        h = h + g2[:, None, :] * _moe(hn, moe_router_w[l], moe_router_b[l], moe_w1[l],
                                      moe_b1[l], moe_w2[l], moe_b2[l])
    fmod = cs @ final_ada_w + final_ada_b
    f_shift, f_scale = jnp.split(fmod, 2, axis=-1)
    return _modulate(_rmsnorm(h, final_norm_g), f_shift, f_scale)
```

```python
from contextlib import ExitStack
from concourse.bass_utils import run_bass_kernel_spmd
import numpy as np
import concourse.bass as bass
import concourse.mybir as mybir

F32 = mybir.dt.float32
BF16 = mybir.dt.bfloat16
AF = mybir.ActivationFunctionType
ALU = mybir.AluOpType
AX = mybir.AxisListType


class Unit:
    __slots__ = ("last_write", "reads", "name", "excl")

    def __init__(self, name=""):
        self.excl = False
        self.last_write = None
        self.reads = []
        self.name = name


class V:
    __slots__ = ("ap", "units")

    def __init__(self, ap, units):
        self.ap = ap
        self.units = units


class T:
    def __init__(self, handle, name, nunits=1):
        self.h = handle
        self.name = name
        self.units = [Unit(f"{name}.{i}") for i in range(nunits)]

    def __getitem__(self, idx):
        return V(self.h[idx], [self.units[0]])

    def at(self, i):
        return _At(self, i)

    def all(self, idx=slice(None)):
        return V(self.h[idx], list(self.units))

    def ap(self):
        return self.h


class _At:
    def __init__(self, t, i):
        self.t, self.i = t, i

    def __getitem__(self, idx):
        return V(self.t.h[idx], [self.t.units[self.i]])


class Op:
    __slots__ = ("eng", "emit", "waits", "idx", "needed", "num", "sem", "is_dma")

    def __init__(self, eng, emit, is_dma=False):
        self.eng = eng
        self.emit = emit
        self.waits = []
        self.idx = -1
        self.needed = False
        self.num = -1
        self.sem = None
        self.is_dma = is_dma


class _Scope:
    def __init__(self, kb):
        self.kb = kb

    def __enter__(self):
        self.mark = (self.kb.sb_off, self.kb.ps_off)
        return self

    def __exit__(self, *a):
        self.kb.barrier()
        self.kb.sb_off, self.kb.ps_off = self.mark
        return False


class KB:
    CE = ("pe", "dve", "act", "pool", "sp")

    def __init__(self, nc, stack, n_dma_sems=24):
        self.nc = nc
        self.stack = stack
        self.eng = {"pe": nc.tensor, "dve": nc.vector, "act": nc.scalar,
                    "pool": nc.gpsimd, "sp": nc.sync}
        self.sem = {e: stack.enter_context(nc.semaphore(f"s_{e}")) for e in self.CE}
        self.dma_sems = [stack.enter_context(nc.semaphore(f"s_dma{i}")) for i in range(n_dma_sems)]
        self.dma_last = [None] * n_dma_sems
        self.dma_rr = 0
        self.ops = []
        self.stream_len = {e: 0 for e in self.CE}
        self.seen = {e: {} for e in self.CE}
        self.sb_big = None
        self.ps_big = None
        self.sb_off = 0
        self.ps_off = 0
        self.sb_peak = 0
        self.same_engine_sync = {"pe": False, "dve": True, "act": True, "pool": True, "sp": True}

    SB_WORDS = 51200
    PS_WORDS = 4096

    def scope(self):
        return _Scope(self)

    def _carve(self, big, off, shape, dtype):
        n = 1
        for d in shape[1:]:
            n *= d
        esz = mybir.dt.size(dtype)
        words = (n * esz + 3) // 4
        words = (words + 7) // 8 * 8
        ap = big[0:shape[0], off:off + words]
        if dtype != F32:
            ap = ap.bitcast(dtype)
        ap = ap[:, 0:n]
        if len(shape) > 2:
            names = " ".join(f"d{i}" for i in range(1, len(shape)))
            kw = {f"d{i}": shape[i] for i in range(1, len(shape))}
            ap = ap.rearrange(f"p ({names}) -> p {names}", **kw)
        return ap, words

    def sbuf(self, name, shape, dtype, nunits=1):
        if self.sb_big is None:
            self.sb_big = self.stack.enter_context(self.nc.sbuf_tensor("sb_big", [128, self.SB_WORDS], F32))
        ap, words = self._carve(self.sb_big, self.sb_off, shape, dtype)
        self.sb_off += words
        self.sb_peak = max(self.sb_peak, self.sb_off)
        assert self.sb_off <= self.SB_WORDS, f"SBUF overflow allocating {name}: {self.sb_off * 4} B"
        return T(ap, name, nunits)

    def psum(self, name, shape, dtype, nunits=1):
        if self.ps_big is None:
            self.ps_big = self.stack.enter_context(self.nc.psum_tensor("ps_big", [128, self.PS_WORDS], F32))
        n = 1
        for d in shape[1:]:
            n *= d
        w = (n * mybir.dt.size(dtype) + 3) // 4
        if (self.ps_off % 512) + min(w, 512) > 512:
            self.ps_off = (self.ps_off + 511) // 512 * 512
        ap, words = self._carve(self.ps_big, self.ps_off, shape, dtype)
        self.ps_off += words
        assert self.ps_off <= self.PS_WORDS, f"PSUM overflow allocating {name}"
        t = T(ap, name, nunits)
        for u in t.units:
            u.excl = True
        return t

    def barrier(self):
        last = {}
        for o in self.ops:
            if o.emit is not None:
                last[self._stream_key(o)] = o
        for e in self.CE:
            b = Op(e, None)
            b.idx = self.stream_len[e]
            b.sem = self.sem[e]
            for o in last.values():
                if (not o.is_dma) and o.eng == e:
                    continue
                self._add_wait(b, o)
            self.ops.append(b)

    def dram(self, name, shape, dtype, kind="Internal", nunits=1):
        h = self.nc.dram_tensor(name, list(shape), dtype, kind=kind).ap()
        return T(h, name, nunits)

    def _stream_key(self, op):
        return ("dma", id(op.sem)) if op.is_dma else op.eng

    def _add_wait(self, op, prod):
        if prod is None or prod is op:
            return
        if (not prod.is_dma) and prod.eng == op.eng and not self.same_engine_sync[op.eng]:
            return
        key = self._stream_key(prod)
        pidx = prod.idx
        if self.seen[op.eng].get(key, -1) >= pidx:
            return
        self.seen[op.eng][key] = pidx
        prod.needed = True
        op.waits.append(prod)

    def _track(self, op, reads, writes):
        ex = [v for v in reads if any(u.excl for u in v.units)]
        if ex:
            reads = [v for v in reads if v not in ex]
            writes = list(writes) + ex
        for v in reads:
            for u in v.units:
                self._add_wait(op, u.last_write)
        for v in writes:
            for u in v.units:
                self._add_wait(op, u.last_write)
                for r in reversed(u.reads):
                    self._add_wait(op, r)
        for v in reads:
            for u in v.units:
                u.reads.append(op)
                if len(u.reads) > 64:
                    u.reads = u.reads[-48:]
        for v in writes:
            for u in v.units:
                u.last_write = op
                u.reads = []

    WRITE_KW = ("out", "accum_out", "out_max", "out_indices")

    def op(self, e, fn, *args, **kw):
        reads, writes = [], []
        for i, a in enumerate(args):
            if isinstance(a, V):
                (writes if i == 0 else reads).append(a)
        for k, a in kw.items():
            if isinstance(a, V):
                (writes if k in self.WRITE_KW else reads).append(a)
        extra_r = kw.pop("_reads", [])
        extra_w = kw.pop("_writes", [])
        reads += extra_r
        writes += extra_w
        rargs = [a.ap if isinstance(a, V) else a for a in args]
        rkw = {k: (a.ap if isinstance(a, V) else a) for k, a in kw.items()}
        engine = self.eng[e]

        def emit():
            return getattr(engine, fn)(*rargs, **rkw)

        o = Op(e, emit)
        o.idx = self.stream_len[e]
        self.stream_len[e] += 1
        o.sem = self.sem[e]
        self._track(o, reads, writes)
        self.ops.append(o)
        return o

    def dma(self, q, out, in_, **kw):
        engine = self.eng[q]
        si = self.dma_rr
        self.dma_rr = (self.dma_rr + 1) % len(self.dma_sems)
        oap, iap = out.ap, in_.ap

        def emit():
            return engine.dma_start(out=oap, in_=iap, **kw)

        o = Op(q, emit, is_dma=True)
        o.sem = self.dma_sems[si]
        prev = self.dma_last[si]
        o.idx = (prev.idx + 1) if prev is not None else 0
        self._add_wait(o, prev)
        self.dma_last[si] = o
        self._track(o, [in_], [out])
        o.needed = True
        self.ops.append(o)
        return o

    def collective(self, kind, op, groups, in_, out):
        engine = self.eng["pool"]
        si = self.dma_rr
        self.dma_rr = (self.dma_rr + 1) % len(self.dma_sems)
        oap, iap = out.ap, in_.ap

        def emit():
            return engine.collective_compute(kind, op, replica_groups=groups, ins=[iap], outs=[oap])

        o = Op("pool", emit, is_dma=True)
        o.sem = self.dma_sems[si]
        prev = self.dma_last[si]
        o.idx = (prev.idx + 1) if prev is not None else 0
        self._add_wait(o, prev)
        self.dma_last[si] = o
        self._track(o, [in_], [out])
        o.needed = True
        self.ops.append(o)
        return o

    def finish(self):
        fin = Op("sp", None)
        fin.idx = self.stream_len["sp"]
        last = {}
        for o in self.ops:
            if o.emit is not None:
                last[self._stream_key(o)] = o
        for o in last.values():
            if o.eng == "sp" and not o.is_dma:
                continue
            self._add_wait(fin, o)
        counters = {}
        for o in self.ops:
            if o.needed:
                key = self._stream_key(o)
                counters[key] = counters.get(key, 0) + 1
                o.num = counters[key]
        nwaits = 0
        for o in self.ops + [fin]:
            engine = self.eng[o.eng]
            for p in o.waits:
                val = p.num * (16 if p.is_dma else 1)
                engine.wait_ge(p.sem, val)
                nwaits += 1
            if o.emit is None:
                continue
            ins = o.emit()
            if o.needed:
                ins.then_inc(o.sem, 16 if o.is_dma else 1)
        self.stats = dict(nops=len(self.ops), nwaits=nwaits,
                          counters={str(k): v for k, v in counters.items()})
        return self.stats


I32 = mybir.dt.int32
D = 2048
NE = 32
FF = 768
ALPHA = 1.702
LIMIT = 7.0
F7 = ALPHA * LIMIT / (1.0 + np.exp(-ALPHA * LIMIT))
RMS_EPS = 1e-6


def bc(t, idx, shape):
    return V(t.h[idx].to_broadcast(list(shape)), t.units)


def make_consts(kb):
    C = {}
    io = kb.sbuf("c_io", [128, 128], I32)
    iof = kb.sbuf("c_iof", [128, 128], F32)
    C["ident"] = kb.sbuf("c_ident", [128, 128], F32)
    C["identb"] = kb.sbuf("c_identb", [128, 128], BF16)
    kb.op("pool", "iota", io[:], [[1, 128]], base=0, channel_multiplier=-1)
    kb.op("dve", "tensor_copy", iof[:], io[:])
    kb.op("dve", "tensor_single_scalar", C["ident"][:], iof[:], 0.0, ALU.is_equal)
    kb.op("dve", "tensor_copy", C["identb"][:], C["ident"][:])
    C["jmp"] = iof
    return C


def emit_mod(kb, ctD, wD, bD, modD, ncols):
    with kb.scope():
        cs = kb.sbuf("cs", [128, 16], F32)
        kb.dma("sp", cs[:], ctD[:])
        kb.op("act", "activation", cs[:], cs[:], AF.Silu)
        ws = [kb.sbuf(f"adaw{i}", [128, 16, 512], F32) for i in range(2)]
        brow = kb.sbuf("adab", [1, ncols], F32)
        orow = kb.sbuf("modrow", [1, ncols], F32)
        kb.dma("sp", brow[:], bD[:])
        ps = [kb.psum(f"modps{i}", [1, 512], F32) for i in range(2)]
        wv = wD.h.rearrange("(kc p) n -> p kc n", p=128)
        for cb in range(ncols // 512):
            w = ws[cb % 2]
            kb.dma("sp", w[:], V(wv[:, :, cb * 512:(cb + 1) * 512], wD.units))
            p = ps[cb % 2]
            for kc in range(16):
                kb.op("pe", "matmul", p[:], cs[:, kc:kc + 1], w[:, kc, :], start=(kc == 0), stop=(kc == 15))
            kb.op("dve", "tensor_tensor", orow[:, cb * 512:(cb + 1) * 512], p[:], brow[:, cb * 512:(cb + 1) * 512], ALU.add)
        kb.dma("sp", modD[:], orow[:])


def load_bc(kb, t, dramT, row_ap):
    kb.dma("sp", t[:], V(row_ap.partition_broadcast(t.h.shape[0]), dramT.units))


def emit_rmsnorm_tile(kb, ht, A, sh, hn, ss, junk):
    kb.op("act", "activation", junk[:], ht[:], AF.Square, accum_out=ss[:, 0:1])
    kb.op("dve", "tensor_scalar", ss[:, 1:2], ss[:, 0:1], 1.0 / D, RMS_EPS, ALU.mult, ALU.add)
    kb.op("act", "activation", ss[:, 2:3], ss[:, 1:2], AF.Sqrt)
    kb.op("dve", "reciprocal", ss[:, 3:4], ss[:, 2:3])
    kb.op("dve", "scalar_tensor_tensor", junk[:], ht[:], ss[:, 3:4], A[:], ALU.mult, ALU.mult)
    kb.op("dve", "tensor_tensor", hn[:], junk[:], sh[:], ALU.add)


STOP = ""


def emit_moe(kb, C, NT, hinD, houtD, modD, moff, ngD, rwD, rbD, w1D, b1D, w2D, b2D, fin=None):
    SP = 1024
    npass = NT // SP
    ntile = SP // 128
    with kb.scope():
        hnT = kb.sbuf("hnT", [128, 16, SP], BF16)
        acc = kb.sbuf("acc", [128, ntile, D], F32, nunits=ntile)
        gcol = kb.sbuf("gcol", [128, ntile, NE], F32)
        b1t = kb.sbuf("b1t", [128, NE, 12], F32)
        b1a = kb.sbuf("b1a", [128, NE, 6], F32)
        kb.dma("sp", b1t[:], b1D[:])
        kb.op("dve", "tensor_scalar", b1a[:], b1t[:, :, 0:6], ALPHA, None, ALU.mult)
        po = kb.psum("po", [128, 2048], F32, nunits=2)
        pgu = kb.psum("pgu", [128, 4, 512], F32, nunits=4)
        for ps_ in range(npass):
            with kb.scope():
                A2 = kb.sbuf("A2", [128, D], F32)
                sh2 = kb.sbuf("sh2", [128, D], F32)
                tmpg = kb.sbuf("tmpg", [128, D], F32)
                load_bc(kb, A2, modD, modD.h[0:1, moff[1]:moff[1] + D])
                load_bc(kb, tmpg, ngD, ngD.h[0:1, :])
                load_bc(kb, sh2, modD, modD.h[0:1, moff[0]:moff[0] + D])
                kb.op("dve", "scalar_tensor_tensor", A2[:], A2[:], 1.0, tmpg[:], ALU.add, ALU.mult)
                rw = kb.sbuf("rw", [128, 16, NE], F32)
                kb.dma("sp", rw[:], V(rwD.h.rearrange("(kc p) e -> p kc e", p=128), rwD.units))
                rb = kb.sbuf("rb", [128, NE], F32)
                load_bc(kb, rb, rbD, rbD.h[0:1, :])
                b2s = kb.sbuf("b2s", [NE, D], F32)
                kb.dma("sp", b2s[:], b2D[:])
                hts = [kb.sbuf(f"ht{i}", [128, D], F32) for i in range(2)]
                hns = [kb.sbuf(f"hn{i}", [128, D], F32) for i in range(2)]
                hT32 = kb.sbuf("hT32", [128, D], F32)
                sss = [kb.sbuf(f"ss{i}", [128, 4], F32) for i in range(2)]
                lgs = kb.sbuf("lgs", [128, NE], F32)
                m8 = kb.sbuf("m8", [128, 8], F32)
                msk = kb.sbuf("msk", [128, NE], F32)
                ex = kb.sbuf("ex", [128, NE], F32)
                sm = kb.sbuf("sm", [128, 4], F32)
                comb = kb.sbuf("comb", [128, NE], F32)
                combT = kb.sbuf("combT", [NE, 128], F32)
                lg = V(pgu.h[:, 0, 0:NE], [pgu.units[0]])
                cT = V(pgu.h[0:NE, 1, 0:128], [pgu.units[1]])
                for i in range(ntile):
                    gt = ps_ * ntile + i
                    ht, hn, ss = hts[i % 2], hns[i % 2], sss[i % 2]
                    kb.dma("sp", ht[:], hinD[gt * 128:(gt + 1) * 128, :])
                    emit_rmsnorm_tile(kb, ht, A2, sh2, hn, ss, tmpg)
                    tp = po.all()
                    for kc in range(16):
                        kb.op("pe", "transpose", V(po.h[:, kc * 128:(kc + 1) * 128], po.units),
                              hn[:, kc * 128:(kc + 1) * 128], C["ident"][:])
                    for bk in range(4):
                        kb.op("act", "copy", hT32[:, bk * 512:(bk + 1) * 512], V(po.h[:, bk * 512:(bk + 1) * 512], po.units))
                        kb.op("dve", "tensor_copy", hnT[:, bk * 4:(bk + 1) * 4, i * 128:(i + 1) * 128],
                              V(po.h[:, bk * 512:(bk + 1) * 512].rearrange("p (kc t) -> p kc t", t=128), po.units))
                    for kc in range(16):
                        kb.op("pe", "matmul", lg, hT32[:, kc * 128:(kc + 1) * 128], rw[:, kc, :],
                              start=(kc == 0), stop=(kc == 15))
                    kb.op("dve", "tensor_tensor", lgs[:], lg, rb[:], ALU.add)
                    kb.op("dve", "max", m8[:], lgs[:])
                    kb.op("dve", "tensor_scalar", msk[:], lgs[:], m8[:, 3:4], None, ALU.is_ge)
                    kb.op("dve", "tensor_scalar", sm[:, 0:1], m8[:, 0:1], -1.0, None, ALU.mult)
                    kb.op("act", "activation", ex[:], lgs[:], AF.Exp, bias=sm[:, 0:1])
                    kb.op("dve", "tensor_tensor", ex[:], ex[:], msk[:], ALU.mult)
                    kb.op("dve", "reduce_sum", sm[:, 1:2], ex[:], AX.X)
                    kb.op("dve", "reciprocal", sm[:, 2:3], sm[:, 1:2])
                    kb.op("dve", "tensor_scalar", comb[:], ex[:], sm[:, 2:3], None, ALU.mult)
                    kb.op("dve", "tensor_scalar", gcol[:, i, :], comb[:], 1.0 / ALPHA, None, ALU.mult)
                    kb.op("pe", "transpose", cT, comb[:], C["ident"][:])
                    kb.op("act", "copy", combT[:], cT)
                    for db in range(4):
                        kb.op("pe", "matmul", V(po.h[:, db * 512:(db + 1) * 512], po.units),
                              combT[:], b2s[:, db * 512:(db + 1) * 512], start=True, stop=True)
                    for bk in range(4):
                        kb.op("act", "copy", acc.at(i)[:, i, bk * 512:(bk + 1) * 512], V(po.h[:, bk * 512:(bk + 1) * 512], po.units))
            with kb.scope():
              if STOP != "stage0":
                  actT = [kb.sbuf(f"actT{i}", [128, 6, SP], BF16) for i in range(2)]
                  w1s = [kb.sbuf(f"w1s{i}", [128, 16, 256], BF16) for i in range(4)]
                  w2s = [kb.sbuf(f"w2s{i}", [128, 2, D], BF16) for i in range(3)]
                  a1s = [kb.sbuf(f"a1_{i}", [128, 512], F32) for i in range(2)]
                  u1s = [kb.sbuf(f"u1_{i}", [128, 512], F32) for i in range(2)]
                  nblk = SP // 512
                  for j in range(4):
                      kb.dma("pool", w1s[j][:], w1D[0, j])
                  for j in range(3):
                      kb.dma("pool", w2s[j][:], w2D[0, j])
                  cnt = 0
                  for e in range(NE):
                      aT = actT[e % 2]
                      for mp in range(6):
                          n = e * 6 + mp
                          w = w1s[n % 4]
                          for tb in range(nblk):
                              pg = pgu.at((cnt % 2) * 2)[:, (cnt % 2) * 2, :]
                              pu = pgu.at((cnt % 2) * 2 + 1)[:, (cnt % 2) * 2 + 1, :]
                              a1, u1 = a1s[cnt % 2], u1s[cnt % 2]
                              cnt += 1
                              for kc in range(16):
                                  kb.op("pe", "matmul", pg, w[:, kc, 0:128], hnT[:, kc, tb * 512:(tb + 1) * 512],
                                        start=(kc == 0), stop=(kc == 15))
                              for kc in range(16):
                                  kb.op("pe", "matmul", pu, w[:, kc, 128:256], hnT[:, kc, tb * 512:(tb + 1) * 512],
                                        start=(kc == 0), stop=(kc == 15))
                              kb.op("act", "activation", a1[:], pg, AF.Silu, bias=b1a[:, e, mp:mp + 1], scale=ALPHA)
                              kb.op("dve", "tensor_scalar", u1[:], pu, b1t[:, e, 6 + mp:7 + mp], LIMIT, ALU.add, ALU.min)
                              kb.op("dve", "tensor_scalar", u1[:], u1[:], -LIMIT, 1.0, ALU.max, ALU.add)
                              kb.op("dve", "scalar_tensor_tensor", aT[:, mp, tb * 512:(tb + 1) * 512], a1[:], float(F7),
                                    u1[:], ALU.min, ALU.mult)
                          n2 = n + 4
                          if n2 < NE * 6:
                              kb.dma("pool", w1s[n2 % 4][:], w1D[n2 // 6, n2 % 6])
                      for tt in range(ntile):
                          for hf in range(2):
                              pv = po.at(hf)
                              for dbh in range(2):
                                  db = hf * 2 + dbh
                                  for k in range(6):
                                      kb.op("pe", "matmul", pv[:, db * 512:(db + 1) * 512],
                                            aT[:, k, tt * 128:(tt + 1) * 128], w2s[k // 2][:, k % 2, db * 512:(db + 1) * 512],
                                            start=(k == 0), stop=(k == 5))
                              for dbh in range(2):
                                  sl = slice((hf * 2 + dbh) * 512, (hf * 2 + dbh + 1) * 512)
                                  kb.op("dve", "scalar_tensor_tensor", acc.at(tt)[:, tt, sl], pv[:, sl],
                                        gcol[:, tt, e:e + 1], acc.at(tt)[:, tt, sl], ALU.mult, ALU.add)
                      if e + 1 < NE:
                          for j in range(3):
                              kb.dma("pool", w2s[j][:], w2D[e + 1, j])
            with kb.scope():
                g2 = kb.sbuf("g2", [128, D], F32)
                load_bc(kb, g2, modD, modD.h[0:1, moff[2]:moff[2] + D])
                hts = [kb.sbuf(f"hr{i}", [128, D], F32) for i in range(2)]
                if fin is not None:
                    fmodD, fgD, outD = fin
                    fA = kb.sbuf("fA", [128, D], F32)
                    fsh = kb.sbuf("fsh", [128, D], F32)
                    tg = kb.sbuf("ftg", [128, D], F32)
                    load_bc(kb, fA, fmodD, fmodD.h[0:1, D:2 * D])
                    load_bc(kb, tg, fgD, fgD.h[0:1, :])
                    load_bc(kb, fsh, fmodD, fmodD.h[0:1, 0:D])
                    kb.op("dve", "scalar_tensor_tensor", fA[:], fA[:], 1.0, tg[:], ALU.add, ALU.mult)
                    fss = [kb.sbuf(f"fss{i}", [128, 4], F32) for i in range(2)]
                    fo = [kb.sbuf(f"fo{i}", [128, D], F32) for i in range(2)]
                for i in range(ntile):
                    gt = ps_ * ntile + i
                    ht = hts[i % 2]
                    kb.dma("sp", ht[:], hinD[gt * 128:(gt + 1) * 128, :])
                    kb.op("dve", "tensor_tensor", acc.at(i)[:, i, :], acc.at(i)[:, i, :], g2[:], ALU.mult)
                    kb.op("dve", "tensor_tensor", ht[:], ht[:], acc.at(i)[:, i, :], ALU.add)
                    if fin is None:
                        kb.dma("sp", houtD[gt * 128:(gt + 1) * 128, :], ht[:])
                    else:
                        emit_rmsnorm_tile(kb, ht, fA, fsh, fo[i % 2], fss[i % 2], tg)
                        kb.dma("sp", outD[gt * 128:(gt + 1) * 128, :], fo[i % 2][:])


TS = 512


def make_sel(kb, C):
    rowsel = kb.sbuf("rowsel", [128, 128, 128], BF16)
    colsel = kb.sbuf("colsel", [128, 128, 128], BF16)
    kb.op("dve", "tensor_copy", rowsel[:], V(C["identb"].h[:, :].unsqueeze(2).to_broadcast([128, 128, 128]), C["identb"].units))
    scr = kb.dram("identscr", [1, 128 * 128], BF16)
    kb.dma("sp", V(scr.h.rearrange("o (a b) -> (o a) b", a=128), scr.units), C["identb"][:])
    kb.dma("sp", V(colsel.h.rearrange("p a b -> p (a b)"), colsel.units), V(scr.h[0:1, :].partition_broadcast(128), scr.units))
    C["rowsel"], C["colsel"] = rowsel, colsel
    ones = kb.sbuf("ones32", [128, 128], F32)
    kb.op("dve", "memset", ones[:], 1.0)
    C["ones"] = ones


def emit_norm_segment(kb, C, hfullD, seg, modD, moff, ngD, hnT, ps):
    with kb.scope():
        A1 = kb.sbuf("A1", [128, D], F32)
        sh1 = kb.sbuf("sh1", [128, D], F32)
        tg = kb.sbuf("tg1", [128, D], F32)
        load_bc(kb, A1, modD, modD.h[0:1, moff[1]:moff[1] + D])
        load_bc(kb, tg, ngD, ngD.h[0:1, :])
        load_bc(kb, sh1, modD, modD.h[0:1, moff[0]:moff[0] + D])
        kb.op("dve", "scalar_tensor_tensor", A1[:], A1[:], 1.0, tg[:], ALU.add, ALU.mult)
        ht = kb.sbuf("ht_m", [128, D], F32)
        hn = kb.sbuf("hn_m", [128, D], F32)
        ss = kb.sbuf("ss_m", [128, 4], F32)
        for i in range(TS // 128):
            r0 = seg * TS + i * 128
            kb.dma("sp", ht[:], hfullD[r0:r0 + 128, :])
            emit_rmsnorm_tile(kb, ht, A1, sh1, hn, ss, tg)
            for bk in range(4):
                p = ps[bk % 2]
                for j in range(4):
                    kc = bk * 4 + j
                    kb.op("pe", "transpose", p[:, j * 128:(j + 1) * 128], hn[:, kc * 128:(kc + 1) * 128], C["ident"][:])
                kb.op("dve" if bk % 2 else "act", "tensor_copy" if bk % 2 else "copy",
                      hnT[:, bk * 4:(bk + 1) * 4, i * 128:(i + 1) * 128],
                      V(p.h[:, :].rearrange("p (kc t) -> p kc t", t=128), p.units))


def emit_proj_chunk(kb, wslot, wD_slab, hnT, p, mcols=128):
    src = wD_slab if mcols == 128 else V(wD_slab.ap[:, :, 0:mcols], wD_slab.units)
    kb.dma("pool", wslot[:, :, 0:mcols], src)
    for kc in range(16):
        kb.op("pe", "matmul", p[0:mcols, :], wslot[:, kc, 0:mcols], hnT[:, kc, :], start=(kc == 0), stop=(kc == 15))


def emit_gla(kb, C, decT, kT, qT, vhi, vlo, vrow0, nv, S, sbase, po, porow0, first, last, pbs, tmps, cnt):
    for v in range(nv):
        pb = pbs[cnt[0] % 2]
        d1, st, pq = tmps[cnt[0] % 2]
        cnt[0] += 1
        sel = C["rowsel"][:, vrow0 + v, :]
        kb.op("pe", "matmul", pb[:], sel, vhi, start=True, stop=False)
        kb.op("pe", "matmul", pb[:], sel, vlo, start=False, stop=True)
        kb.op("dve", "tensor_tensor", d1[:], pb[:], kT, ALU.mult)
        sc = S.at(sbase + v)[:, sbase + v:sbase + v + 1]
        kb.op("dve", "tensor_tensor_scan", st[:], decT, d1[:], sc, ALU.mult, ALU.add)
        kb.op("act", "copy", sc, st[:, TS - 1:TS])
        kb.op("pool", "tensor_tensor", pq[:], st[:], qT, ALU.mult)
        kb.op("pe", "matmul", po, C["colsel"][:, porow0 + v, :], pq[:],
              start=(first and v == 0), stop=(last and v == nv - 1))


def emit_hgrn_M(kb, C, L, lidx, hfullD, modD, ngD, wslabD, lbrawD, hggD, oTD):
    nseg = L // TS
    make_sel(kb, C)
    with kb.scope():
        lbr = kb.sbuf("lbr", [128, 4, 4], F32)
        lbe = kb.sbuf("lbe", [128, 4, 4], F32)
        lbs = kb.sbuf("lbs", [128, 4], F32)
        lb = kb.sbuf("lb", [128, 4], F32)
        oml = kb.sbuf("oml", [128, 4], F32)
        kb.dma("sp", lbr[:], lbrawD[:])
        kb.op("dve", "reduce_max", lbs[:], lbr[:], AX.X)
        kb.op("dve", "tensor_tensor", lbe[:], lbr[:], bc(lbs, (slice(None), slice(None)), [128, 4]) if False else
              V(lbs.h[:, :].unsqueeze(2).to_broadcast([128, 4, 4]), lbs.units), ALU.subtract)
        kb.op("act", "activation", lbe[:], lbe[:], AF.Exp)
        kb.op("dve", "reduce_sum", lbs[:], lbe[:], AX.X)
        kb.op("dve", "reciprocal", lbs[:], lbs[:])
        kb.op("dve", "reduce_sum", lb[:], lbe[:, :, 1:lidx + 1], AX.X)
        kb.op("dve", "tensor_tensor", lb[:], lb[:], lbs[:], ALU.mult)
        kb.op("dve", "tensor_scalar", oml[:], lb[:], -1.0, 1.0, ALU.mult, ALU.add)
        hgg = kb.sbuf("hgg", [128, 1], F32)
        kb.dma("sp", hgg[:], hggD[:])
        S = kb.sbuf("Sst", [128, 512], F32, nunits=512)
        kb.op("dve", "memset", S.all(), 0.0)
        hnT = kb.sbuf("hnT_m", [128, 16, TS], BF16)
        pss = [kb.psum(f"psA{i}", [128, 512], F32) for i in range(2)]
        pbs = [kb.psum(f"psB{i}", [128, 512], F32) for i in range(2)]
        po = kb.psum("psO", [128, 512], F32)
        pn = kb.psum("psN", [128, 512], F32)
        cnt = [0]
        pc = 0
        for seg in range(nseg):
            emit_norm_segment(kb, C, hfullD, seg, modD, (0, D, 2 * D), ngD, hnT, pss)
            with kb.scope():
                wsl = [kb.sbuf(f"wsl{i}", [128, 16, 128], BF16) for i in range(4)]
                decT = kb.sbuf("decT", [128, 4, TS], F32)
                kkT = kb.sbuf("kkT", [128, 4, TS], F32)
                qT = kb.sbuf("qT", [128, 4, TS], F32)
                ogs = kb.sbuf("ogs", [128, 4, TS], F32)
                ihi = kb.sbuf("ihi", [128, 4, TS], BF16, nunits=4)
                ilo = kb.sbuf("ilo", [128, 4, TS], BF16, nunits=4)
                tmps = [(kb.sbuf(f"d1_{i}", [128, TS], F32), kb.sbuf(f"st_{i}", [128, TS], F32),
                         kb.sbuf(f"pq_{i}", [128, TS], BF16)) for i in range(2)]
                sq = kb.sbuf("sq", [128, TS], F32)
                osb = kb.sbuf("osb", [128, TS], F32)
                rs = kb.sbuf("rs", [128, TS], F32)
                res = kb.sbuf("res", [128, TS], F32)
                for hh in range(4):
                    for ty in range(4):
                        p = pss[pc % 2]
                        w = wsl[pc % 4]
                        pc += 1
                        emit_proj_chunk(kb, w, wslabD[hh * 4 + ty], hnT, p)
                        if ty == 0:
                            kb.op("act", "activation", qT[:, hh, :], p[:], AF.Silu)
                        elif ty == 1:
                            kb.op("act", "activation", decT[:, hh, :], p[:], AF.Sigmoid)
                            kb.op("dve", "tensor_scalar", decT[:, hh, :], decT[:, hh, :], oml[:, hh:hh + 1], lb[:, hh:hh + 1],
                                  ALU.mult, ALU.add)
                            kb.op("dve", "tensor_scalar", kkT[:, hh, :], decT[:, hh, :], -1.0, 1.0, ALU.mult, ALU.add)
                        elif ty == 2:
                            kb.op("act", "copy", ihi.at(hh)[:, hh, :], p[:])
                            kb.op("dve", "tensor_tensor", ilo.at(hh)[:, hh, :], p[:], ihi.at(hh)[:, hh, :], ALU.subtract)
                        else:
                            kb.op("act", "activation", ogs[:, hh, :], p[:], AF.Silu)
                for hh in range(4):
                    emit_gla(kb, C, decT[:, hh, :], kkT[:, hh, :], qT[:, hh, :], ihi.at(hh)[:, hh, :], ilo.at(hh)[:, hh, :],
                             0, 128, S, hh * 128, po[:], 0, True, True, pbs, tmps, cnt)
                    kb.op("act", "activation", sq[:], po[:], AF.Square)
                    kb.op("dve", "tensor_copy", osb[:], po[:])
                    kb.op("pe", "matmul", pn[:], C["ones"][:], sq[:], start=True, stop=True)
                    kb.op("dve", "tensor_scalar", rs[:], pn[:], 1.0 / 128, RMS_EPS, ALU.mult, ALU.add)
                    kb.op("act", "activation", rs[:], rs[:], AF.Sqrt)
                    kb.op("dve", "reciprocal", rs[:], rs[:])
                    kb.op("dve", "tensor_tensor", res[:], osb[:], rs[:], ALU.mult)
                    kb.op("dve", "scalar_tensor_tensor", res[:], res[:], hgg[:, 0:1], ogs[:, hh, :], ALU.mult, ALU.mult)
                    kb.dma("sp", oTD[hh * 128:(hh + 1) * 128, seg * TS:(seg + 1) * TS], res[:])


def emit_outproj(kb, NT, hinD, hmidD, modD, g1off, srcs, woutD, nkc):
    with kb.scope():
        woutb = kb.sbuf("woutb", [128, nkc, D], BF16)
        wv = woutD.h.rearrange("(kc p) n -> p kc n", p=128)
        for k0 in range(0, nkc, 8):
            kb.dma("pool", woutb[:, k0:k0 + 8, :], V(wv[:, k0:k0 + 8, :], woutD.units))
        g1 = kb.sbuf("g1t", [128, D], F32)
        load_bc(kb, g1, modD, modD.h[0:1, g1off:g1off + D])
        mixs = [kb.sbuf(f"mixT{i}", [128, nkc, 512], BF16) for i in range(2 if nkc <= 16 else 1)]
        hts = [kb.sbuf(f"hto{i}", [128, D], F32) for i in range(2)]
        tmp = kb.sbuf("tmpo", [128, 512], F32)
        pss = [kb.psum(f"pso{i}", [128, 4, 512], F32, nunits=4) for i in range(2)]
        it = 0
        for tb in range(NT // 512):
            mx = mixs[tb % len(mixs)]
            k0 = 0
            for (sD, nch) in srcs:
                sv = sD.h.rearrange("(kc p) t -> p kc t", p=128)
                kb.dma("pool" if sD.h.dtype == F32 else "sp", mx[:, k0:k0 + nch, :], V(sv[:, :, tb * 512:(tb + 1) * 512], sD.units))
                k0 += nch
            for tt in range(4):
                gt = tb * 4 + tt
                ps = pss[it % 2]
                ht = hts[it % 2]
                it += 1
                kb.dma("sp", ht[:], hinD[gt * 128:(gt + 1) * 128, :])
                for db in range(4):
                    for kc in range(nkc):
                        kb.op("pe", "matmul", ps.at(db)[:, db, :], mx[:, kc, tt * 128:(tt + 1) * 128],
                              woutb[:, kc, db * 512:(db + 1) * 512], start=(kc == 0), stop=(kc == nkc - 1))
                for db in range(4):
                    sl = slice(db * 512, (db + 1) * 512)
                    kb.op("dve", "tensor_tensor", tmp[:], ps.at(db)[:, db, :], g1[:, sl], ALU.mult)
                    kb.op("dve", "tensor_tensor", ht[:, sl], ht[:, sl], tmp[:], ALU.add)
                kb.dma("sp", hmidD[gt * 128:(gt + 1) * 128, :], ht[:])


TWO_PI = 6.28318


def emit_ab_M(kb, C, L, hfullD, modD, ngD, wslabD, P, yaTD, ybTD):
    nseg = L // TS
    make_sel(kb, C)
    tabD = kb.dram("s5tab", [16, 128, 2, TS], F32)
    with kb.scope():
        ident = C["ident"]
        hnT = kb.sbuf("hnT_m", [128, 16, TS], BF16)
        S = kb.sbuf("Sst", [128, 512], F32, nunits=512)
        kb.op("dve", "memset", S.all(), 0.0)
        xpre = kb.sbuf("xpre", [128, 6, TS + 3], F32)
        kb.op("dve", "memset", xpre[:], 0.0)
        sprev = kb.sbuf("sprev", [128, 16, 2], F32)
        kb.op("dve", "memset", sprev[:], 0.0)
        convw = kb.sbuf("convw", [128, 6, 4], F32)
        convb = kb.sbuf("convb", [128, 6], F32)
        dtb = kb.sbuf("dtb", [8, 1], F32)
        negA = kb.sbuf("negA", [8, 1], F32)
        ssdD = kb.sbuf("ssdD", [128, 4], F32)
        normg = kb.sbuf("normg", [128, 4], F32)
        s5d = kb.sbuf("s5d", [32, 16], F32)
        for t_, n_ in ((convw, "convw"), (convb, "convb"), (dtb, "dtb"), (negA, "alog"), (ssdD, "ssdD"), (normg, "normg"), (s5d, "s5d")):
            kb.dma("sp", t_[:], P[n_][:])
        kb.op("act", "activation", negA[:], negA[:], AF.Exp)
        kb.op("dve", "tensor_scalar", negA[:], negA[:], -1.0, None, ALU.mult)
        sel8 = kb.sbuf("sel8", [8, 8, 128], F32)
        sel8b = kb.sbuf("sel8b", [8, 4, 128], F32)
        kb.op("dve", "tensor_copy", sel8[:], V(ident.h[0:8, 0:8].unsqueeze(2).to_broadcast([8, 8, 128]), ident.units))
        for c in range(4):
            kb.op("dve", "tensor_copy", sel8b[:, c, 0:64], sel8[:, 2 * c, 0:64])
            kb.op("dve", "tensor_copy", sel8b[:, c, 64:128], sel8[:, 2 * c + 1, 64:128])
        BbTr = kb.sbuf("BbTr", [32, 16, 128], BF16)
        BbTi = kb.sbuf("BbTi", [32, 16, 128], BF16)
        Creb = kb.sbuf("Creb", [128, 16, 32], BF16)
        nCimb = kb.sbuf("nCimb", [128, 16, 32], BF16)
        rho = kb.sbuf("rho", [128, 16], F32)
        cth = kb.sbuf("cth", [128, 16], F32)
        sth = kb.sbuf("sth", [128, 16], F32)
        pss = [kb.psum(f"psA{i}", [128, 512], F32) for i in range(2)]
        pbs = [kb.psum(f"psB{i}", [128, 512], F32) for i in range(2)]
        po = kb.psum("psO", [128, 512], F32)
        pn = kb.psum("psN", [128, 512], F32)
        with kb.scope():
            lre = kb.sbuf("lre", [128, 16], F32)
            lim = kb.sbuf("lim", [128, 16], F32)
            stp = kb.sbuf("stp", [128, 16], F32)
            thr = kb.sbuf("thr", [128, 16], F32)
            kb.dma("sp", lre[:], P["lre"][:])
            kb.dma("sp", lim[:], P["lim"][:])
            kb.dma("sp", stp[:], P["lstep"][:])
            kb.op("act", "activation", stp[:], stp[:], AF.Exp)
            kb.op("dve", "tensor_tensor", rho[:], lre[:], stp[:], ALU.mult)
            kb.op("act", "activation", rho[:], rho[:], AF.Exp)
            kb.op("dve", "tensor_tensor", thr[:], lim[:], stp[:], ALU.mult)
            kb.op("dve", "tensor_scalar", thr[:], thr[:], float(1.0 / (2 * np.pi)), None, ALU.mult)
            ioi = kb.sbuf("ioi", [128, TS], I32)
            iot = kb.sbuf("iot", [128, TS], F32)
            kb.op("pool", "iota", ioi[:], [[1, TS]], base=0, channel_multiplier=0)
            kb.op("dve", "tensor_copy", iot[:], ioi[:])
            ur = kb.sbuf("ur", [128, TS], F32)
            ki = kb.sbuf("ki", [128, TS], I32)
            kf = kb.sbuf("kf", [128, TS], F32)
            fr = kb.sbuf("fr", [128, TS], F32)
            mk = kb.sbuf("mk", [128, TS], F32)
            tabs = [kb.sbuf(f"tab{i}", [128, 2, TS], F32) for i in range(2)]
            for s in range(16):
                tb_ = tabs[s % 2]
                kb.op("dve", "tensor_scalar", ur[:], iot[:], thr[:, s:s + 1], None, ALU.mult)
                kb.op("dve", "tensor_copy", ki[:], ur[:])
                kb.op("dve", "tensor_copy", kf[:], ki[:])
                kb.op("dve", "tensor_tensor", fr[:], ur[:], kf[:], ALU.subtract)
                kb.op("act", "activation", tb_[:, 1, :], fr[:], AF.Sin, scale=TWO_PI)
                kb.op("dve", "tensor_scalar", fr[:], fr[:], 0.25, None, ALU.add)
                kb.op("dve", "tensor_single_scalar", mk[:], fr[:], 0.5, ALU.is_gt)
                kb.op("dve", "tensor_tensor", fr[:], fr[:], mk[:], ALU.subtract)
                kb.op("act", "activation", tb_[:, 0, :], fr[:], AF.Sin, scale=TWO_PI)
                kb.op("dve", "tensor_copy", cth[:, s:s + 1], tb_[:, 0, 1:2])
                kb.op("dve", "tensor_copy", sth[:, s:s + 1], tb_[:, 1, 1:2])
                kb.dma("sp", tabD[s], tb_[:])
            lbr = kb.sbuf("lbr_", [128, 16], F32)
            lbi = kb.sbuf("lbi_", [128, 16], F32)
            den = kb.sbuf("den", [128, 16], F32)
            t1 = kb.sbuf("t1_", [128, 16], F32)
            gre = kb.sbuf("gre", [128, 16], F32)
            gim = kb.sbuf("gim", [128, 16], F32)
            kb.op("dve", "tensor_tensor", lbr[:], rho[:], cth[:], ALU.mult)
            kb.op("dve", "tensor_scalar", lbr[:], lbr[:], -1.0, None, ALU.add)
            kb.op("dve", "tensor_tensor", lbi[:], rho[:], sth[:], ALU.mult)
            kb.op("dve", "tensor_tensor", den[:], lre[:], lre[:], ALU.mult)
            kb.op("dve", "tensor_tensor", t1[:], lim[:], lim[:], ALU.mult)
            kb.op("dve", "tensor_tensor", den[:], den[:], t1[:], ALU.add)
            kb.op("dve", "reciprocal", den[:], den[:])
            kb.op("dve", "tensor_tensor", gre[:], lbr[:], lre[:], ALU.mult)
            kb.op("dve", "tensor_tensor", t1[:], lbi[:], lim[:], ALU.mult)
            kb.op("dve", "tensor_tensor", gre[:], gre[:], t1[:], ALU.add)
            kb.op("dve", "tensor_tensor", gre[:], gre[:], den[:], ALU.mult)
            kb.op("dve", "tensor_tensor", gim[:], lbi[:], lre[:], ALU.mult)
            kb.op("dve", "tensor_tensor", t1[:], lbr[:], lim[:], ALU.mult)
            kb.op("dve", "tensor_tensor", gim[:], gim[:], t1[:], ALU.subtract)
            kb.op("dve", "tensor_tensor", gim[:], gim[:], den[:], ALU.mult)
            bre = kb.sbuf("bre", [128, 16, 32], F32)
            bim = kb.sbuf("bim", [128, 16, 32], F32)
            bbr = kb.sbuf("bbr", [128, 16, 32], F32)
            bbi = kb.sbuf("bbi", [128, 16, 32], F32)
            tt_ = kb.sbuf("tt_", [128, 16, 32], F32)
            kb.dma("sp", bre[:], P["bre"][:])
            kb.dma("sp", bim[:], P["bim"][:])
            greb = V(gre.h[:, :].unsqueeze(2).to_broadcast([128, 16, 32]), gre.units)
            gimb = V(gim.h[:, :].unsqueeze(2).to_broadcast([128, 16, 32]), gim.units)
            kb.op("dve", "tensor_tensor", bbr[:], bre[:], greb, ALU.mult)
            kb.op("dve", "tensor_tensor", tt_[:], bim[:], gimb, ALU.mult)
            kb.op("dve", "tensor_tensor", bbr[:], bbr[:], tt_[:], ALU.subtract)
            kb.op("dve", "tensor_tensor", bbi[:], bim[:], greb, ALU.mult)
            kb.op("dve", "tensor_tensor", tt_[:], bre[:], gimb, ALU.mult)
            kb.op("dve", "tensor_tensor", bbi[:], bbi[:], tt_[:], ALU.add)
            for s in range(16):
                for (src, dst) in ((bbr, BbTr), (bbi, BbTi)):
                    p = pss[s % 2]
                    kb.op("pe", "transpose", p[0:32, 0:128], src[:, s, :], ident[:])
                    kb.op("act", "copy", dst[:, s, :], p[0:32, 0:128])
            kb.dma("sp", bre[:], P["cre"][:])
            kb.dma("sp", bim[:], P["cim"][:])
            kb.op("dve", "tensor_copy", Creb[:], bre[:])
            kb.op("dve", "tensor_scalar", nCimb[:], bim[:], -1.0, None, ALU.mult)
        cnt = [0]
        pc = 0
        for seg in range(nseg):
            cs = slice(seg * TS, (seg + 1) * TS)
            emit_norm_segment(kb, C, hfullD, seg, modD, (0, D, 2 * D), ngD, hnT, pss)
            with kb.scope():
                wsl = [kb.sbuf(f"wsl{i}", [128, 16, 128], BF16) for i in range(2)]
                zs = kb.sbuf("zs", [128, 4, TS], F32)
                xc = kb.sbuf("xc", [128, 6, TS], F32)
                decb = kb.sbuf("decb", [128, 8, TS], F32)
                xhi = kb.sbuf("xhi", [128, 4, TS], BF16, nunits=4)
                xlo = kb.sbuf("xlo", [128, 4, TS], BF16, nunits=4)
                vch = kb.sbuf("vch", [128, 4, TS], F32)
                tmps = [(kb.sbuf(f"d1_{i}", [128, TS], F32), kb.sbuf(f"st_{i}", [128, TS], F32),
                         kb.sbuf(f"pq_{i}", [128, TS], BF16)) for i in range(2)]
                dtt = kb.sbuf("dtt", [8, TS], F32)
                dA = kb.sbuf("dA", [8, TS], F32)
                ycv = kb.sbuf("ycv", [128, TS], F32)
                sq = kb.sbuf("sq", [128, TS], F32)
                rs = kb.sbuf("rs", [128, TS], F32)
                ob = [kb.sbuf(f"ob{i}", [128, TS], F32) for i in range(2)]
                for c in range(4):
                    p = pss[pc % 2]; w = wsl[pc % 2]; pc += 1
                    emit_proj_chunk(kb, w, wslabD[c], hnT, p)
                    kb.op("act", "activation", zs[:, c, :], p[:], AF.Silu)
                for c in range(6):
                    p = pss[pc % 2]; w = wsl[pc % 2]; pc += 1
                    emit_proj_chunk(kb, w, wslabD[4 + c], hnT, p)
                    kb.op("act", "copy", xpre[:, c, 3:TS + 3], p[:])
                    kb.op("dve", "tensor_scalar", ycv[:], xpre[:, c, 0:TS], convw[:, c, 0:1], None, ALU.mult)
                    for k in range(1, 4):
                        kb.op("dve", "scalar_tensor_tensor", ycv[:], xpre[:, c, k:k + TS], convw[:, c, k:k + 1], ycv[:], ALU.mult, ALU.add)
                    kb.op("act", "activation", xc[:, c, :], ycv[:], AF.Silu, bias=convb[:, c:c + 1])
                    kb.op("dve", "tensor_copy", xpre[:, c, 0:3], xpre[:, c, TS:TS + 3])
                p = pss[pc % 2]; w = wsl[pc % 2]; pc += 1
                emit_proj_chunk(kb, w, wslabD[10], hnT, p, mcols=8)
                kb.op("act", "activation", dtt[:], p[0:8, :], AF.Exp, bias=dtb[:, 0:1])
                kb.op("act", "activation", dtt[:], dtt[:], AF.Ln, bias=1.0)
                kb.op("act", "activation", dA[:], dtt[:], AF.Exp, scale=negA[:, 0:1])
                for hh in range(8):
                    p = pss[pc % 2]; pc += 1
                    kb.op("pe", "matmul", p[:], sel8[:, hh, :], dA[:], start=True, stop=True)
                    kb.op("act", "copy", decb[:, hh, :], p[:])
                for c in range(4):
                    p = pss[pc % 2]; pc += 1
                    kb.op("pe", "matmul", p[:], sel8b[:, c, :], dtt[:], start=True, stop=True)
                    kb.op("dve", "tensor_tensor", ycv[:], xc[:, c, :], p[:], ALU.mult)
                    kb.op("act", "copy", xhi.at(c)[:, c, :], ycv[:])
                    kb.op("dve", "tensor_tensor", xlo.at(c)[:, c, :], ycv[:], xhi.at(c)[:, c, :], ALU.subtract)
                for c in range(4):
                    for h2 in range(2):
                        hh = 2 * c + h2
                        emit_gla(kb, C, decb[:, hh, :], xc[:, 4, :], xc[:, 5, :], xhi.at(c)[:, c, :], xlo.at(c)[:, c, :],
                                 h2 * 64, 64, S, hh * 64, po[:], h2 * 64, h2 == 0, h2 == 1, pbs, tmps, cnt)
                    kb.op("dve", "scalar_tensor_tensor", ycv[:], xc[:, c, :], ssdD[:, c:c + 1], po[:], ALU.mult, ALU.add)
                    kb.op("dve", "tensor_tensor", vch[:, c, :], ycv[:], zs[:, c, :], ALU.mult)
                    kb.op("act", "activation", sq[:], vch[:, c, :], AF.Square)
                    kb.op("pe", "matmul", pn[:], C["ones"][:], sq[:], start=(c == 0), stop=(c == 3))
                kb.op("dve", "tensor_scalar", rs[:], pn[:], 1.0 / 512, 1e-5, ALU.mult, ALU.add)
                kb.op("act", "activation", rs[:], rs[:], AF.Sqrt)
                kb.op("dve", "reciprocal", rs[:], rs[:])
                for c in range(4):
                    o_ = ob[c % 2]
                    kb.op("dve", "scalar_tensor_tensor", o_[:], vch[:, c, :], normg[:, c:c + 1], rs[:], ALU.mult, ALU.mult)
                    kb.dma("sp", yaTD[c * 128:(c + 1) * 128, cs], o_[:])
            with kb.scope():
                wsl = [kb.sbuf(f"wsl{i}", [128, 16, 128], BF16) for i in range(4)]
                tabs = [kb.sbuf(f"tabl{i}", [128, 2, TS], F32) for i in range(2)]
                uf = [kb.sbuf(f"uf{i}", [32, TS], F32) for i in range(2)]
                ub = [kb.sbuf(f"ub{i}", [32, TS], BF16) for i in range(2)]
                bur = kb.sbuf("bur", [128, TS], F32)
                bui = kb.sbuf("bui", [128, TS], F32)
                m1 = kb.sbuf("m1", [128, TS], F32)
                m2 = kb.sbuf("m2", [128, TS], F32)
                aa = kb.sbuf("aa", [128, TS], F32)
                bb = kb.sbuf("bb", [128, TS], F32)
                rhoB = kb.sbuf("rhoB", [128, TS], F32)
                onesT = kb.sbuf("onesT", [128, TS], F32)
                kb.op("pool", "memset", onesT[:], 1.0)
                wre = kb.sbuf("wre", [128, TS], F32)
                wim = kb.sbuf("wim", [128, TS], F32)
                sre = kb.sbuf("sre", [128, TS], F32)
                sim = kb.sbuf("sim", [128, TS], F32)
                sreb = kb.sbuf("sreb", [128, TS], BF16)
                simb = kb.sbuf("simb", [128, TS], BF16)
                ini = kb.sbuf("ini", [128, 4], F32)
                yb = [kb.sbuf(f"yb{i}", [32, TS], F32) for i in range(2)]
                for s in range(16):
                    p = pss[pc % 2]; w = wsl[pc % 4]; pc += 1
                    tb_ = tabs[s % 2]
                    ct_, st_ = tb_[:, 0, :], tb_[:, 1, :]
                    kb.dma("sp", tb_[:], tabD[s])
                    emit_proj_chunk(kb, w, wslabD[11 + s], hnT, p, mcols=32)
                    kb.op("act", "copy", uf[s % 2][:], p[0:32, :])
                    kb.op("dve", "tensor_copy", ub[s % 2][:], p[0:32, :])
                    kb.op("pe", "matmul", pbs[0][:], BbTr[:, s, :], ub[s % 2][:], start=True, stop=True)
                    kb.op("pe", "matmul", pbs[1][:], BbTi[:, s, :], ub[s % 2][:], start=True, stop=True)
                    kb.op("act", "copy", bur[:], pbs[0][:])
                    kb.op("act", "copy", bui[:], pbs[1][:])
                    kb.op("pool", "tensor_tensor", m1[:], bur[:], ct_, ALU.mult)
                    kb.op("pool", "tensor_tensor", m2[:], bui[:], st_, ALU.mult)
                    kb.op("pool", "tensor_tensor", aa[:], m1[:], m2[:], ALU.add)
                    kb.op("pool", "tensor_tensor", m1[:], bui[:], ct_, ALU.mult)
                    kb.op("pool", "tensor_tensor", m2[:], bur[:], st_, ALU.mult)
                    kb.op("pool", "tensor_tensor", bb[:], m1[:], m2[:], ALU.subtract)
                    kb.op("pool", "tensor_scalar", rhoB[:], onesT[:], rho[:, s:s + 1], None, ALU.mult)
                    kb.op("dve", "tensor_tensor", ini[:, 2:3], sprev[:, s, 1:2], sth[:, s:s + 1], ALU.mult)
                    kb.op("dve", "scalar_tensor_tensor", ini[:, 0:1], sprev[:, s, 0:1], cth[:, s:s + 1], ini[:, 2:3], ALU.mult, ALU.subtract)
                    kb.op("dve", "tensor_tensor", ini[:, 3:4], sprev[:, s, 1:2], cth[:, s:s + 1], ALU.mult)
                    kb.op("dve", "scalar_tensor_tensor", ini[:, 1:2], sprev[:, s, 0:1], sth[:, s:s + 1], ini[:, 3:4], ALU.mult, ALU.add)
                    kb.op("dve", "tensor_tensor_scan", wre[:], rhoB[:], aa[:], ini[:, 0:1], ALU.mult, ALU.add)
                    kb.op("dve", "tensor_tensor_scan", wim[:], rhoB[:], bb[:], ini[:, 1:2], ALU.mult, ALU.add)
                    kb.op("pool", "tensor_tensor", m1[:], wre[:], ct_, ALU.mult)
                    kb.op("pool", "tensor_tensor", m2[:], wim[:], st_, ALU.mult)
                    kb.op("pool", "tensor_tensor", sre[:], m1[:], m2[:], ALU.subtract)
                    kb.op("pool", "tensor_tensor", m1[:], wre[:], st_, ALU.mult)
                    kb.op("pool", "tensor_tensor", m2[:], wim[:], ct_, ALU.mult)
                    kb.op("pool", "tensor_tensor", sim[:], m1[:], m2[:], ALU.add)
                    kb.op("act", "copy", sprev[:, s, 0:1], sre[:, TS - 1:TS])
                    kb.op("act", "copy", sprev[:, s, 1:2], sim[:, TS - 1:TS])
                    kb.op("act", "copy", sreb[:], sre[:])
                    kb.op("act", "copy", simb[:], sim[:])
                    kb.op("pe", "matmul", po[0:32, :], Creb[:, s, :], sreb[:], start=True, stop=False)
                    kb.op("pe", "matmul", po[0:32, :], nCimb[:, s, :], simb[:], start=False, stop=True)
                    kb.op("dve", "scalar_tensor_tensor", yb[s % 2][:], uf[s % 2][:], s5d[:, s:s + 1], po[0:32, :], ALU.mult, ALU.add)
                    kb.dma("sp", ybTD[s * 32:(s + 1) * 32, cs], yb[s % 2][:])


def emit_glu(kb, NT, ybTD, gluwD, glubD, ybfD):
    with kb.scope():
        gw = kb.sbuf("gluw", [128, 16, D], BF16)
        wv = gluwD.h.rearrange("(kc p) n -> p kc n", p=128)
        for k0 in range(0, 16, 8):
            kb.dma("pool", gw[:, k0:k0 + 8, :], V(wv[:, k0:k0 + 8, :], gluwD.units))
        gb = kb.sbuf("glub", [128, 16], F32)
        kb.dma("sp", gb[:], glubD[:])
        yb = kb.sbuf("ybl", [128, 16, 512], F32)
        glT = kb.sbuf("glT", [128, 16, 512], BF16)
        obf = kb.sbuf("obf", [128, 16, 512], BF16)
        t1 = [kb.sbuf(f"gt1_{i}", [128, 512], F32) for i in range(2)]
        t2 = [kb.sbuf(f"gt2_{i}", [128, 512], F32) for i in range(2)]
        pss = [kb.psum(f"psg{i}", [128, 512], F32) for i in range(2)]
        yv = ybTD.h.rearrange("(kc p) t -> p kc t", p=128)
        ov = ybfD.h.rearrange("(kc p) t -> p kc t", p=128)
        for tb in range(NT // 512):
            cs = slice(tb * 512, (tb + 1) * 512)
            kb.dma("sp", yb[:], V(yv[:, :, cs], ybTD.units))
            for kc in range(16):
                a, b = t1[kc % 2], t2[kc % 2]
                kb.op("act", "activation", a[:], yb[:, kc, :], AF.Square)
                kb.op("dve", "tensor_scalar", a[:], a[:], 0.044715, 1.0, ALU.mult, ALU.add)
                kb.op("pool", "tensor_tensor", a[:], a[:], yb[:, kc, :], ALU.mult)
                kb.op("act", "activation", b[:], a[:], AF.Sigmoid, scale=1.5957691216057308)
                kb.op("pool", "tensor_tensor", glT[:, kc, :], b[:], yb[:, kc, :], ALU.mult)
            for m in range(16):
                p = pss[m % 2]
                a = t1[m % 2]
                for kc in range(16):
                    kb.op("pe", "matmul", p[:], gw[:, kc, m * 128:(m + 1) * 128], glT[:, kc, :], start=(kc == 0), stop=(kc == 15))
                kb.op("act", "activation", a[:], p[:], AF.Sigmoid, bias=gb[:, m:m + 1])
                kb.op("dve", "tensor_tensor", obf[:, m, :], a[:], yb[:, m, :], ALU.mult)
            kb.dma("sp", V(ov[:, :, cs], ybfD.units), obf[:])


def lay_c(c_b):
    return np.ascontiguousarray(c_b.reshape(16, 128).T)
def lay_w1(w1_l):
    NE = w1_l.shape[0]
    g = w1_l[:, :, 0::2].reshape(NE, 16, 128, 6, 128)
    u = w1_l[:, :, 1::2].reshape(NE, 16, 128, 6, 128)
    gu = np.concatenate([g, u], axis=-1)
    return np.ascontiguousarray(gu.transpose(0, 3, 2, 1, 4))
def lay_b1(b1_l):
    NE = b1_l.shape[0]
    g = b1_l[:, 0::2].reshape(NE, 6, 128)
    u = b1_l[:, 1::2].reshape(NE, 6, 128)
    gu = np.concatenate([g, u], axis=1)
    return np.ascontiguousarray(gu.transpose(2, 0, 1))
def lay_w2(w2_l):
    NE = w2_l.shape[0]
    return np.ascontiguousarray(w2_l.reshape(NE, 3, 2, 128, 2048).transpose(0, 1, 3, 2, 4))
def lay_slabs(w, cols_list):
    out = np.zeros((len(cols_list), 128, 16, 128), np.float32)
    for i, cols in enumerate(cols_list):
        blk = w[:, cols].reshape(16, 128, len(cols))
        out[i, :, :, :len(cols)] = blk.transpose(1, 0, 2)
    return out
def hgrn_slabs(w_in, j):
    cl = []
    for hh in range(4):
        hd = 4 * j + hh
        for ty in range(4):
            cl.append(np.arange(ty * 2048 + hd * 128, ty * 2048 + (hd + 1) * 128))
    return lay_slabs(w_in, cl)
def hgrn_lbraw(lb_all, j):
    x = lb_all[:, j * 512:(j + 1) * 512].reshape(4, 4, 128)
    return np.ascontiguousarray(x.transpose(2, 1, 0))
def ab_slabs(w_in, g):
    cl = []
    for c in range(4): cl.append(g * 512 + c * 128 + np.arange(128))
    for c in range(4): cl.append(2048 + g * 512 + c * 128 + np.arange(128))
    cl.append(4096 + g * 128 + np.arange(128))
    cl.append(4608 + g * 128 + np.arange(128))
    cl.append(5120 + g * 8 + np.arange(8))
    for s in range(16): cl.append(5152 + g * 512 + s * 32 + np.arange(32))
    return lay_slabs(w_in, cl)
def ab_params(inp, i, g):
    P = {}
    cw = inp["ab_conv_w"][i]; cb = inp["ab_conv_b"][i]
    chs = [g * 512 + c * 128 + np.arange(128) for c in range(4)] + [2048 + g * 128 + np.arange(128), 2560 + g * 128 + np.arange(128)]
    P["convw"] = np.ascontiguousarray(np.stack([cw[:, ch].T for ch in chs], axis=1))
    P["convb"] = np.ascontiguousarray(np.stack([cb[ch] for ch in chs], axis=1))
    P["dtb"] = np.ascontiguousarray(inp["ssd_dt_bias"][i][g * 8:(g + 1) * 8, None])
    P["alog"] = np.ascontiguousarray(inp["ssd_a_log"][i][g * 8:(g + 1) * 8, None])
    heads = g * 8 + (np.arange(512) // 64)
    P["ssdD"] = np.ascontiguousarray(inp["ssd_d"][i][heads].reshape(4, 128).T)
    P["normg"] = np.ascontiguousarray(inp["ssd_norm_g"][i][g * 512:(g + 1) * 512].reshape(4, 128).T)
    G = (32 * g + np.arange(32)).reshape(16, 2)
    def st(a):
        return np.ascontiguousarray(a[G].transpose(1, 2, 0).reshape(128, 16))
    P["lre"] = st(inp["s5_lam_re"][i]); P["lim"] = st(inp["s5_lam_im"][i]); P["lstep"] = st(inp["s5_log_step"][i])
    def bd(a):
        out = np.zeros((2, 64, 16, 2, 16), np.float32)
        for gg in range(2):
            out[gg, :, :, gg, :] = a[:, gg].transpose(1, 0, 2)
        return out.reshape(128, 16, 32)
    P["bre"] = bd(inp["s5_b_re"][i][G]); P["bim"] = bd(inp["s5_b_im"][i][G])
    P["cre"] = bd(inp["s5_c_re"][i][G].transpose(0, 1, 3, 2)); P["cim"] = bd(inp["s5_c_im"][i][G].transpose(0, 1, 3, 2))
    P["s5d"] = np.ascontiguousarray(inp["s5_d"][i][g * 512:(g + 1) * 512].reshape(16, 32).T)
    return P
AB_PSHAPES = dict(convw=[128, 6, 4], convb=[128, 6], dtb=[8, 1], alog=[8, 1], ssdD=[128, 4], normg=[128, 4],
                  lre=[128, 16], lim=[128, 16], lstep=[128, 16], bre=[128, 16, 32], bim=[128, 16, 32],
                  cre=[128, 16, 32], cim=[128, 16, 32], s5d=[32, 16])


NT_CORE = 2048
SEQ = 8192
_PROGS = {}


def _common(kb):
    ct = kb.dram("ct", [128, 16], F32, kind="ExternalInput")
    adaw = kb.dram("adaw", [D, 6 * D], F32, kind="ExternalInput")
    adab = kb.dram("adab", [1, 6 * D], F32, kind="ExternalInput")
    modD = kb.dram("modD", [1, 6 * D], F32)
    C = make_consts(kb)
    emit_mod(kb, ct, adaw, adab, modD, 6 * D)
    return C, modD, ct


def _build_M_hg(lidx):
    nc = bass.Bass("TRN2", target_bir_lowering=False)
    with ExitStack() as st:
        kb = KB(nc, st)
        hfull = kb.dram("hfull", [SEQ, D], F32, kind="ExternalInput")
        ng = kb.dram("ng", [1, D], F32, kind="ExternalInput")
        wsl = kb.dram("wsl", [16, 128, 16, 128], F32, kind="ExternalInput")
        lbraw = kb.dram("lbraw", [128, 4, 4], F32, kind="ExternalInput")
        hgg = kb.dram("hgg", [128, 1], F32, kind="ExternalInput")
        oT = kb.dram("oT", [512, SEQ], F32, kind="ExternalOutput")
        C, modD, ct = _common(kb)
        emit_hgrn_M(kb, C, SEQ, lidx, hfull, modD, ng, wsl, lbraw, hgg, oT)
        kb.finish()
    return nc


def _build_M_ab():
    nc = bass.Bass("TRN2", target_bir_lowering=False)
    with ExitStack() as st:
        kb = KB(nc, st)
        hfull = kb.dram("hfull", [SEQ, D], F32, kind="ExternalInput")
        ng = kb.dram("ng", [1, D], F32, kind="ExternalInput")
        wsl = kb.dram("wsl", [27, 128, 16, 128], F32, kind="ExternalInput")
        P = {k: kb.dram("p_" + k, shp, F32, kind="ExternalInput") for k, shp in AB_PSHAPES.items()}
        yaT = kb.dram("yaT", [512, SEQ], F32, kind="ExternalOutput")
        ybT = kb.dram("ybT", [512, SEQ], F32, kind="ExternalOutput")
        C, modD, ct = _common(kb)
        emit_ab_M(kb, C, SEQ, hfull, modD, ng, wsl, P, yaT, ybT)
        kb.finish()
    return nc


def _build_P(kind, final):
    nc = bass.Bass("TRN2", target_bir_lowering=False)
    with ExitStack() as st:
        kb = KB(nc, st)
        NT = NT_CORE
        hin = kb.dram("hin", [NT, D], F32, kind="ExternalInput")
        ng = kb.dram("ng", [1, D], F32, kind="ExternalInput")
        rw = kb.dram("rw", [D, NE], F32, kind="ExternalInput")
        rb = kb.dram("rb", [1, NE], F32, kind="ExternalInput")
        w1 = kb.dram("w1", [NE, 6, 128, 16, 256], F32, kind="ExternalInput")
        b1 = kb.dram("b1", [128, NE, 12], F32, kind="ExternalInput")
        w2 = kb.dram("w2", [NE, 3, 128, 2, D], F32, kind="ExternalInput")
        b2 = kb.dram("b2", [NE, D], F32, kind="ExternalInput")
        hout = kb.dram("hout", [NT, D], F32, kind="ExternalOutput")
        hmid = kb.dram("hmid", [NT, D], F32)
        C, modD, ct = _common(kb)
        if kind == "ab":
            yaT = kb.dram("yaT", [D, NT], F32, kind="ExternalInput")
            ybT = kb.dram("ybT", [D, NT], F32, kind="ExternalInput")
            gluw = kb.dram("gluw", [D, D], F32, kind="ExternalInput")
            glub = kb.dram("glub", [128, 16], F32, kind="ExternalInput")
            wout = kb.dram("wout", [2 * D, D], F32, kind="ExternalInput")
            ybfD = kb.dram("ybfD", [D, NT], BF16)
            emit_glu(kb, NT, ybT, gluw, glub, ybfD)
            emit_outproj(kb, NT, hin, hmid, modD, 2 * D, [(yaT, 16), (ybfD, 16)], wout, 32)
        else:
            oT = kb.dram("oT", [D, NT], F32, kind="ExternalInput")
            wout = kb.dram("wout", [D, D], F32, kind="ExternalInput")
            emit_outproj(kb, NT, hin, hmid, modD, 2 * D, [(oT, 16)], wout, 16)
        fin = None
        if final:
            faw = kb.dram("faw", [D, 2 * D], F32, kind="ExternalInput")
            fab = kb.dram("fab", [1, 2 * D], F32, kind="ExternalInput")
            fg = kb.dram("fg", [1, D], F32, kind="ExternalInput")
            fmodD = kb.dram("fmodD", [1, 2 * D], F32)
            emit_mod(kb, ct, faw, fab, fmodD, 2 * D)
            fin = (fmodD, fg, hout)
        emit_moe(kb, C, NT, hmid, hout, modD, (3 * D, 4 * D, 5 * D), ng, rw, rb, w1, b1, w2, b2, fin=fin)
        kb.finish()
    return nc


def _prog(key, fn, *a):
    if key not in _PROGS:
        _PROGS[key] = fn(*a)
    return _PROGS[key]


def kernel(**inp):
    inp = {k: np.asarray(v) for k, v in inp.items()}
    x = inp["x"].astype(np.float32, copy=False)
    c = inp["c"].astype(np.float32, copy=False)
    B, L, _ = x.shape
    nq = L // NT_CORE
    ncore = B * nq
    A = np.ascontiguousarray
    h = [A(x[cc // nq, (cc % nq) * NT_CORE:(cc % nq + 1) * NT_CORE]) for cc in range(ncore)]
    cts = [lay_c(c[b]) for b in range(B)]
    depth = inp["ada_w"].shape[0]
    for l in range(depth):
        i = l // 2
        final = (l == depth - 1)
        adaw, adab = A(inp["ada_w"][l]), A(inp["ada_b"][l][None])
        hfull = [np.concatenate(h[b * nq:(b + 1) * nq], axis=0) for b in range(B)]
        if l % 2 == 0:
            nc = _prog("M_ab", _build_M_ab)
            in_maps = []
            for cc in range(ncore):
                b, g = cc // 4, cc % 4
                d = dict(hfull=hfull[b], ct=cts[b], adaw=adaw, adab=adab, ng=A(inp["norm1_g"][l][None]),
                         wsl=ab_slabs(inp["ab_w_in"][i], g))
                for k, v in ab_params(inp, i, g).items():
                    d["p_" + k] = v
                in_maps.append(d)
            res = run_bass_kernel_spmd(nc, in_maps, core_ids=list(range(ncore)))
            yaT = [np.concatenate([res.results[b * 4 + g]["yaT"] for g in range(4)], axis=0) for b in range(B)]
            ybT = [np.concatenate([res.results[b * 4 + g]["ybT"] for g in range(4)], axis=0) for b in range(B)]
            extra = lambda b, q: dict(yaT=A(yaT[b][:, q * NT_CORE:(q + 1) * NT_CORE]), ybT=A(ybT[b][:, q * NT_CORE:(q + 1) * NT_CORE]),
                                      gluw=A(inp["s5_glu_w"][i]), glub=A(inp["s5_glu_b"][i].reshape(16, 128).T),
                                      wout=A(inp["ab_w_out"][i]))
            kind = "ab"
        else:
            nc = _prog(("M_hg", l), _build_M_hg, l)
            in_maps = []
            for cc in range(ncore):
                b, j = cc // 4, cc % 4
                in_maps.append(dict(hfull=hfull[b], ct=cts[b], adaw=adaw, adab=adab, ng=A(inp["norm1_g"][l][None]),
                                    wsl=hgrn_slabs(inp["hg_w_in"][i], j), lbraw=hgrn_lbraw(inp["hg_lower_bounds"], j),
                                    hgg=A(inp["hg_norm_g"][i][:, None])))
            res = run_bass_kernel_spmd(nc, in_maps, core_ids=list(range(ncore)))
            oT = [np.concatenate([res.results[b * 4 + j]["oT"] for j in range(4)], axis=0) for b in range(B)]
            extra = lambda b, q: dict(oT=A(oT[b][:, q * NT_CORE:(q + 1) * NT_CORE]), wout=A(inp["hg_w_out"][i]))
            kind = "hg"
        del res
        nc = _prog(("P", kind, final), _build_P, kind, final)
        shared = dict(adaw=adaw, adab=adab, ng=A(inp["norm2_g"][l][None]), rw=A(inp["moe_router_w"][l]),
                      rb=A(inp["moe_router_b"][l][None]), w1=lay_w1(inp["moe_w1"][l]), b1=lay_b1(inp["moe_b1"][l]),
                      w2=lay_w2(inp["moe_w2"][l]), b2=A(inp["moe_b2"][l]))
        if final:
            shared.update(faw=A(inp["final_ada_w"]), fab=A(inp["final_ada_b"][None]), fg=A(inp["final_norm_g"][None]))
        in_maps = []
        for cc in range(ncore):
            b, q = cc // nq, cc % nq
            d = dict(shared, hin=h[cc], ct=cts[b])
            d.update(extra(b, q))
            in_maps.append(d)
        res = run_bass_kernel_spmd(nc, in_maps, core_ids=list(range(ncore)))
        h = [np.asarray(res.results[cc]["hout"]) for cc in range(ncore)]
        del res
    out = np.stack([np.concatenate(h[b * nq:(b + 1) * nq], axis=0) for b in range(B)], axis=0)
    return out.astype(np.float32)
```

```python
from contextlib import ExitStack
from concourse.bass_utils import run_bass_kernel_spmd
import numpy as np
import concourse.bass as bass
import concourse.mybir as mybir

F32 = mybir.dt.float32
BF16 = mybir.dt.bfloat16
AF = mybir.ActivationFunctionType
ALU = mybir.AluOpType
AX = mybir.AxisListType


class Unit:
    __slots__ = ("last_write", "reads", "name", "excl")

    def __init__(self, name=""):
        self.excl = False
        self.last_write = None
        self.reads = []
        self.name = name


class V:
    __slots__ = ("ap", "units")

    def __init__(self, ap, units):
        self.ap = ap
        self.units = units


class T:
    def __init__(self, handle, name, nunits=1):
        self.h = handle
        self.name = name
        self.units = [Unit(f"{name}.{i}") for i in range(nunits)]

    def __getitem__(self, idx):
        return V(self.h[idx], [self.units[0]])

    def at(self, i):
        return _At(self, i)

    def all(self, idx=slice(None)):
        return V(self.h[idx], list(self.units))

    def ap(self):
        return self.h


class _At:
    def __init__(self, t, i):
        self.t, self.i = t, i

    def __getitem__(self, idx):
        return V(self.t.h[idx], [self.t.units[self.i]])


class Op:
    __slots__ = ("eng", "emit", "waits", "idx", "needed", "num", "sem", "is_dma")

    def __init__(self, eng, emit, is_dma=False):
        self.eng = eng
        self.emit = emit
        self.waits = []
        self.idx = -1
        self.needed = False
        self.num = -1
        self.sem = None
        self.is_dma = is_dma


class _Scope:
    def __init__(self, kb):
        self.kb = kb

    def __enter__(self):
        self.mark = (self.kb.sb_off, self.kb.ps_off)
        return self

    def __exit__(self, *a):
        self.kb.barrier()
        self.kb.sb_off, self.kb.ps_off = self.mark
        return False


class KB:
    CE = ("pe", "dve", "act", "pool", "sp")

    def __init__(self, nc, stack, n_dma_sems=24):
        self.nc = nc
        self.stack = stack
        self.eng = {"pe": nc.tensor, "dve": nc.vector, "act": nc.scalar,
                    "pool": nc.gpsimd, "sp": nc.sync}
        self.sem = {e: stack.enter_context(nc.semaphore(f"s_{e}")) for e in self.CE}
        self.dma_sems = [stack.enter_context(nc.semaphore(f"s_dma{i}")) for i in range(n_dma_sems)]
        self.dma_last = [None] * n_dma_sems
        self.dma_rr = 0
        self.ops = []
        self.stream_len = {e: 0 for e in self.CE}
        self.seen = {e: {} for e in self.CE}
        self.sb_big = None
        self.ps_big = None
        self.sb_off = 0
        self.ps_off = 0
        self.sb_peak = 0
        ses = True
        self.same_engine_sync = {"pe": False, "dve": ses, "act": ses, "pool": ses, "sp": True}

    SB_WORDS = 51200
    PS_WORDS = 4096

    def scope(self):
        return _Scope(self)

    def _carve(self, big, off, shape, dtype):
        n = 1
        for d in shape[1:]:
            n *= d
        esz = mybir.dt.size(dtype)
        words = (n * esz + 3) // 4
        words = (words + 7) // 8 * 8
        ap = big[0:shape[0], off:off + words]
        if dtype != F32:
            ap = ap.bitcast(dtype)
        ap = ap[:, 0:n]
        if len(shape) > 2:
            names = " ".join(f"d{i}" for i in range(1, len(shape)))
            kw = {f"d{i}": shape[i] for i in range(1, len(shape))}
            ap = ap.rearrange(f"p ({names}) -> p {names}", **kw)
        return ap, words

    def sbuf(self, name, shape, dtype, nunits=1):
        if self.sb_big is None:
            self.sb_big = self.stack.enter_context(self.nc.sbuf_tensor("sb_big", [128, self.SB_WORDS], F32))
        ap, words = self._carve(self.sb_big, self.sb_off, shape, dtype)
        self.sb_off += words
        self.sb_peak = max(self.sb_peak, self.sb_off)
        assert self.sb_off <= self.SB_WORDS, f"SBUF overflow allocating {name}: {self.sb_off * 4} B"
        return T(ap, name, nunits)

    def psum(self, name, shape, dtype, nunits=1):
        if self.ps_big is None:
            self.ps_big = self.stack.enter_context(self.nc.psum_tensor("ps_big", [128, self.PS_WORDS], F32))
        n = 1
        for d in shape[1:]:
            n *= d
        w = (n * mybir.dt.size(dtype) + 3) // 4
        if (self.ps_off % 512) + min(w, 512) > 512:
            self.ps_off = (self.ps_off + 511) // 512 * 512
        ap, words = self._carve(self.ps_big, self.ps_off, shape, dtype)
        self.ps_off += words
        assert self.ps_off <= self.PS_WORDS, f"PSUM overflow allocating {name}"
        t = T(ap, name, nunits)
        for u in t.units:
            u.excl = True
        return t

    def barrier(self):
        last = {}
        for o in self.ops:
            if o.emit is not None:
                last[self._stream_key(o)] = o
        for e in self.CE:
            b = Op(e, None)
            b.idx = self.stream_len[e]
            b.sem = self.sem[e]
            for o in last.values():
                if (not o.is_dma) and o.eng == e:
                    continue
                self._add_wait(b, o)
            self.ops.append(b)

    def dram(self, name, shape, dtype, kind="Internal", nunits=1):
        h = self.nc.dram_tensor(name, list(shape), dtype, kind=kind).ap()
        return T(h, name, nunits)

    def _stream_key(self, op):
        return ("dma", id(op.sem)) if op.is_dma else op.eng

    def _add_wait(self, op, prod):
        if prod is None or prod is op:
            return
        if (not prod.is_dma) and prod.eng == op.eng and not self.same_engine_sync[op.eng]:
            return
        key = self._stream_key(prod)
        pidx = prod.idx
        if self.seen[op.eng].get(key, -1) >= pidx:
            return
        self.seen[op.eng][key] = pidx
        prod.needed = True
        op.waits.append(prod)

    def _track(self, op, reads, writes):
        ex = [v for v in reads if any(u.excl for u in v.units)]
        if ex:
            reads = [v for v in reads if v not in ex]
            writes = list(writes) + ex
        for v in reads:
            for u in v.units:
                self._add_wait(op, u.last_write)
        for v in writes:
            for u in v.units:
                self._add_wait(op, u.last_write)
                for r in reversed(u.reads):
                    self._add_wait(op, r)
        for v in reads:
            for u in v.units:
                u.reads.append(op)
                if len(u.reads) > 64:
                    u.reads = u.reads[-48:]
        for v in writes:
            for u in v.units:
                u.last_write = op
                u.reads = []

    WRITE_KW = ("out", "accum_out", "out_max", "out_indices")

    def op(self, e, fn, *args, **kw):
        reads, writes = [], []
        for i, a in enumerate(args):
            if isinstance(a, V):
                (writes if i == 0 else reads).append(a)
        for k, a in kw.items():
            if isinstance(a, V):
                (writes if k in self.WRITE_KW else reads).append(a)
        extra_r = kw.pop("_reads", [])
        extra_w = kw.pop("_writes", [])
        reads += extra_r
        writes += extra_w
        rargs = [a.ap if isinstance(a, V) else a for a in args]
        rkw = {k: (a.ap if isinstance(a, V) else a) for k, a in kw.items()}
        engine = self.eng[e]

        def emit():
            return getattr(engine, fn)(*rargs, **rkw)

        o = Op(e, emit)
        o.idx = self.stream_len[e]
        self.stream_len[e] += 1
        o.sem = self.sem[e]
        self._track(o, reads, writes)
        self.ops.append(o)
        return o

    def dma(self, q, out, in_, **kw):
        engine = self.eng[q]
        si = self.dma_rr
        self.dma_rr = (self.dma_rr + 1) % len(self.dma_sems)
        oap, iap = out.ap, in_.ap

        def emit():
            return engine.dma_start(out=oap, in_=iap, **kw)

        o = Op(q, emit, is_dma=True)
        o.sem = self.dma_sems[si]
        prev = self.dma_last[si]
        o.idx = (prev.idx + 1) if prev is not None else 0
        self._add_wait(o, prev)
        self.dma_last[si] = o
        self._track(o, [in_], [out])
        o.needed = True
        self.ops.append(o)
        return o

    def collective(self, kind, op, groups, in_, out):
        engine = self.eng["pool"]
        si = self.dma_rr
        self.dma_rr = (self.dma_rr + 1) % len(self.dma_sems)
        oap, iap = out.ap, in_.ap

        def emit():
            return engine.collective_compute(kind, op, replica_groups=groups, ins=[iap], outs=[oap])

        o = Op("pool", emit, is_dma=True)
        o.sem = self.dma_sems[si]
        prev = self.dma_last[si]
        o.idx = (prev.idx + 1) if prev is not None else 0
        self._add_wait(o, prev)
        self.dma_last[si] = o
        self._track(o, [in_], [out])
        o.needed = True
        self.ops.append(o)
        return o

    def finish(self):
        fin = Op("sp", None)
        fin.idx = self.stream_len["sp"]
        last = {}
        for o in self.ops:
            if o.emit is not None:
                last[self._stream_key(o)] = o
        for o in last.values():
            if o.eng == "sp" and not o.is_dma:
                continue
            self._add_wait(fin, o)
        counters = {}
        for o in self.ops:
            if o.needed:
                key = self._stream_key(o)
                counters[key] = counters.get(key, 0) + 1
                o.num = counters[key]
        nwaits = 0
        for o in self.ops + [fin]:
            engine = self.eng[o.eng]
            for p in o.waits:
                val = p.num * (16 if p.is_dma else 1)
                engine.wait_ge(p.sem, val)
                nwaits += 1
            if o.emit is None:
                continue
            ins = o.emit()
            if o.needed:
                ins.then_inc(o.sem, 16 if o.is_dma else 1)
        self.stats = dict(nops=len(self.ops), nwaits=nwaits,
                          counters={str(k): v for k, v in counters.items()})
        return self.stats


I32 = mybir.dt.int32
D = 2048
NE = 32
FF = 768
ALPHA = 1.702
LIMIT = 7.0
F7 = ALPHA * LIMIT / (1.0 + np.exp(-ALPHA * LIMIT))
RMS_EPS = 1e-6


def bc(t, idx, shape):
    return V(t.h[idx].to_broadcast(list(shape)), t.units)


def make_consts(kb):
    C = {}
    io = kb.sbuf("c_io", [128, 128], I32)
    iof = kb.sbuf("c_iof", [128, 128], F32)
    C["ident"] = kb.sbuf("c_ident", [128, 128], F32)
    C["identb"] = kb.sbuf("c_identb", [128, 128], BF16)
    kb.op("pool", "iota", io[:], [[1, 128]], base=0, channel_multiplier=-1)
    kb.op("dve", "tensor_copy", iof[:], io[:])
    kb.op("dve", "tensor_single_scalar", C["ident"][:], iof[:], 0.0, ALU.is_equal)
    kb.op("dve", "tensor_copy", C["identb"][:], C["ident"][:])
    C["jmp"] = iof
    return C


def emit_mod(kb, ctD, wD, bD, modD, ncols):
    with kb.scope():
        cs = kb.sbuf("cs", [128, 16], F32)
        kb.dma("sp", cs[:], ctD[:])
        kb.op("act", "activation", cs[:], cs[:], AF.Silu)
        ws = [kb.sbuf(f"adaw{i}", [128, 16, 512], F32) for i in range(2)]
        brow = kb.sbuf("adab", [1, ncols], F32)
        orow = kb.sbuf("modrow", [1, ncols], F32)
        kb.dma("sp", brow[:], bD[:])
        ps = [kb.psum(f"modps{i}", [1, 512], F32) for i in range(2)]
        wv = wD.h.rearrange("(kc p) n -> p kc n", p=128)
        for cb in range(ncols // 512):
            w = ws[cb % 2]
            kb.dma("sp", w[:], V(wv[:, :, cb * 512:(cb + 1) * 512], wD.units))
            p = ps[cb % 2]
            for kc in range(16):
                kb.op("pe", "matmul", p[:], cs[:, kc:kc + 1], w[:, kc, :], start=(kc == 0), stop=(kc == 15))
            kb.op("dve", "tensor_tensor", orow[:, cb * 512:(cb + 1) * 512], p[:], brow[:, cb * 512:(cb + 1) * 512], ALU.add)
        kb.dma("sp", modD[:], orow[:])


def load_bc(kb, t, dramT, row_ap):
    kb.dma("sp", t[:], V(row_ap.partition_broadcast(t.h.shape[0]), dramT.units))


def emit_rmsnorm_tile(kb, ht, A, sh, hn, ss, junk):
    kb.op("act", "activation", junk[:], ht[:], AF.Square, accum_out=ss[:, 0:1])
    kb.op("dve", "tensor_scalar", ss[:, 1:2], ss[:, 0:1], 1.0 / D, RMS_EPS, ALU.mult, ALU.add)
    kb.op("act", "activation", ss[:, 2:3], ss[:, 1:2], AF.Sqrt)
    kb.op("dve", "reciprocal", ss[:, 3:4], ss[:, 2:3])
    kb.op("dve", "scalar_tensor_tensor", junk[:], ht[:], ss[:, 3:4], A[:], ALU.mult, ALU.mult)
    kb.op("dve", "tensor_tensor", hn[:], junk[:], sh[:], ALU.add)


STOP = ""


def emit_moe(kb, C, NT, hinD, houtD, modD, moff, ngD, rwD, rbD, w1D, b1D, w2D, b2D, fin=None):
    SP = 1024
    npass = NT // SP
    ntile = SP // 128
    with kb.scope():
        hnT = kb.sbuf("hnT", [128, 16, SP], BF16)
        acc = kb.sbuf("acc", [128, ntile, D], F32, nunits=ntile)
        gcol = kb.sbuf("gcol", [128, ntile, NE], F32)
        b1t = kb.sbuf("b1t", [128, NE, 12], F32)
        b1a = kb.sbuf("b1a", [128, NE, 6], F32)
        kb.dma("sp", b1t[:], b1D[:])
        kb.op("dve", "tensor_scalar", b1a[:], b1t[:, :, 0:6], ALPHA, None, ALU.mult)
        po = kb.psum("po", [128, 2048], F32, nunits=2)
        pgu = kb.psum("pgu", [128, 4, 512], F32, nunits=4)
        for ps_ in range(npass):
            with kb.scope():
                A2 = kb.sbuf("A2", [128, D], F32)
                sh2 = kb.sbuf("sh2", [128, D], F32)
                tmpg = kb.sbuf("tmpg", [128, D], F32)
                load_bc(kb, A2, modD, modD.h[0:1, moff[1]:moff[1] + D])
                load_bc(kb, tmpg, ngD, ngD.h[0:1, :])
                load_bc(kb, sh2, modD, modD.h[0:1, moff[0]:moff[0] + D])
                kb.op("dve", "scalar_tensor_tensor", A2[:], A2[:], 1.0, tmpg[:], ALU.add, ALU.mult)
                rw = kb.sbuf("rw", [128, 16, NE], F32)
                kb.dma("sp", rw[:], V(rwD.h.rearrange("(kc p) e -> p kc e", p=128), rwD.units))
                rb = kb.sbuf("rb", [128, NE], F32)
                load_bc(kb, rb, rbD, rbD.h[0:1, :])
                b2s = kb.sbuf("b2s", [NE, D], F32)
                kb.dma("sp", b2s[:], b2D[:])
                hts = [kb.sbuf(f"ht{i}", [128, D], F32) for i in range(2)]
                hns = [kb.sbuf(f"hn{i}", [128, D], F32) for i in range(2)]
                hT32 = kb.sbuf("hT32", [128, D], F32)
                sss = [kb.sbuf(f"ss{i}", [128, 4], F32) for i in range(2)]
                lgs = kb.sbuf("lgs", [128, NE], F32)
                m8 = kb.sbuf("m8", [128, 8], F32)
                msk = kb.sbuf("msk", [128, NE], F32)
                ex = kb.sbuf("ex", [128, NE], F32)
                sm = kb.sbuf("sm", [128, 4], F32)
                comb = kb.sbuf("comb", [128, NE], F32)
                combT = kb.sbuf("combT", [NE, 128], F32)
                lg = V(pgu.h[:, 0, 0:NE], [pgu.units[0]])
                cT = V(pgu.h[0:NE, 1, 0:128], [pgu.units[1]])
                for i in range(ntile):
                    gt = ps_ * ntile + i
                    ht, hn, ss = hts[i % 2], hns[i % 2], sss[i % 2]
                    kb.dma("sp", ht[:], hinD[gt * 128:(gt + 1) * 128, :])
                    emit_rmsnorm_tile(kb, ht, A2, sh2, hn, ss, tmpg)
                    tp = po.all()
                    for kc in range(16):
                        kb.op("pe", "transpose", V(po.h[:, kc * 128:(kc + 1) * 128], po.units),
                              hn[:, kc * 128:(kc + 1) * 128], C["ident"][:])
                    for bk in range(4):
                        kb.op("act", "copy", hT32[:, bk * 512:(bk + 1) * 512], V(po.h[:, bk * 512:(bk + 1) * 512], po.units))
                        kb.op("dve", "tensor_copy", hnT[:, bk * 4:(bk + 1) * 4, i * 128:(i + 1) * 128],
                              V(po.h[:, bk * 512:(bk + 1) * 512].rearrange("p (kc t) -> p kc t", t=128), po.units))
                    for kc in range(16):
                        kb.op("pe", "matmul", lg, hT32[:, kc * 128:(kc + 1) * 128], rw[:, kc, :],
                              start=(kc == 0), stop=(kc == 15))
                    kb.op("dve", "tensor_tensor", lgs[:], lg, rb[:], ALU.add)
                    kb.op("dve", "max", m8[:], lgs[:])
                    kb.op("dve", "tensor_scalar", msk[:], lgs[:], m8[:, 3:4], None, ALU.is_ge)
                    kb.op("dve", "tensor_scalar", sm[:, 0:1], m8[:, 0:1], -1.0, None, ALU.mult)
                    kb.op("act", "activation", ex[:], lgs[:], AF.Exp, bias=sm[:, 0:1])
                    kb.op("dve", "tensor_tensor", ex[:], ex[:], msk[:], ALU.mult)
                    kb.op("dve", "reduce_sum", sm[:, 1:2], ex[:], AX.X)
                    kb.op("dve", "reciprocal", sm[:, 2:3], sm[:, 1:2])
                    kb.op("dve", "tensor_scalar", comb[:], ex[:], sm[:, 2:3], None, ALU.mult)
                    kb.op("dve", "tensor_scalar", gcol[:, i, :], comb[:], 1.0 / ALPHA, None, ALU.mult)
                    kb.op("pe", "transpose", cT, comb[:], C["ident"][:])
                    kb.op("act", "copy", combT[:], cT)
                    for db in range(4):
                        kb.op("pe", "matmul", V(po.h[:, db * 512:(db + 1) * 512], po.units),
                              combT[:], b2s[:, db * 512:(db + 1) * 512], start=True, stop=True)
                    for bk in range(4):
                        kb.op("act", "copy", acc.at(i)[:, i, bk * 512:(bk + 1) * 512], V(po.h[:, bk * 512:(bk + 1) * 512], po.units))
            with kb.scope():
              if STOP != "stage0":
                  actT = [kb.sbuf(f"actT{i}", [128, 6, SP], BF16) for i in range(2)]
                  w1s = [kb.sbuf(f"w1s{i}", [128, 16, 256], BF16) for i in range(4)]
                  w2s = [kb.sbuf(f"w2s{i}", [128, 2, D], BF16) for i in range(3)]
                  a1s = [kb.sbuf(f"a1_{i}", [128, 512], F32) for i in range(2)]
                  u1s = [kb.sbuf(f"u1_{i}", [128, 512], F32) for i in range(2)]
                  nblk = SP // 512
                  for j in range(4):
                      kb.dma("pool", w1s[j][:], w1D[0, j])
                  for j in range(3):
                      kb.dma("pool", w2s[j][:], w2D[0, j])
                  cnt = 0
                  for e in range(NE):
                      aT = actT[e % 2]
                      for mp in range(6):
                          n = e * 6 + mp
                          w = w1s[n % 4]
                          for tb in range(nblk):
                              pg = pgu.at((cnt % 2) * 2)[:, (cnt % 2) * 2, :]
                              pu = pgu.at((cnt % 2) * 2 + 1)[:, (cnt % 2) * 2 + 1, :]
                              a1, u1 = a1s[cnt % 2], u1s[cnt % 2]
                              cnt += 1
                              for kc in range(16):
                                  kb.op("pe", "matmul", pg, w[:, kc, 0:128], hnT[:, kc, tb * 512:(tb + 1) * 512],
                                        start=(kc == 0), stop=(kc == 15))
                              for kc in range(16):
                                  kb.op("pe", "matmul", pu, w[:, kc, 128:256], hnT[:, kc, tb * 512:(tb + 1) * 512],
                                        start=(kc == 0), stop=(kc == 15))
                              kb.op("act", "activation", a1[:], pg, AF.Silu, bias=b1a[:, e, mp:mp + 1], scale=ALPHA)
                              kb.op("dve", "tensor_scalar", u1[:], pu, b1t[:, e, 6 + mp:7 + mp], LIMIT, ALU.add, ALU.min)
                              kb.op("dve", "tensor_scalar", u1[:], u1[:], -LIMIT, 1.0, ALU.max, ALU.add)
                              kb.op("dve", "scalar_tensor_tensor", aT[:, mp, tb * 512:(tb + 1) * 512], a1[:], float(F7),
                                    u1[:], ALU.min, ALU.mult)
                          n2 = n + 4
                          if n2 < NE * 6:
                              kb.dma("pool", w1s[n2 % 4][:], w1D[n2 // 6, n2 % 6])
                      for tt in range(ntile):
                          for hf in range(2):
                              pv = po.at(hf)
                              for dbh in range(2):
                                  db = hf * 2 + dbh
                                  for k in range(6):
                                      kb.op("pe", "matmul", pv[:, db * 512:(db + 1) * 512],
                                            aT[:, k, tt * 128:(tt + 1) * 128], w2s[k // 2][:, k % 2, db * 512:(db + 1) * 512],
                                            start=(k == 0), stop=(k == 5))
                              for dbh in range(2):
                                  sl = slice((hf * 2 + dbh) * 512, (hf * 2 + dbh + 1) * 512)
                                  kb.op("dve", "scalar_tensor_tensor", acc.at(tt)[:, tt, sl], pv[:, sl],
                                        gcol[:, tt, e:e + 1], acc.at(tt)[:, tt, sl], ALU.mult, ALU.add)
                      if e + 1 < NE:
                          for j in range(3):
                              kb.dma("pool", w2s[j][:], w2D[e + 1, j])
            with kb.scope():
                g2 = kb.sbuf("g2", [128, D], F32)
                load_bc(kb, g2, modD, modD.h[0:1, moff[2]:moff[2] + D])
                hts = [kb.sbuf(f"hr{i}", [128, D], F32) for i in range(2)]
                if fin is not None:
                    fmodD, fgD, outD = fin
                    fA = kb.sbuf("fA", [128, D], F32)
                    fsh = kb.sbuf("fsh", [128, D], F32)
                    tg = kb.sbuf("ftg", [128, D], F32)
                    load_bc(kb, fA, fmodD, fmodD.h[0:1, D:2 * D])
                    load_bc(kb, tg, fgD, fgD.h[0:1, :])
                    load_bc(kb, fsh, fmodD, fmodD.h[0:1, 0:D])
                    kb.op("dve", "scalar_tensor_tensor", fA[:], fA[:], 1.0, tg[:], ALU.add, ALU.mult)
                    fss = [kb.sbuf(f"fss{i}", [128, 4], F32) for i in range(2)]
                    fo = [kb.sbuf(f"fo{i}", [128, D], F32) for i in range(2)]
                for i in range(ntile):
                    gt = ps_ * ntile + i
                    ht = hts[i % 2]
                    kb.dma("sp", ht[:], hinD[gt * 128:(gt + 1) * 128, :])
                    kb.op("dve", "tensor_tensor", acc.at(i)[:, i, :], acc.at(i)[:, i, :], g2[:], ALU.mult)
                    kb.op("dve", "tensor_tensor", ht[:], ht[:], acc.at(i)[:, i, :], ALU.add)
                    if fin is None:
                        kb.dma("sp", houtD[gt * 128:(gt + 1) * 128, :], ht[:])
                    else:
                        emit_rmsnorm_tile(kb, ht, fA, fsh, fo[i % 2], fss[i % 2], tg)
                        kb.dma("sp", outD[gt * 128:(gt + 1) * 128, :], fo[i % 2][:])


TS = 512


def make_sel(kb, C):
    rowsel = kb.sbuf("rowsel", [128, 128, 128], BF16)
    colsel = kb.sbuf("colsel", [128, 128, 128], BF16)
    kb.op("dve", "tensor_copy", rowsel[:], V(C["identb"].h[:, :].unsqueeze(2).to_broadcast([128, 128, 128]), C["identb"].units))
    scr = kb.dram("identscr", [1, 128 * 128], BF16)
    kb.dma("sp", V(scr.h.rearrange("o (a b) -> (o a) b", a=128), scr.units), C["identb"][:])
    kb.dma("sp", V(colsel.h.rearrange("p a b -> p (a b)"), colsel.units), V(scr.h[0:1, :].partition_broadcast(128), scr.units))
    C["rowsel"], C["colsel"] = rowsel, colsel
    ones = kb.sbuf("ones32", [128, 128], F32)
    kb.op("dve", "memset", ones[:], 1.0)
    C["ones"] = ones


def emit_norm_segment(kb, C, hfullD, seg, modD, moff, ngD, hnT, ps):
    with kb.scope():
        A1 = kb.sbuf("A1", [128, D], F32)
        sh1 = kb.sbuf("sh1", [128, D], F32)
        tg = kb.sbuf("tg1", [128, D], F32)
        load_bc(kb, A1, modD, modD.h[0:1, moff[1]:moff[1] + D])
        load_bc(kb, tg, ngD, ngD.h[0:1, :])
        load_bc(kb, sh1, modD, modD.h[0:1, moff[0]:moff[0] + D])
        kb.op("dve", "scalar_tensor_tensor", A1[:], A1[:], 1.0, tg[:], ALU.add, ALU.mult)
        ht = kb.sbuf("ht_m", [128, D], F32)
        hn = kb.sbuf("hn_m", [128, D], F32)
        ss = kb.sbuf("ss_m", [128, 4], F32)
        for i in range(TS // 128):
            r0 = seg * TS + i * 128
            kb.dma("sp", ht[:], hfullD[r0:r0 + 128, :])
            emit_rmsnorm_tile(kb, ht, A1, sh1, hn, ss, tg)
            for bk in range(4):
                p = ps[bk % 2]
                for j in range(4):
                    kc = bk * 4 + j
                    kb.op("pe", "transpose", p[:, j * 128:(j + 1) * 128], hn[:, kc * 128:(kc + 1) * 128], C["ident"][:])
                kb.op("dve" if bk % 2 else "act", "tensor_copy" if bk % 2 else "copy",
                      hnT[:, bk * 4:(bk + 1) * 4, i * 128:(i + 1) * 128],
                      V(p.h[:, :].rearrange("p (kc t) -> p kc t", t=128), p.units))


def emit_proj_chunk(kb, wslot, wD_slab, hnT, p, mcols=128):
    src = wD_slab if mcols == 128 else V(wD_slab.ap[:, :, 0:mcols], wD_slab.units)
    kb.dma("pool", wslot[:, :, 0:mcols], src)
    for kc in range(16):
        kb.op("pe", "matmul", p[0:mcols, :], wslot[:, kc, 0:mcols], hnT[:, kc, :], start=(kc == 0), stop=(kc == 15))


GLA_PQ = "alt"
GLA_D1 = "dve"


def emit_gla(kb, C, decT, kT, qT, vhi, vlo, vrow0, nv, S, sbase, po, porow0, first, last, pbs, tmps, cnt):
    for v in range(nv):
        pb = pbs[cnt[0] % len(pbs)]
        d1, st, pq = tmps[cnt[0] % len(tmps)]
        cnt[0] += 1
        sel = C["rowsel"][:, vrow0 + v, :]
        kb.op("pe", "matmul", pb[:], sel, vhi, start=True, stop=False)
        kb.op("pe", "matmul", pb[:], sel, vlo, start=False, stop=True)
        if GLA_D1 == "dve":
            kb.op("dve", "tensor_tensor", d1[:], pb[:], kT, ALU.mult)
        else:
            kb.op("act", "copy", d1[:], pb[:])
            kb.op("pool", "tensor_tensor", d1[:], d1[:], kT, ALU.mult)
        sc = S.at(sbase + v)[:, sbase + v:sbase + v + 1]
        kb.op("dve", "tensor_tensor_scan", st[:], decT, d1[:], sc, ALU.mult, ALU.add)
        kb.op("act", "copy", sc, st[:, TS - 1:TS])
        pe_ = GLA_PQ if GLA_PQ in ("pool", "dve") else ("pool" if cnt[0] % 2 else "dve")
        kb.op(pe_, "tensor_tensor", pq[:], st[:], qT, ALU.mult)
        kb.op("pe", "matmul", po, C["colsel"][:, porow0 + v, :], pq[:],
              start=(first and v == 0), stop=(last and v == nv - 1))


def emit_hgrn_M(kb, C, L, lidx, hfullD, modD, ngD, wslabD, lbrawD, hggD, oTD):
    nseg = L // TS
    make_sel(kb, C)
    with kb.scope():
        lbr = kb.sbuf("lbr", [128, 4, 4], F32)
        lbe = kb.sbuf("lbe", [128, 4, 4], F32)
        lbs = kb.sbuf("lbs", [128, 4], F32)
        lb = kb.sbuf("lb", [128, 4], F32)
        oml = kb.sbuf("oml", [128, 4], F32)
        kb.dma("sp", lbr[:], lbrawD[:])
        kb.op("dve", "reduce_max", lbs[:], lbr[:], AX.X)
        kb.op("dve", "tensor_tensor", lbe[:], lbr[:], bc(lbs, (slice(None), slice(None)), [128, 4]) if False else
              V(lbs.h[:, :].unsqueeze(2).to_broadcast([128, 4, 4]), lbs.units), ALU.subtract)
        kb.op("act", "activation", lbe[:], lbe[:], AF.Exp)
        kb.op("dve", "reduce_sum", lbs[:], lbe[:], AX.X)
        kb.op("dve", "reciprocal", lbs[:], lbs[:])
        kb.op("dve", "reduce_sum", lb[:], lbe[:, :, 1:lidx + 1], AX.X)
        kb.op("dve", "tensor_tensor", lb[:], lb[:], lbs[:], ALU.mult)
        kb.op("dve", "tensor_scalar", oml[:], lb[:], -1.0, 1.0, ALU.mult, ALU.add)
        hgg = kb.sbuf("hgg", [128, 1], F32)
        kb.dma("sp", hgg[:], hggD[:])
        S = kb.sbuf("Sst", [128, 512], F32, nunits=512)
        kb.op("dve", "memset", S.all(), 0.0)
        hnT = kb.sbuf("hnT_m", [128, 16, TS], BF16)
        pss = [kb.psum(f"psA{i}", [128, 512], F32) for i in range(2)]
        pbs = [kb.psum(f"psB{i}", [128, 512], F32) for i in range(4)]
        po = kb.psum("psO", [128, 512], F32)
        pn = kb.psum("psN", [128, 512], F32)
        cnt = [0]
        pc = 0
        for seg in range(nseg):
            emit_norm_segment(kb, C, hfullD, seg, modD, (0, D, 2 * D), ngD, hnT, pss)
            with kb.scope():
                wsl = [kb.sbuf(f"wsl{i}", [128, 16, 128], BF16) for i in range(4)]
                decT = kb.sbuf("decT", [128, 4, TS], F32)
                kkT = kb.sbuf("kkT", [128, 4, TS], F32)
                qT = kb.sbuf("qT", [128, 4, TS], F32)
                ogs = kb.sbuf("ogs", [128, 4, TS], F32)
                ihi = kb.sbuf("ihi", [128, 4, TS], BF16, nunits=4)
                ilo = kb.sbuf("ilo", [128, 4, TS], BF16, nunits=4)
                tmps = [(kb.sbuf(f"d1_{i}", [128, TS], F32), kb.sbuf(f"st_{i}", [128, TS], F32),
                         kb.sbuf(f"pq_{i}", [128, TS], BF16)) for i in range(4)]
                sq = kb.sbuf("sq", [128, TS], F32)
                osb = kb.sbuf("osb", [128, TS], F32)
                rs = kb.sbuf("rs", [128, TS], F32)
                res = kb.sbuf("res", [128, TS], F32)
                for hh in range(4):
                    for ty in range(4):
                        p = pss[pc % 2]
                        w = wsl[pc % 4]
                        pc += 1
                        emit_proj_chunk(kb, w, wslabD[hh * 4 + ty], hnT, p)
                        if ty == 0:
                            kb.op("act", "activation", qT[:, hh, :], p[:], AF.Silu)
                        elif ty == 1:
                            kb.op("act", "activation", decT[:, hh, :], p[:], AF.Sigmoid)
                            kb.op("dve", "tensor_scalar", decT[:, hh, :], decT[:, hh, :], oml[:, hh:hh + 1], lb[:, hh:hh + 1],
                                  ALU.mult, ALU.add)
                            kb.op("dve", "tensor_scalar", kkT[:, hh, :], decT[:, hh, :], -1.0, 1.0, ALU.mult, ALU.add)
                        elif ty == 2:
                            kb.op("act", "copy", ihi.at(hh)[:, hh, :], p[:])
                            kb.op("dve", "tensor_tensor", ilo.at(hh)[:, hh, :], p[:], ihi.at(hh)[:, hh, :], ALU.subtract)
                        else:
                            kb.op("act", "activation", ogs[:, hh, :], p[:], AF.Silu)
                for hh in range(4):
                    emit_gla(kb, C, decT[:, hh, :], kkT[:, hh, :], qT[:, hh, :], ihi.at(hh)[:, hh, :], ilo.at(hh)[:, hh, :],
                             0, 128, S, hh * 128, po[:], 0, True, True, pbs, tmps, cnt)
                    kb.op("act", "activation", sq[:], po[:], AF.Square)
                    kb.op("dve", "tensor_copy", osb[:], po[:])
                    kb.op("pe", "matmul", pn[:], C["ones"][:], sq[:], start=True, stop=True)
                    kb.op("dve", "tensor_scalar", rs[:], pn[:], 1.0 / 128, RMS_EPS, ALU.mult, ALU.add)
                    kb.op("act", "activation", rs[:], rs[:], AF.Sqrt)
                    kb.op("dve", "reciprocal", rs[:], rs[:])
                    kb.op("dve", "tensor_tensor", res[:], osb[:], rs[:], ALU.mult)
                    kb.op("dve", "scalar_tensor_tensor", res[:], res[:], hgg[:, 0:1], ogs[:, hh, :], ALU.mult, ALU.mult)
                    kb.dma("sp", oTD[hh * 128:(hh + 1) * 128, seg * TS:(seg + 1) * TS], res[:])


def emit_outproj(kb, NT, hinD, hmidD, modD, g1off, srcs, woutD, nkc):
    with kb.scope():
        woutb = kb.sbuf("woutb", [128, nkc, D], BF16)
        wv = woutD.h.rearrange("(kc p) n -> p kc n", p=128)
        for k0 in range(0, nkc, 8):
            kb.dma("pool", woutb[:, k0:k0 + 8, :], V(wv[:, k0:k0 + 8, :], woutD.units))
        g1 = kb.sbuf("g1t", [128, D], F32)
        load_bc(kb, g1, modD, modD.h[0:1, g1off:g1off + D])
        mixs = [kb.sbuf(f"mixT{i}", [128, nkc, 512], BF16) for i in range(2 if nkc <= 16 else 1)]
        hts = [kb.sbuf(f"hto{i}", [128, D], F32) for i in range(2)]
        tmp = kb.sbuf("tmpo", [128, 512], F32)
        pss = [kb.psum(f"pso{i}", [128, 4, 512], F32, nunits=4) for i in range(2)]
        it = 0
        for tb in range(NT // 512):
            mx = mixs[tb % len(mixs)]
            k0 = 0
            for (sD, nch) in srcs:
                sv = sD.h.rearrange("(kc p) t -> p kc t", p=128)
                kb.dma("pool" if sD.h.dtype == F32 else "sp", mx[:, k0:k0 + nch, :], V(sv[:, :, tb * 512:(tb + 1) * 512], sD.units))
                k0 += nch
            for tt in range(4):
                gt = tb * 4 + tt
                ps = pss[it % 2]
                ht = hts[it % 2]
                it += 1
                kb.dma("sp", ht[:], hinD[gt * 128:(gt + 1) * 128, :])
                for db in range(4):
                    for kc in range(nkc):
                        kb.op("pe", "matmul", ps.at(db)[:, db, :], mx[:, kc, tt * 128:(tt + 1) * 128],
                              woutb[:, kc, db * 512:(db + 1) * 512], start=(kc == 0), stop=(kc == nkc - 1))
                for db in range(4):
                    sl = slice(db * 512, (db + 1) * 512)
                    kb.op("dve", "tensor_tensor", tmp[:], ps.at(db)[:, db, :], g1[:, sl], ALU.mult)
                    kb.op("dve", "tensor_tensor", ht[:, sl], ht[:, sl], tmp[:], ALU.add)
                kb.dma("sp", hmidD[gt * 128:(gt + 1) * 128, :], ht[:])


TWO_PI = 6.28318


def emit_ab_M(kb, C, L, hfullD, modD, ngD, wslabD, P, yaTD, ybTD):
    nseg = L // TS
    make_sel(kb, C)
    tabD = kb.dram("s5tab", [16, 128, 2, TS], F32)
    with kb.scope():
        ident = C["ident"]
        hnT = kb.sbuf("hnT_m", [128, 16, TS], BF16)
        S = kb.sbuf("Sst", [128, 512], F32, nunits=512)
        kb.op("dve", "memset", S.all(), 0.0)
        xpre = kb.sbuf("xpre", [128, 6, TS + 3], F32)
        kb.op("dve", "memset", xpre[:], 0.0)
        sprev = kb.sbuf("sprev", [128, 16, 2], F32)
        kb.op("dve", "memset", sprev[:], 0.0)
        convw = kb.sbuf("convw", [128, 6, 4], F32)
        convb = kb.sbuf("convb", [128, 6], F32)
        dtb = kb.sbuf("dtb", [8, 1], F32)
        negA = kb.sbuf("negA", [8, 1], F32)
        ssdD = kb.sbuf("ssdD", [128, 4], F32)
        normg = kb.sbuf("normg", [128, 4], F32)
        s5d = kb.sbuf("s5d", [32, 16], F32)
        for t_, n_ in ((convw, "convw"), (convb, "convb"), (dtb, "dtb"), (negA, "alog"), (ssdD, "ssdD"), (normg, "normg"), (s5d, "s5d")):
            kb.dma("sp", t_[:], P[n_][:])
        kb.op("act", "activation", negA[:], negA[:], AF.Exp)
        kb.op("dve", "tensor_scalar", negA[:], negA[:], -1.0, None, ALU.mult)
        sel8 = kb.sbuf("sel8", [8, 8, 128], F32)
        sel8b = kb.sbuf("sel8b", [8, 4, 128], F32)
        kb.op("dve", "tensor_copy", sel8[:], V(ident.h[0:8, 0:8].unsqueeze(2).to_broadcast([8, 8, 128]), ident.units))
        for c in range(4):
            kb.op("dve", "tensor_copy", sel8b[:, c, 0:64], sel8[:, 2 * c, 0:64])
            kb.op("dve", "tensor_copy", sel8b[:, c, 64:128], sel8[:, 2 * c + 1, 64:128])
        wz = kb.sbuf("wz", [128, 16, 512], BF16)
        wdt = kb.sbuf("wdt", [128, 16, 8], BF16)
        kb.dma("pool", wz[:], P["wz"][:])
        kb.dma("pool", wdt[:], P["wdt"][:])
        dtbB = kb.sbuf("dtbB", [128, 8], F32)
        negAB = kb.sbuf("negAB", [128, 8], F32)
        DB = kb.sbuf("DB", [128, 512], F32)
        NB = kb.sbuf("NB", [128, 512], F32)
        load_bc(kb, dtbB, P["dtbr"], P["dtbr"].h[0:1, :])
        load_bc(kb, negAB, P["alogr"], P["alogr"].h[0:1, :])
        load_bc(kb, DB, P["ssdDr"], P["ssdDr"].h[0:1, :])
        load_bc(kb, NB, P["normgr"], P["normgr"].h[0:1, :])
        kb.op("act", "activation", negAB[:], negAB[:], AF.Exp)
        kb.op("dve", "tensor_scalar", negAB[:], negAB[:], -1.0, None, ALU.mult)
        mle = kb.sbuf("mle", [128, 128], F32)
        ugt = kb.sbuf("ugt", [128, 128], F32)
        nmask = kb.sbuf("nmask", [128, 128], F32)
        kb.op("dve", "tensor_single_scalar", mle[:], C["jmp"][:], 0.0, ALU.is_ge)
        kb.op("dve", "tensor_single_scalar", ugt[:], C["jmp"][:], 0.0, ALU.is_lt)
        kb.op("dve", "tensor_scalar", nmask[:], ugt[:], -1.0e4, None, ALU.mult)
        Sf = kb.sbuf("Sf", [128, 512], F32)
        Sb = kb.sbuf("Sb", [128, 512], BF16)
        kb.op("dve", "memset", Sf[:], 0.0)
        kb.op("dve", "memset", Sb[:], 0.0)
        BbTr = kb.sbuf("BbTr", [32, 16, 128], BF16)
        BbTi = kb.sbuf("BbTi", [32, 16, 128], BF16)
        Creb = kb.sbuf("Creb", [128, 16, 32], BF16)
        nCimb = kb.sbuf("nCimb", [128, 16, 32], BF16)
        rho = kb.sbuf("rho", [128, 16], F32)
        cth = kb.sbuf("cth", [128, 16], F32)
        sth = kb.sbuf("sth", [128, 16], F32)
        pss = [kb.psum(f"psA{i}", [128, 512], F32) for i in range(2)]
        pbs = [kb.psum(f"psB{i}", [128, 512], F32) for i in range(2)]
        po = kb.psum("psO", [128, 512], F32)
        pn = kb.psum("psN", [128, 512], F32)
        with kb.scope():
            lre = kb.sbuf("lre", [128, 16], F32)
            lim = kb.sbuf("lim", [128, 16], F32)
            stp = kb.sbuf("stp", [128, 16], F32)
            thr = kb.sbuf("thr", [128, 16], F32)
            kb.dma("sp", lre[:], P["lre"][:])
            kb.dma("sp", lim[:], P["lim"][:])
            kb.dma("sp", stp[:], P["lstep"][:])
            kb.op("act", "activation", stp[:], stp[:], AF.Exp)
            kb.op("dve", "tensor_tensor", rho[:], lre[:], stp[:], ALU.mult)
            kb.op("act", "activation", rho[:], rho[:], AF.Exp)
            kb.op("dve", "tensor_tensor", thr[:], lim[:], stp[:], ALU.mult)
            kb.op("dve", "tensor_scalar", thr[:], thr[:], float(1.0 / (2 * np.pi)), None, ALU.mult)
            ioi = kb.sbuf("ioi", [128, TS], I32)
            iot = kb.sbuf("iot", [128, TS], F32)
            kb.op("pool", "iota", ioi[:], [[1, TS]], base=0, channel_multiplier=0)
            kb.op("dve", "tensor_copy", iot[:], ioi[:])
            ur = kb.sbuf("ur", [128, TS], F32)
            ki = kb.sbuf("ki", [128, TS], I32)
            kf = kb.sbuf("kf", [128, TS], F32)
            fr = kb.sbuf("fr", [128, TS], F32)
            mk = kb.sbuf("mk", [128, TS], F32)
            tabs = [kb.sbuf(f"tab{i}", [128, 2, TS], F32) for i in range(2)]
            for s in range(16):
                tb_ = tabs[s % 2]
                kb.op("dve", "tensor_scalar", ur[:], iot[:], thr[:, s:s + 1], None, ALU.mult)
                kb.op("dve", "tensor_copy", ki[:], ur[:])
                kb.op("dve", "tensor_copy", kf[:], ki[:])
                kb.op("dve", "tensor_tensor", fr[:], ur[:], kf[:], ALU.subtract)
                kb.op("act", "activation", tb_[:, 1, :], fr[:], AF.Sin, scale=TWO_PI)
                kb.op("dve", "tensor_scalar", fr[:], fr[:], 0.25, None, ALU.add)
                kb.op("dve", "tensor_single_scalar", mk[:], fr[:], 0.5, ALU.is_gt)
                kb.op("dve", "tensor_tensor", fr[:], fr[:], mk[:], ALU.subtract)
                kb.op("act", "activation", tb_[:, 0, :], fr[:], AF.Sin, scale=TWO_PI)
                kb.op("dve", "tensor_copy", cth[:, s:s + 1], tb_[:, 0, 1:2])
                kb.op("dve", "tensor_copy", sth[:, s:s + 1], tb_[:, 1, 1:2])
                kb.dma("sp", tabD[s], tb_[:])
            lbr = kb.sbuf("lbr_", [128, 16], F32)
            lbi = kb.sbuf("lbi_", [128, 16], F32)
            den = kb.sbuf("den", [128, 16], F32)
            t1 = kb.sbuf("t1_", [128, 16], F32)
            gre = kb.sbuf("gre", [128, 16], F32)
            gim = kb.sbuf("gim", [128, 16], F32)
            kb.op("dve", "tensor_tensor", lbr[:], rho[:], cth[:], ALU.mult)
            kb.op("dve", "tensor_scalar", lbr[:], lbr[:], -1.0, None, ALU.add)
            kb.op("dve", "tensor_tensor", lbi[:], rho[:], sth[:], ALU.mult)
            kb.op("dve", "tensor_tensor", den[:], lre[:], lre[:], ALU.mult)
            kb.op("dve", "tensor_tensor", t1[:], lim[:], lim[:], ALU.mult)
            kb.op("dve", "tensor_tensor", den[:], den[:], t1[:], ALU.add)
            kb.op("dve", "reciprocal", den[:], den[:])
            kb.op("dve", "tensor_tensor", gre[:], lbr[:], lre[:], ALU.mult)
            kb.op("dve", "tensor_tensor", t1[:], lbi[:], lim[:], ALU.mult)
            kb.op("dve", "tensor_tensor", gre[:], gre[:], t1[:], ALU.add)
            kb.op("dve", "tensor_tensor", gre[:], gre[:], den[:], ALU.mult)
            kb.op("dve", "tensor_tensor", gim[:], lbi[:], lre[:], ALU.mult)
            kb.op("dve", "tensor_tensor", t1[:], lbr[:], lim[:], ALU.mult)
            kb.op("dve", "tensor_tensor", gim[:], gim[:], t1[:], ALU.subtract)
            kb.op("dve", "tensor_tensor", gim[:], gim[:], den[:], ALU.mult)
            bre = kb.sbuf("bre", [128, 16, 32], F32)
            bim = kb.sbuf("bim", [128, 16, 32], F32)
            bbr = kb.sbuf("bbr", [128, 16, 32], F32)
            bbi = kb.sbuf("bbi", [128, 16, 32], F32)
            tt_ = kb.sbuf("tt_", [128, 16, 32], F32)
            kb.dma("sp", bre[:], P["bre"][:])
            kb.dma("sp", bim[:], P["bim"][:])
            greb = V(gre.h[:, :].unsqueeze(2).to_broadcast([128, 16, 32]), gre.units)
            gimb = V(gim.h[:, :].unsqueeze(2).to_broadcast([128, 16, 32]), gim.units)
            kb.op("dve", "tensor_tensor", bbr[:], bre[:], greb, ALU.mult)
            kb.op("dve", "tensor_tensor", tt_[:], bim[:], gimb, ALU.mult)
            kb.op("dve", "tensor_tensor", bbr[:], bbr[:], tt_[:], ALU.subtract)
            kb.op("dve", "tensor_tensor", bbi[:], bim[:], greb, ALU.mult)
            kb.op("dve", "tensor_tensor", tt_[:], bre[:], gimb, ALU.mult)
            kb.op("dve", "tensor_tensor", bbi[:], bbi[:], tt_[:], ALU.add)
            for s in range(16):
                for (src, dst) in ((bbr, BbTr), (bbi, BbTi)):
                    p = pss[s % 2]
                    kb.op("pe", "transpose", p[0:32, 0:128], src[:, s, :], ident[:])
                    kb.op("act", "copy", dst[:, s, :], p[0:32, 0:128])
            kb.dma("sp", bre[:], P["cre"][:])
            kb.dma("sp", bim[:], P["cim"][:])
            kb.op("dve", "tensor_copy", Creb[:], bre[:])
            kb.op("dve", "tensor_scalar", nCimb[:], bim[:], -1.0, None, ALU.mult)
        cnt = [0]
        pc = 0
        for seg in range(nseg):
            cs = slice(seg * TS, (seg + 1) * TS)
            emit_norm_segment(kb, C, hfullD, seg, modD, (0, D, 2 * D), ngD, hnT, pss)
            with kb.scope():
                wsl = [kb.sbuf(f"wsl{i}", [128, 16, 128], BF16) for i in range(2)]
                xc = kb.sbuf("xc", [128, 6, TS], F32)
                xcb = kb.sbuf("xcb", [128, 2, TS], BF16)
                ycv = kb.sbuf("ycv", [128, TS], F32)
                zs = kb.sbuf("zs", [128, 512], F32)
                dtt = kb.sbuf("dtt", [128, 8], F32)
                aa_ = kb.sbuf("a_", [128, 8], F32)
                acum = kb.sbuf("acum", [128, 8], F32)
                eacum = kb.sbuf("eacum", [128, 8], F32)
                wend = kb.sbuf("wend", [128, 8], F32)
                eatot = kb.sbuf("eatot", [128, 8], F32)
                xtm = kb.sbuf("xtm", [128, 8, 64], F32)
                xdtb = kb.sbuf("xdtb", [128, 8, 64], BF16)
                xwb = kb.sbuf("xwb", [128, 8, 64], BF16)
                btm = kb.sbuf("btm", [128, 128], BF16)
                Gs = kb.sbuf("Gs", [128, 128], F32)
                am = kb.sbuf("am", [128, 8, 128], F32)
                Ee = kb.sbuf("Ee", [128, 8, 128], F32)
                Wb = kb.sbuf("Wb", [128, 8, 128], BF16)
                yt = kb.sbuf("yt", [128, 8, 64], F32)
                vv = kb.sbuf("vv", [128, 512], F32)
                ssn = kb.sbuf("ssn", [128, 4], F32)
                ob = [kb.sbuf(f"ob{i}", [128, 512], F32) for i in range(2)]
                for c in range(6):
                    p = pss[pc % 2]; w = wsl[pc % 2]; pc += 1
                    emit_proj_chunk(kb, w, wslabD[4 + c], hnT, p)
                    kb.op("act", "copy", xpre[:, c, 3:TS + 3], p[:])
                    kb.op("dve", "tensor_scalar", ycv[:], xpre[:, c, 0:TS], convw[:, c, 0:1], None, ALU.mult)
                    for k in range(1, 4):
                        kb.op("dve", "scalar_tensor_tensor", ycv[:], xpre[:, c, k:k + TS], convw[:, c, k:k + 1], ycv[:], ALU.mult, ALU.add)
                    kb.op("act", "activation", xc[:, c, :], ycv[:], AF.Silu, bias=convb[:, c:c + 1])
                    kb.op("dve", "tensor_copy", xpre[:, c, 0:3], xpre[:, c, TS:TS + 3])
                kb.op("act", "copy", xcb[:], xc[:, 4:6, :])
                psm, pxy, pbg, pS_ = pn, po, pbs[0], pbs[1]
                pdf = kb.psum("pdf", [128, 8, 128], F32)
                for ck in range(4):
                    cc_ = slice(ck * 128, (ck + 1) * 128)
                    r0 = seg * TS + ck * 128
                    p = pss[pc % 2]; pc += 1
                    for kc in range(16):
                        kb.op("pe", "matmul", p[:], hnT[:, kc, cc_], wz[:, kc, :], start=(kc == 0), stop=(kc == 15))
                    kb.op("act", "activation", zs[:], p[:], AF.Silu)
                    for kc in range(16):
                        kb.op("pe", "matmul", psm[:, 0:8], hnT[:, kc, cc_], wdt[:, kc, :], start=(kc == 0), stop=(kc == 15))
                    kb.op("dve", "tensor_tensor", dtt[:], psm[:, 0:8], dtbB[:], ALU.add)
                    kb.op("act", "activation", dtt[:], dtt[:], AF.Exp)
                    kb.op("act", "activation", dtt[:], dtt[:], AF.Ln, bias=1.0)
                    kb.op("dve", "tensor_tensor", aa_[:], dtt[:], negAB[:], ALU.mult)
                    kb.op("pe", "matmul", psm[:, 8:16], mle[:], aa_[:], start=True, stop=True)
                    kb.op("pe", "matmul", psm[:, 16:24], C["ones"][:], aa_[:], start=True, stop=True)
                    kb.op("dve", "tensor_copy", acum[:], psm[:, 8:16])
                    kb.op("act", "activation", eacum[:], acum[:], AF.Exp)
                    kb.op("dve", "tensor_tensor", wend[:], psm[:, 16:24], acum[:], ALU.subtract)
                    kb.op("act", "activation", wend[:], wend[:], AF.Exp)
                    kb.op("act", "activation", eatot[:], psm[:, 16:24], AF.Exp)
                    kb.op("dve", "tensor_tensor", wend[:], wend[:], dtt[:], ALU.mult)
                    for c in range(4):
                        kb.op("pe", "transpose", pxy[:, c * 128:(c + 1) * 128], xc[:, c, cc_], ident[:])
                    kb.op("act", "copy", V(xtm.h.rearrange("p a b -> p (a b)"), xtm.units), pxy[:])
                    kb.op("dve", "tensor_tensor", xdtb[:], xtm[:], V(dtt.h[:, :].unsqueeze(2).to_broadcast([128, 8, 64]), dtt.units), ALU.mult)
                    kb.op("dve", "tensor_tensor", xwb[:], xtm[:], V(wend.h[:, :].unsqueeze(2).to_broadcast([128, 8, 64]), wend.units), ALU.mult)
                    kb.op("pe", "transpose", pbg[:, 0:128], xc[:, 4, cc_], ident[:])
                    kb.op("act", "copy", btm[:], pbg[:, 0:128])
                    kb.op("pe", "matmul", pbg[:, 128:256], xcb[:, 0, cc_], xcb[:, 1, cc_], start=True, stop=True)
                    kb.op("act", "copy", Gs[:], pbg[:, 128:256])
                    kb.op("dve", "tensor_tensor", am[:], V(aa_.h[:, :].unsqueeze(2).to_broadcast([128, 8, 128]), aa_.units),
                          V(mle.h[:, :].unsqueeze(1).to_broadcast([128, 8, 128]), mle.units), ALU.mult)
                    for hb in range(2):
                        kb.op("pe", "matmul", V(pdf.h[:, hb * 4:(hb + 1) * 4, :], pdf.units), ugt[:], am[:, hb * 4:(hb + 1) * 4, :], start=True, stop=True)
                    for hb in range(2):
                        kb.op("dve", "tensor_tensor", Ee[:, hb * 4:(hb + 1) * 4, :], V(pdf.h[:, hb * 4:(hb + 1) * 4, :], pdf.units),
                              V(nmask.h[:, :].unsqueeze(1).to_broadcast([128, 4, 128]), nmask.units), ALU.add)
                    kb.op("act", "activation", Ee[:], Ee[:], AF.Exp)
                    kb.op("dve", "tensor_tensor", Wb[:], Ee[:], V(Gs.h[:, :].unsqueeze(1).to_broadcast([128, 8, 128]), Gs.units), ALU.mult)
                    for hh in range(8):
                        kb.op("pe", "matmul", pxy[:, hh * 64:(hh + 1) * 64], Wb[:, hh, :], xdtb[:, hh, :], start=True, stop=True)
                    kb.op("pe", "matmul", pbg[:], xcb[:, 1, cc_], Sb[:], start=True, stop=True)
                    kb.op("pe", "matmul", pS_[:], btm[:], V(xwb.h.rearrange("p a b -> p (a b)"), xwb.units), start=True, stop=True)
                    kb.op("dve", "tensor_tensor", yt[:], V(pbg.h[:, :].rearrange("p (a b) -> p a b", b=64), pbg.units),
                          V(eacum.h[:, :].unsqueeze(2).to_broadcast([128, 8, 64]), eacum.units), ALU.mult)
                    ytf = V(yt.h.rearrange("p a b -> p (a b)"), yt.units)
                    kb.op("dve", "tensor_tensor", ytf, ytf, pxy[:], ALU.add)
                    xtf = V(xtm.h.rearrange("p a b -> p (a b)"), xtm.units)
                    kb.op("dve", "tensor_tensor", vv[:], xtf, DB[:], ALU.mult)
                    kb.op("dve", "tensor_tensor", vv[:], vv[:], ytf, ALU.add)
                    S3 = V(Sf.h[:, :].rearrange("p (a b) -> p a b", b=64), Sf.units)
                    kb.op("dve", "tensor_tensor", S3, S3, V(eatot.h[:, :].unsqueeze(2).to_broadcast([128, 8, 64]), eatot.units), ALU.mult)
                    kb.op("dve", "tensor_tensor", Sf[:], Sf[:], pS_[:], ALU.add)
                    kb.op("act", "copy", Sb[:], Sf[:])
                    kb.op("dve", "tensor_tensor", vv[:], vv[:], zs[:], ALU.mult)
                    o_ = ob[ck % 2]
                    kb.op("act", "activation", o_[:], vv[:], AF.Square, accum_out=ssn[:, 0:1])
                    kb.op("dve", "tensor_scalar", ssn[:, 1:2], ssn[:, 0:1], 1.0 / 512, 1e-5, ALU.mult, ALU.add)
                    kb.op("act", "activation", ssn[:, 2:3], ssn[:, 1:2], AF.Sqrt)
                    kb.op("dve", "reciprocal", ssn[:, 3:4], ssn[:, 2:3])
                    kb.op("dve", "scalar_tensor_tensor", o_[:], vv[:], ssn[:, 3:4], NB[:], ALU.mult, ALU.mult)
                    kb.dma("sp", yaTD[r0:r0 + 128, :], o_[:])
            with kb.scope():
                wsl = [kb.sbuf(f"wsl{i}", [128, 16, 128], BF16) for i in range(4)]
                tabs = [kb.sbuf(f"tabl{i}", [128, 2, TS], F32) for i in range(2)]
                uf = [kb.sbuf(f"uf{i}", [32, TS], F32) for i in range(2)]
                ub = [kb.sbuf(f"ub{i}", [32, TS], BF16) for i in range(2)]
                bur = kb.sbuf("bur", [128, TS], F32)
                bui = kb.sbuf("bui", [128, TS], F32)
                m1 = kb.sbuf("m1", [128, TS], F32)
                m2 = kb.sbuf("m2", [128, TS], F32)
                aa = kb.sbuf("aa", [128, TS], F32)
                bb = kb.sbuf("bb", [128, TS], F32)
                rhoB = kb.sbuf("rhoB", [128, TS], F32)
                onesT = kb.sbuf("onesT", [128, TS], F32)
                kb.op("pool", "memset", onesT[:], 1.0)
                wre = kb.sbuf("wre", [128, TS], F32)
                wim = kb.sbuf("wim", [128, TS], F32)
                sre = kb.sbuf("sre", [128, TS], F32)
                sim = kb.sbuf("sim", [128, TS], F32)
                sreb = kb.sbuf("sreb", [128, TS], BF16)
                simb = kb.sbuf("simb", [128, TS], BF16)
                ini = kb.sbuf("ini", [128, 4], F32)
                yb = [kb.sbuf(f"yb{i}", [32, TS], F32) for i in range(2)]
                for s in range(16):
                    p = pss[pc % 2]; w = wsl[pc % 4]; pc += 1
                    tb_ = tabs[s % 2]
                    ct_, st_ = tb_[:, 0, :], tb_[:, 1, :]
                    kb.dma("sp", tb_[:], tabD[s])
                    emit_proj_chunk(kb, w, wslabD[11 + s], hnT, p, mcols=32)
                    kb.op("act", "copy", uf[s % 2][:], p[0:32, :])
                    kb.op("dve", "tensor_copy", ub[s % 2][:], p[0:32, :])
                    kb.op("pe", "matmul", pbs[0][:], BbTr[:, s, :], ub[s % 2][:], start=True, stop=True)
                    kb.op("pe", "matmul", pbs[1][:], BbTi[:, s, :], ub[s % 2][:], start=True, stop=True)
                    kb.op("act", "copy", bur[:], pbs[0][:])
                    kb.op("act", "copy", bui[:], pbs[1][:])
                    kb.op("pool", "tensor_tensor", m1[:], bur[:], ct_, ALU.mult)
                    kb.op("pool", "tensor_tensor", m2[:], bui[:], st_, ALU.mult)
                    kb.op("pool", "tensor_tensor", aa[:], m1[:], m2[:], ALU.add)
                    kb.op("pool", "tensor_tensor", m1[:], bui[:], ct_, ALU.mult)
                    kb.op("pool", "tensor_tensor", m2[:], bur[:], st_, ALU.mult)
                    kb.op("pool", "tensor_tensor", bb[:], m1[:], m2[:], ALU.subtract)
                    kb.op("pool", "tensor_scalar", rhoB[:], onesT[:], rho[:, s:s + 1], None, ALU.mult)
                    kb.op("dve", "tensor_tensor", ini[:, 2:3], sprev[:, s, 1:2], sth[:, s:s + 1], ALU.mult)
                    kb.op("dve", "scalar_tensor_tensor", ini[:, 0:1], sprev[:, s, 0:1], cth[:, s:s + 1], ini[:, 2:3], ALU.mult, ALU.subtract)
                    kb.op("dve", "tensor_tensor", ini[:, 3:4], sprev[:, s, 1:2], cth[:, s:s + 1], ALU.mult)
                    kb.op("dve", "scalar_tensor_tensor", ini[:, 1:2], sprev[:, s, 0:1], sth[:, s:s + 1], ini[:, 3:4], ALU.mult, ALU.add)
                    kb.op("dve", "tensor_tensor_scan", wre[:], rhoB[:], aa[:], ini[:, 0:1], ALU.mult, ALU.add)
                    kb.op("dve", "tensor_tensor_scan", wim[:], rhoB[:], bb[:], ini[:, 1:2], ALU.mult, ALU.add)
                    kb.op("pool", "tensor_tensor", m1[:], wre[:], ct_, ALU.mult)
                    kb.op("pool", "tensor_tensor", m2[:], wim[:], st_, ALU.mult)
                    kb.op("pool", "tensor_tensor", sre[:], m1[:], m2[:], ALU.subtract)
                    kb.op("pool", "tensor_tensor", m1[:], wre[:], st_, ALU.mult)
                    kb.op("pool", "tensor_tensor", m2[:], wim[:], ct_, ALU.mult)
                    kb.op("pool", "tensor_tensor", sim[:], m1[:], m2[:], ALU.add)
                    kb.op("act", "copy", sprev[:, s, 0:1], sre[:, TS - 1:TS])
                    kb.op("act", "copy", sprev[:, s, 1:2], sim[:, TS - 1:TS])
                    kb.op("act", "copy", sreb[:], sre[:])
                    kb.op("act", "copy", simb[:], sim[:])
                    kb.op("pe", "matmul", po[0:32, :], Creb[:, s, :], sreb[:], start=True, stop=False)
                    kb.op("pe", "matmul", po[0:32, :], nCimb[:, s, :], simb[:], start=False, stop=True)
                    kb.op("dve", "scalar_tensor_tensor", yb[s % 2][:], uf[s % 2][:], s5d[:, s:s + 1], po[0:32, :], ALU.mult, ALU.add)
                    kb.dma("sp", ybTD[s * 32:(s + 1) * 32, cs], yb[s % 2][:])


def emit_glu(kb, NT, ybTD, gluwD, glubD, ybfD):
    with kb.scope():
        gw = kb.sbuf("gluw", [128, 16, D], BF16)
        wv = gluwD.h.rearrange("(kc p) n -> p kc n", p=128)
        for k0 in range(0, 16, 8):
            kb.dma("pool", gw[:, k0:k0 + 8, :], V(wv[:, k0:k0 + 8, :], gluwD.units))
        gb = kb.sbuf("glub", [128, 16], F32)
        kb.dma("sp", gb[:], glubD[:])
        yb = kb.sbuf("ybl", [128, 16, 512], F32)
        glT = kb.sbuf("glT", [128, 16, 512], BF16)
        obf = kb.sbuf("obf", [128, 16, 512], BF16)
        t1 = [kb.sbuf(f"gt1_{i}", [128, 512], F32) for i in range(2)]
        t2 = [kb.sbuf(f"gt2_{i}", [128, 512], F32) for i in range(2)]
        pss = [kb.psum(f"psg{i}", [128, 512], F32) for i in range(2)]
        yv = ybTD.h.rearrange("(kc p) t -> p kc t", p=128)
        ov = ybfD.h.rearrange("(kc p) t -> p kc t", p=128)
        for tb in range(NT // 512):
            cs = slice(tb * 512, (tb + 1) * 512)
            kb.dma("sp", yb[:], V(yv[:, :, cs], ybTD.units))
            for kc in range(16):
                a, b = t1[kc % 2], t2[kc % 2]
                kb.op("act", "activation", a[:], yb[:, kc, :], AF.Square)
                kb.op("dve", "tensor_scalar", a[:], a[:], 0.044715, 1.0, ALU.mult, ALU.add)
                kb.op("pool", "tensor_tensor", a[:], a[:], yb[:, kc, :], ALU.mult)
                kb.op("act", "activation", b[:], a[:], AF.Sigmoid, scale=1.5957691216057308)
                kb.op("pool", "tensor_tensor", glT[:, kc, :], b[:], yb[:, kc, :], ALU.mult)
            for m in range(16):
                p = pss[m % 2]
                a = t1[m % 2]
                for kc in range(16):
                    kb.op("pe", "matmul", p[:], gw[:, kc, m * 128:(m + 1) * 128], glT[:, kc, :], start=(kc == 0), stop=(kc == 15))
                kb.op("act", "activation", a[:], p[:], AF.Sigmoid, bias=gb[:, m:m + 1])
                kb.op("dve", "tensor_tensor", obf[:, m, :], a[:], yb[:, m, :], ALU.mult)
            kb.dma("sp", V(ov[:, :, cs], ybfD.units), obf[:])


def lay_c(c_b):
    return np.ascontiguousarray(c_b.reshape(16, 128).T)
def lay_w1(w1_l):
    NE = w1_l.shape[0]
    g = w1_l[:, :, 0::2].reshape(NE, 16, 128, 6, 128)
    u = w1_l[:, :, 1::2].reshape(NE, 16, 128, 6, 128)
    gu = np.concatenate([g, u], axis=-1)
    return np.ascontiguousarray(gu.transpose(0, 3, 2, 1, 4))
def lay_b1(b1_l):
    NE = b1_l.shape[0]
    g = b1_l[:, 0::2].reshape(NE, 6, 128)
    u = b1_l[:, 1::2].reshape(NE, 6, 128)
    gu = np.concatenate([g, u], axis=1)
    return np.ascontiguousarray(gu.transpose(2, 0, 1))
def lay_w2(w2_l):
    NE = w2_l.shape[0]
    return np.ascontiguousarray(w2_l.reshape(NE, 3, 2, 128, 2048).transpose(0, 1, 3, 2, 4))
def lay_slabs(w, cols_list):
    out = np.zeros((len(cols_list), 128, 16, 128), np.float32)
    for i, cols in enumerate(cols_list):
        blk = w[:, cols].reshape(16, 128, len(cols))
        out[i, :, :, :len(cols)] = blk.transpose(1, 0, 2)
    return out
def hgrn_slabs(w_in, j):
    cl = []
    for hh in range(4):
        hd = 4 * j + hh
        for ty in range(4):
            cl.append(np.arange(ty * 2048 + hd * 128, ty * 2048 + (hd + 1) * 128))
    return lay_slabs(w_in, cl)
def hgrn_lbraw(lb_all, j):
    x = lb_all[:, j * 512:(j + 1) * 512].reshape(4, 4, 128)
    return np.ascontiguousarray(x.transpose(2, 1, 0))
def ab_slabs(w_in, g):
    cl = []
    for c in range(4): cl.append(g * 512 + c * 128 + np.arange(128))
    for c in range(4): cl.append(2048 + g * 512 + c * 128 + np.arange(128))
    cl.append(4096 + g * 128 + np.arange(128))
    cl.append(4608 + g * 128 + np.arange(128))
    cl.append(5120 + g * 8 + np.arange(8))
    for s in range(16): cl.append(5152 + g * 512 + s * 32 + np.arange(32))
    return lay_slabs(w_in, cl)
def ab_params(inp, i, g):
    P = {}
    cw = inp["ab_conv_w"][i]; cb = inp["ab_conv_b"][i]
    chs = [g * 512 + c * 128 + np.arange(128) for c in range(4)] + [2048 + g * 128 + np.arange(128), 2560 + g * 128 + np.arange(128)]
    P["convw"] = np.ascontiguousarray(np.stack([cw[:, ch].T for ch in chs], axis=1))
    P["convb"] = np.ascontiguousarray(np.stack([cb[ch] for ch in chs], axis=1))
    P["dtb"] = np.ascontiguousarray(inp["ssd_dt_bias"][i][g * 8:(g + 1) * 8, None])
    P["alog"] = np.ascontiguousarray(inp["ssd_a_log"][i][g * 8:(g + 1) * 8, None])
    heads = g * 8 + (np.arange(512) // 64)
    P["ssdD"] = np.ascontiguousarray(inp["ssd_d"][i][heads].reshape(4, 128).T)
    P["normg"] = np.ascontiguousarray(inp["ssd_norm_g"][i][g * 512:(g + 1) * 512].reshape(4, 128).T)
    G = (32 * g + np.arange(32)).reshape(16, 2)
    def st(a):
        return np.ascontiguousarray(a[G].transpose(1, 2, 0).reshape(128, 16))
    P["lre"] = st(inp["s5_lam_re"][i]); P["lim"] = st(inp["s5_lam_im"][i]); P["lstep"] = st(inp["s5_log_step"][i])
    def bd(a):
        out = np.zeros((2, 64, 16, 2, 16), np.float32)
        for gg in range(2):
            out[gg, :, :, gg, :] = a[:, gg].transpose(1, 0, 2)
        return out.reshape(128, 16, 32)
    P["bre"] = bd(inp["s5_b_re"][i][G]); P["bim"] = bd(inp["s5_b_im"][i][G])
    P["cre"] = bd(inp["s5_c_re"][i][G].transpose(0, 1, 3, 2)); P["cim"] = bd(inp["s5_c_im"][i][G].transpose(0, 1, 3, 2))
    P["s5d"] = np.ascontiguousarray(inp["s5_d"][i][g * 512:(g + 1) * 512].reshape(16, 32).T)
    w_in = inp["ab_w_in"][i]
    P["wz"] = np.ascontiguousarray(w_in[:, g * 512:(g + 1) * 512].reshape(16, 128, 512).transpose(1, 0, 2))
    P["wdt"] = np.ascontiguousarray(w_in[:, 5120 + g * 8:5120 + (g + 1) * 8].reshape(16, 128, 8).transpose(1, 0, 2))
    P["dtbr"] = np.ascontiguousarray(inp["ssd_dt_bias"][i][None, g * 8:(g + 1) * 8])
    P["alogr"] = np.ascontiguousarray(inp["ssd_a_log"][i][None, g * 8:(g + 1) * 8])
    P["ssdDr"] = np.ascontiguousarray(inp["ssd_d"][i][heads][None, :])
    P["normgr"] = np.ascontiguousarray(inp["ssd_norm_g"][i][None, g * 512:(g + 1) * 512])
    return P
AB_PSHAPES = dict(convw=[128, 6, 4], convb=[128, 6], dtb=[8, 1], alog=[8, 1], ssdD=[128, 4], normg=[128, 4],
                  lre=[128, 16], lim=[128, 16], lstep=[128, 16], bre=[128, 16, 32], bim=[128, 16, 32],
                  cre=[128, 16, 32], cim=[128, 16, 32], s5d=[32, 16],
                  wz=[128, 16, 512], wdt=[128, 16, 8], dtbr=[1, 8], alogr=[1, 8], ssdDr=[1, 512], normgr=[1, 512])


NT_CORE = 2048
SEQ = 8192
_PROGS = {}


def _common(kb):
    ct = kb.dram("ct", [128, 16], F32, kind="ExternalInput")
    adaw = kb.dram("adaw", [D, 6 * D], F32, kind="ExternalInput")
    adab = kb.dram("adab", [1, 6 * D], F32, kind="ExternalInput")
    modD = kb.dram("modD", [1, 6 * D], F32)
    C = make_consts(kb)
    emit_mod(kb, ct, adaw, adab, modD, 6 * D)
    return C, modD, ct


def _build_M_hg(lidx):
    nc = bass.Bass("TRN2", target_bir_lowering=False)
    with ExitStack() as st:
        kb = KB(nc, st)
        hfull = kb.dram("hfull", [SEQ, D], F32, kind="ExternalInput")
        ng = kb.dram("ng", [1, D], F32, kind="ExternalInput")
        wsl = kb.dram("wsl", [16, 128, 16, 128], F32, kind="ExternalInput")
        lbraw = kb.dram("lbraw", [128, 4, 4], F32, kind="ExternalInput")
        hgg = kb.dram("hgg", [128, 1], F32, kind="ExternalInput")
        oT = kb.dram("oT", [512, SEQ], F32, kind="ExternalOutput")
        C, modD, ct = _common(kb)
        emit_hgrn_M(kb, C, SEQ, lidx, hfull, modD, ng, wsl, lbraw, hgg, oT)
        kb.finish()
    return nc


def _build_M_ab():
    nc = bass.Bass("TRN2", target_bir_lowering=False)
    with ExitStack() as st:
        kb = KB(nc, st)
        hfull = kb.dram("hfull", [SEQ, D], F32, kind="ExternalInput")
        ng = kb.dram("ng", [1, D], F32, kind="ExternalInput")
        wsl = kb.dram("wsl", [27, 128, 16, 128], F32, kind="ExternalInput")
        P = {k: kb.dram("p_" + k, shp, F32, kind="ExternalInput") for k, shp in AB_PSHAPES.items()}
        yaT = kb.dram("yaT", [SEQ, 512], F32, kind="ExternalOutput")
        ybT = kb.dram("ybT", [512, SEQ], F32, kind="ExternalOutput")
        C, modD, ct = _common(kb)
        emit_ab_M(kb, C, SEQ, hfull, modD, ng, wsl, P, yaT, ybT)
        kb.finish()
    return nc


def _build_P(kind, final):
    nc = bass.Bass("TRN2", target_bir_lowering=False)
    with ExitStack() as st:
        kb = KB(nc, st)
        NT = NT_CORE
        hin = kb.dram("hin", [NT, D], F32, kind="ExternalInput")
        ng = kb.dram("ng", [1, D], F32, kind="ExternalInput")
        rw = kb.dram("rw", [D, NE], F32, kind="ExternalInput")
        rb = kb.dram("rb", [1, NE], F32, kind="ExternalInput")
        w1 = kb.dram("w1", [NE, 6, 128, 16, 256], F32, kind="ExternalInput")
        b1 = kb.dram("b1", [128, NE, 12], F32, kind="ExternalInput")
        w2 = kb.dram("w2", [NE, 3, 128, 2, D], F32, kind="ExternalInput")
        b2 = kb.dram("b2", [NE, D], F32, kind="ExternalInput")
        hout = kb.dram("hout", [NT, D], F32, kind="ExternalOutput")
        hmid = kb.dram("hmid", [NT, D], F32)
        C, modD, ct = _common(kb)
        if kind == "ab":
            yaT = kb.dram("yaT", [D, NT], F32, kind="ExternalInput")
            ybT = kb.dram("ybT", [D, NT], F32, kind="ExternalInput")
            gluw = kb.dram("gluw", [D, D], F32, kind="ExternalInput")
            glub = kb.dram("glub", [128, 16], F32, kind="ExternalInput")
            wout = kb.dram("wout", [2 * D, D], F32, kind="ExternalInput")
            ybfD = kb.dram("ybfD", [D, NT], BF16)
            emit_glu(kb, NT, ybT, gluw, glub, ybfD)
            emit_outproj(kb, NT, hin, hmid, modD, 2 * D, [(yaT, 16), (ybfD, 16)], wout, 32)
        else:
            oT = kb.dram("oT", [D, NT], F32, kind="ExternalInput")
            wout = kb.dram("wout", [D, D], F32, kind="ExternalInput")
            emit_outproj(kb, NT, hin, hmid, modD, 2 * D, [(oT, 16)], wout, 16)
        fin = None
        if final:
            faw = kb.dram("faw", [D, 2 * D], F32, kind="ExternalInput")
            fab = kb.dram("fab", [1, 2 * D], F32, kind="ExternalInput")
            fg = kb.dram("fg", [1, D], F32, kind="ExternalInput")
            fmodD = kb.dram("fmodD", [1, 2 * D], F32)
            emit_mod(kb, ct, faw, fab, fmodD, 2 * D)
            fin = (fmodD, fg, hout)
        emit_moe(kb, C, NT, hmid, hout, modD, (3 * D, 4 * D, 5 * D), ng, rw, rb, w1, b1, w2, b2, fin=fin)
        kb.finish()
    return nc


def _prog(key, fn, *a):
    if key not in _PROGS:
        _PROGS[key] = fn(*a)
    return _PROGS[key]


def kernel(**inp):
    inp = {k: np.asarray(v) for k, v in inp.items()}
    x = inp["x"].astype(np.float32, copy=False)
    c = inp["c"].astype(np.float32, copy=False)
    B, L, _ = x.shape
    nq = L // NT_CORE
    ncore = B * nq
    A = np.ascontiguousarray
    h = [A(x[cc // nq, (cc % nq) * NT_CORE:(cc % nq + 1) * NT_CORE]) for cc in range(ncore)]
    cts = [lay_c(c[b]) for b in range(B)]
    depth = inp["ada_w"].shape[0]
    for l in range(depth):
        i = l // 2
        final = (l == depth - 1)
        adaw, adab = A(inp["ada_w"][l]), A(inp["ada_b"][l][None])
        hfull = [np.concatenate(h[b * nq:(b + 1) * nq], axis=0) for b in range(B)]
        if l % 2 == 0:
            nc = _prog("M_ab", _build_M_ab)
            in_maps = []
            for cc in range(ncore):
                b, g = cc // 4, cc % 4
                d = dict(hfull=hfull[b], ct=cts[b], adaw=adaw, adab=adab, ng=A(inp["norm1_g"][l][None]),
                         wsl=ab_slabs(inp["ab_w_in"][i], g))
                for k, v in ab_params(inp, i, g).items():
                    d["p_" + k] = v
                in_maps.append(d)
            res = run_bass_kernel_spmd(nc, in_maps, core_ids=list(range(ncore)))
            yaT = [np.concatenate([res.results[b * 4 + g]["yaT"].T for g in range(4)], axis=0) for b in range(B)]
            ybT = [np.concatenate([res.results[b * 4 + g]["ybT"] for g in range(4)], axis=0) for b in range(B)]
            extra = lambda b, q: dict(yaT=A(yaT[b][:, q * NT_CORE:(q + 1) * NT_CORE]), ybT=A(ybT[b][:, q * NT_CORE:(q + 1) * NT_CORE]),
                                      gluw=A(inp["s5_glu_w"][i]), glub=A(inp["s5_glu_b"][i].reshape(16, 128).T),
                                      wout=A(inp["ab_w_out"][i]))
            kind = "ab"
        else:
            nc = _prog(("M_hg", l), _build_M_hg, l)
            in_maps = []
            for cc in range(ncore):
                b, j = cc // 4, cc % 4
                in_maps.append(dict(hfull=hfull[b], ct=cts[b], adaw=adaw, adab=adab, ng=A(inp["norm1_g"][l][None]),
                                    wsl=hgrn_slabs(inp["hg_w_in"][i], j), lbraw=hgrn_lbraw(inp["hg_lower_bounds"], j),
                                    hgg=A(inp["hg_norm_g"][i][:, None])))
            res = run_bass_kernel_spmd(nc, in_maps, core_ids=list(range(ncore)))
            oT = [np.concatenate([res.results[b * 4 + j]["oT"] for j in range(4)], axis=0) for b in range(B)]
            extra = lambda b, q: dict(oT=A(oT[b][:, q * NT_CORE:(q + 1) * NT_CORE]), wout=A(inp["hg_w_out"][i]))
            kind = "hg"
        del res
        nc = _prog(("P", kind, final), _build_P, kind, final)
        shared = dict(adaw=adaw, adab=adab, ng=A(inp["norm2_g"][l][None]), rw=A(inp["moe_router_w"][l]),
                      rb=A(inp["moe_router_b"][l][None]), w1=lay_w1(inp["moe_w1"][l]), b1=lay_b1(inp["moe_b1"][l]),
                      w2=lay_w2(inp["moe_w2"][l]), b2=A(inp["moe_b2"][l]))
        if final:
            shared.update(faw=A(inp["final_ada_w"]), fab=A(inp["final_ada_b"][None]), fg=A(inp["final_norm_g"][None]))
        in_maps = []
        for cc in range(ncore):
            b, q = cc // nq, cc % nq
            d = dict(shared, hin=h[cc], ct=cts[b])
            d.update(extra(b, q))
            in_maps.append(d)
        res = run_bass_kernel_spmd(nc, in_maps, core_ids=list(range(ncore)))
        h = [np.asarray(res.results[cc]["hout"]) for cc in range(ncore)]
        del res
    out = np.stack([np.concatenate(h[b * nq:(b + 1) * nq], axis=0) for b in range(B)], axis=0)
    return out.astype(np.float32)
```

```python
from contextlib import ExitStack
from concourse.bass_utils import run_bass_kernel_spmd
import numpy as np
import concourse.bass as bass
import concourse.mybir as mybir

F32 = mybir.dt.float32
BF16 = mybir.dt.bfloat16
AF = mybir.ActivationFunctionType
ALU = mybir.AluOpType
AX = mybir.AxisListType


class Unit:
    __slots__ = ("last_write", "reads", "name", "excl")

    def __init__(self, name=""):
        self.excl = False
        self.last_write = None
        self.reads = []
        self.name = name


class V:
    __slots__ = ("ap", "units")

    def __init__(self, ap, units):
        self.ap = ap
        self.units = units


class T:
    def __init__(self, handle, name, nunits=1):
        self.h = handle
        self.name = name
        self.units = [Unit(f"{name}.{i}") for i in range(nunits)]

    def __getitem__(self, idx):
        return V(self.h[idx], [self.units[0]])

    def at(self, i):
        return _At(self, i)

    def all(self, idx=slice(None)):
        return V(self.h[idx], list(self.units))

    def ap(self):
        return self.h


class _At:
    def __init__(self, t, i):
        self.t, self.i = t, i

    def __getitem__(self, idx):
        return V(self.t.h[idx], [self.t.units[self.i]])


class Op:
    __slots__ = ("eng", "emit", "waits", "idx", "needed", "num", "sem", "is_dma")

    def __init__(self, eng, emit, is_dma=False):
        self.eng = eng
        self.emit = emit
        self.waits = []
        self.idx = -1
        self.needed = False
        self.num = -1
        self.sem = None
        self.is_dma = is_dma


class _Scope:
    def __init__(self, kb):
        self.kb = kb

    def __enter__(self):
        self.mark = (self.kb.sb_off, self.kb.ps_off)
        return self

    def __exit__(self, *a):
        self.kb.barrier()
        self.kb.sb_off, self.kb.ps_off = self.mark
        return False


class KB:
    CE = ("pe", "dve", "act", "pool", "sp")

    def __init__(self, nc, stack, n_dma_sems=24):
        self.nc = nc
        self.stack = stack
        self.eng = {"pe": nc.tensor, "dve": nc.vector, "act": nc.scalar,
                    "pool": nc.gpsimd, "sp": nc.sync}
        self.sem = {e: stack.enter_context(nc.semaphore(f"s_{e}")) for e in self.CE}
        self.dma_sems = [stack.enter_context(nc.semaphore(f"s_dma{i}")) for i in range(n_dma_sems)]
        self.dma_last = [None] * n_dma_sems
        self.dma_rr = 0
        self.ops = []
        self.stream_len = {e: 0 for e in self.CE}
        self.seen = {e: {} for e in self.CE}
        self.sb_big = None
        self.ps_big = None
        self.sb_off = 0
        self.ps_off = 0
        self.sb_peak = 0
        ses = True
        self.same_engine_sync = {"pe": False, "dve": ses, "act": ses, "pool": ses, "sp": True}

    SB_WORDS = 51200
    PS_WORDS = 4096

    def scope(self):
        return _Scope(self)

    def _carve(self, big, off, shape, dtype):
        n = 1
        for d in shape[1:]:
            n *= d
        esz = mybir.dt.size(dtype)
        words = (n * esz + 3) // 4
        words = (words + 7) // 8 * 8
        ap = big[0:shape[0], off:off + words]
        if dtype != F32:
            ap = ap.bitcast(dtype)
        ap = ap[:, 0:n]
        if len(shape) > 2:
            names = " ".join(f"d{i}" for i in range(1, len(shape)))
            kw = {f"d{i}": shape[i] for i in range(1, len(shape))}
            ap = ap.rearrange(f"p ({names}) -> p {names}", **kw)
        return ap, words

    def sbuf(self, name, shape, dtype, nunits=1):
        if self.sb_big is None:
            self.sb_big = self.stack.enter_context(self.nc.sbuf_tensor("sb_big", [128, self.SB_WORDS], F32))
        ap, words = self._carve(self.sb_big, self.sb_off, shape, dtype)
        self.sb_off += words
        self.sb_peak = max(self.sb_peak, self.sb_off)
        assert self.sb_off <= self.SB_WORDS, f"SBUF overflow allocating {name}: {self.sb_off * 4} B"
        return T(ap, name, nunits)

    def psum(self, name, shape, dtype, nunits=1):
        if self.ps_big is None:
            self.ps_big = self.stack.enter_context(self.nc.psum_tensor("ps_big", [128, self.PS_WORDS], F32))
        n = 1
        for d in shape[1:]:
            n *= d
        w = (n * mybir.dt.size(dtype) + 3) // 4
        if (self.ps_off % 512) + min(w, 512) > 512:
            self.ps_off = (self.ps_off + 511) // 512 * 512
        ap, words = self._carve(self.ps_big, self.ps_off, shape, dtype)
        self.ps_off += words
        assert self.ps_off <= self.PS_WORDS, f"PSUM overflow allocating {name}"
        t = T(ap, name, nunits)
        for u in t.units:
            u.excl = True
        return t

    def barrier(self):
        last = {}
        for o in self.ops:
            if o.emit is not None:
                last[self._stream_key(o)] = o
        for e in self.CE:
            b = Op(e, None)
            b.idx = self.stream_len[e]
            b.sem = self.sem[e]
            for o in last.values():
                if (not o.is_dma) and o.eng == e:
                    continue
                self._add_wait(b, o)
            self.ops.append(b)

    def dram(self, name, shape, dtype, kind="Internal", nunits=1):
        h = self.nc.dram_tensor(name, list(shape), dtype, kind=kind).ap()
        return T(h, name, nunits)

    def _stream_key(self, op):
        return ("dma", id(op.sem)) if op.is_dma else op.eng

    def _add_wait(self, op, prod):
        if prod is None or prod is op:
            return
        if (not prod.is_dma) and prod.eng == op.eng and not self.same_engine_sync[op.eng]:
            return
        key = self._stream_key(prod)
        pidx = prod.idx
        if self.seen[op.eng].get(key, -1) >= pidx:
            return
        self.seen[op.eng][key] = pidx
        prod.needed = True
        op.waits.append(prod)

    def _track(self, op, reads, writes):
        ex = [v for v in reads if any(u.excl for u in v.units)]
        if ex:
            reads = [v for v in reads if v not in ex]
            writes = list(writes) + ex
        for v in reads:
            for u in v.units:
                self._add_wait(op, u.last_write)
        for v in writes:
            for u in v.units:
                self._add_wait(op, u.last_write)
                for r in reversed(u.reads):
                    self._add_wait(op, r)
        for v in reads:
            for u in v.units:
                u.reads.append(op)
                if len(u.reads) > 64:
                    u.reads = u.reads[-48:]
        for v in writes:
            for u in v.units:
                u.last_write = op
                u.reads = []

    WRITE_KW = ("out", "accum_out", "out_max", "out_indices")

    def op(self, e, fn, *args, **kw):
        reads, writes = [], []
        for i, a in enumerate(args):
            if isinstance(a, V):
                (writes if i == 0 else reads).append(a)
        for k, a in kw.items():
            if isinstance(a, V):
                (writes if k in self.WRITE_KW else reads).append(a)
        extra_r = kw.pop("_reads", [])
        extra_w = kw.pop("_writes", [])
        reads += extra_r
        writes += extra_w
        rargs = [a.ap if isinstance(a, V) else a for a in args]
        rkw = {k: (a.ap if isinstance(a, V) else a) for k, a in kw.items()}
        engine = self.eng[e]

        def emit():
            return getattr(engine, fn)(*rargs, **rkw)

        o = Op(e, emit)
        o.idx = self.stream_len[e]
        self.stream_len[e] += 1
        o.sem = self.sem[e]
        self._track(o, reads, writes)
        self.ops.append(o)
        return o

    def dma(self, q, out, in_, **kw):
        engine = self.eng[q]
        si = self.dma_rr
        self.dma_rr = (self.dma_rr + 1) % len(self.dma_sems)
        oap, iap = out.ap, in_.ap

        def emit():
            return engine.dma_start(out=oap, in_=iap, **kw)

        o = Op(q, emit, is_dma=True)
        o.sem = self.dma_sems[si]
        prev = self.dma_last[si]
        o.idx = (prev.idx + 1) if prev is not None else 0
        self._add_wait(o, prev)
        self.dma_last[si] = o
        self._track(o, [in_], [out])
        o.needed = True
        self.ops.append(o)
        return o

    def collective(self, kind, op, groups, in_, out):
        engine = self.eng["pool"]
        si = self.dma_rr
        self.dma_rr = (self.dma_rr + 1) % len(self.dma_sems)
        oap, iap = out.ap, in_.ap

        def emit():
            return engine.collective_compute(kind, op, replica_groups=groups, ins=[iap], outs=[oap])

        o = Op("pool", emit, is_dma=True)
        o.sem = self.dma_sems[si]
        prev = self.dma_last[si]
        o.idx = (prev.idx + 1) if prev is not None else 0
        self._add_wait(o, prev)
        self.dma_last[si] = o
        self._track(o, [in_], [out])
        o.needed = True
        self.ops.append(o)
        return o

    def finish(self):
        fin = Op("sp", None)
        fin.idx = self.stream_len["sp"]
        last = {}
        for o in self.ops:
            if o.emit is not None:
                last[self._stream_key(o)] = o
        for o in last.values():
            if o.eng == "sp" and not o.is_dma:
                continue
            self._add_wait(fin, o)
        counters = {}
        for o in self.ops:
            if o.needed:
                key = self._stream_key(o)
                counters[key] = counters.get(key, 0) + 1
                o.num = counters[key]
        nwaits = 0
        for o in self.ops + [fin]:
            engine = self.eng[o.eng]
            for p in o.waits:
                val = p.num * (16 if p.is_dma else 1)
                engine.wait_ge(p.sem, val)
                nwaits += 1
            if o.emit is None:
                continue
            ins = o.emit()
            if o.needed:
                ins.then_inc(o.sem, 16 if o.is_dma else 1)
        self.stats = dict(nops=len(self.ops), nwaits=nwaits,
                          counters={str(k): v for k, v in counters.items()})
        return self.stats


I32 = mybir.dt.int32
D = 2048
NE = 32
FF = 768
ALPHA = 1.702
LIMIT = 7.0
F7 = ALPHA * LIMIT / (1.0 + np.exp(-ALPHA * LIMIT))
RMS_EPS = 1e-6


def bc(t, idx, shape):
    return V(t.h[idx].to_broadcast(list(shape)), t.units)


def make_consts(kb):
    C = {}
    io = kb.sbuf("c_io", [128, 128], I32)
    iof = kb.sbuf("c_iof", [128, 128], F32)
    C["ident"] = kb.sbuf("c_ident", [128, 128], F32)
    C["identb"] = kb.sbuf("c_identb", [128, 128], BF16)
    kb.op("pool", "iota", io[:], [[1, 128]], base=0, channel_multiplier=-1)
    kb.op("dve", "tensor_copy", iof[:], io[:])
    kb.op("dve", "tensor_single_scalar", C["ident"][:], iof[:], 0.0, ALU.is_equal)
    kb.op("dve", "tensor_copy", C["identb"][:], C["ident"][:])
    C["jmp"] = iof
    return C


def emit_mod(kb, ctD, wD, bD, modD, ncols):
    with kb.scope():
        cs = kb.sbuf("cs", [128, 16], F32)
        kb.dma("sp", cs[:], ctD[:])
        kb.op("act", "activation", cs[:], cs[:], AF.Silu)
        ws = [kb.sbuf(f"adaw{i}", [128, 16, 512], F32) for i in range(2)]
        brow = kb.sbuf("adab", [1, ncols], F32)
        orow = kb.sbuf("modrow", [1, ncols], F32)
        kb.dma("sp", brow[:], bD[:])
        ps = [kb.psum(f"modps{i}", [1, 512], F32) for i in range(2)]
        wv = wD.h.rearrange("(kc p) n -> p kc n", p=128)
        for cb in range(ncols // 512):
            w = ws[cb % 2]
            kb.dma("sp", w[:], V(wv[:, :, cb * 512:(cb + 1) * 512], wD.units))
            p = ps[cb % 2]
            for kc in range(16):
                kb.op("pe", "matmul", p[:], cs[:, kc:kc + 1], w[:, kc, :], start=(kc == 0), stop=(kc == 15))
            kb.op("dve", "tensor_tensor", orow[:, cb * 512:(cb + 1) * 512], p[:], brow[:, cb * 512:(cb + 1) * 512], ALU.add)
        kb.dma("sp", modD[:], orow[:])


def load_bc(kb, t, dramT, row_ap):
    kb.dma("sp", t[:], V(row_ap.partition_broadcast(t.h.shape[0]), dramT.units))


def emit_rmsnorm_tile(kb, ht, A, sh, hn, ss, junk):
    kb.op("act", "activation", junk[:], ht[:], AF.Square, accum_out=ss[:, 0:1])
    kb.op("dve", "tensor_scalar", ss[:, 1:2], ss[:, 0:1], 1.0 / D, RMS_EPS, ALU.mult, ALU.add)
    kb.op("act", "activation", ss[:, 2:3], ss[:, 1:2], AF.Sqrt)
    kb.op("dve", "reciprocal", ss[:, 3:4], ss[:, 2:3])
    kb.op("dve", "scalar_tensor_tensor", junk[:], ht[:], ss[:, 3:4], A[:], ALU.mult, ALU.mult)
    kb.op("dve", "tensor_tensor", hn[:], junk[:], sh[:], ALU.add)


STOP = ""


def emit_moe(kb, C, NT, hinD, houtD, modD, moff, ngD, rwD, rbD, w1D, b1D, w2D, b2D, fin=None):
    SP = 1024
    npass = NT // SP
    ntile = SP // 128
    with kb.scope():
        hnT = kb.sbuf("hnT", [128, 16, SP], BF16)
        acc = kb.sbuf("acc", [128, ntile, D], F32, nunits=ntile)
        gcol = kb.sbuf("gcol", [128, ntile, NE], F32)
        b1t = kb.sbuf("b1t", [128, NE, 12], F32)
        b1a = kb.sbuf("b1a", [128, NE, 6], F32)
        kb.dma("sp", b1t[:], b1D[:])
        kb.op("dve", "tensor_scalar", b1a[:], b1t[:, :, 0:6], ALPHA, None, ALU.mult)
        po = kb.psum("po", [128, 2048], F32, nunits=2)
        pgu = kb.psum("pgu", [128, 4, 512], F32, nunits=4)
        for ps_ in range(npass):
            with kb.scope():
                A2 = kb.sbuf("A2", [128, D], F32)
                sh2 = kb.sbuf("sh2", [128, D], F32)
                tmpg = kb.sbuf("tmpg", [128, D], F32)
                load_bc(kb, A2, modD, modD.h[0:1, moff[1]:moff[1] + D])
                load_bc(kb, tmpg, ngD, ngD.h[0:1, :])
                load_bc(kb, sh2, modD, modD.h[0:1, moff[0]:moff[0] + D])
                kb.op("dve", "scalar_tensor_tensor", A2[:], A2[:], 1.0, tmpg[:], ALU.add, ALU.mult)
                rw = kb.sbuf("rw", [128, 16, NE], F32)
                kb.dma("sp", rw[:], V(rwD.h.rearrange("(kc p) e -> p kc e", p=128), rwD.units))
                rb = kb.sbuf("rb", [128, NE], F32)
                load_bc(kb, rb, rbD, rbD.h[0:1, :])
                b2s = kb.sbuf("b2s", [NE, D], F32)
                kb.dma("sp", b2s[:], b2D[:])
                hts = [kb.sbuf(f"ht{i}", [128, D], F32) for i in range(2)]
                hns = [kb.sbuf(f"hn{i}", [128, D], F32) for i in range(2)]
                hT32 = kb.sbuf("hT32", [128, D], F32)
                sss = [kb.sbuf(f"ss{i}", [128, 4], F32) for i in range(2)]
                lgs = kb.sbuf("lgs", [128, NE], F32)
                m8 = kb.sbuf("m8", [128, 8], F32)
                msk = kb.sbuf("msk", [128, NE], F32)
                ex = kb.sbuf("ex", [128, NE], F32)
                sm = kb.sbuf("sm", [128, 4], F32)
                comb = kb.sbuf("comb", [128, NE], F32)
                combT = kb.sbuf("combT", [NE, 128], F32)
                lg = V(pgu.h[:, 0, 0:NE], [pgu.units[0]])
                cT = V(pgu.h[0:NE, 1, 0:128], [pgu.units[1]])
                for i in range(ntile):
                    gt = ps_ * ntile + i
                    ht, hn, ss = hts[i % 2], hns[i % 2], sss[i % 2]
                    kb.dma("sp", ht[:], hinD[gt * 128:(gt + 1) * 128, :])
                    emit_rmsnorm_tile(kb, ht, A2, sh2, hn, ss, tmpg)
                    tp = po.all()
                    for kc in range(16):
                        kb.op("pe", "transpose", V(po.h[:, kc * 128:(kc + 1) * 128], po.units),
                              hn[:, kc * 128:(kc + 1) * 128], C["ident"][:])
                    for bk in range(4):
                        kb.op("act", "copy", hT32[:, bk * 512:(bk + 1) * 512], V(po.h[:, bk * 512:(bk + 1) * 512], po.units))
                        kb.op("dve", "tensor_copy", hnT[:, bk * 4:(bk + 1) * 4, i * 128:(i + 1) * 128],
                              V(po.h[:, bk * 512:(bk + 1) * 512].rearrange("p (kc t) -> p kc t", t=128), po.units))
                    for kc in range(16):
                        kb.op("pe", "matmul", lg, hT32[:, kc * 128:(kc + 1) * 128], rw[:, kc, :],
                              start=(kc == 0), stop=(kc == 15))
                    kb.op("dve", "tensor_tensor", lgs[:], lg, rb[:], ALU.add)
                    kb.op("dve", "max", m8[:], lgs[:])
                    kb.op("dve", "tensor_scalar", msk[:], lgs[:], m8[:, 3:4], None, ALU.is_ge)
                    kb.op("dve", "tensor_scalar", sm[:, 0:1], m8[:, 0:1], -1.0, None, ALU.mult)
                    kb.op("act", "activation", ex[:], lgs[:], AF.Exp, bias=sm[:, 0:1])
                    kb.op("dve", "tensor_tensor", ex[:], ex[:], msk[:], ALU.mult)
                    kb.op("dve", "reduce_sum", sm[:, 1:2], ex[:], AX.X)
                    kb.op("dve", "reciprocal", sm[:, 2:3], sm[:, 1:2])
                    kb.op("dve", "tensor_scalar", comb[:], ex[:], sm[:, 2:3], None, ALU.mult)
                    kb.op("dve", "tensor_scalar", gcol[:, i, :], comb[:], 1.0 / ALPHA, None, ALU.mult)
                    kb.op("pe", "transpose", cT, comb[:], C["ident"][:])
                    kb.op("act", "copy", combT[:], cT)
                    for db in range(4):
                        kb.op("pe", "matmul", V(po.h[:, db * 512:(db + 1) * 512], po.units),
                              combT[:], b2s[:, db * 512:(db + 1) * 512], start=True, stop=True)
                    for bk in range(4):
                        kb.op("act", "copy", acc.at(i)[:, i, bk * 512:(bk + 1) * 512], V(po.h[:, bk * 512:(bk + 1) * 512], po.units))
            with kb.scope():
              if STOP != "stage0":
                  actT = [kb.sbuf(f"actT{i}", [128, 6, SP], BF16) for i in range(2)]
                  w1s = [kb.sbuf(f"w1s{i}", [128, 16, 256], BF16) for i in range(4)]
                  w2s = [kb.sbuf(f"w2s{i}", [128, 2, D], BF16) for i in range(3)]
                  a1s = [kb.sbuf(f"a1_{i}", [128, 512], F32) for i in range(2)]
                  u1s = [kb.sbuf(f"u1_{i}", [128, 512], F32) for i in range(2)]
                  nblk = SP // 512
                  for j in range(4):
                      kb.dma("pool", w1s[j][:], w1D[0, j])
                  for j in range(3):
                      kb.dma("pool", w2s[j][:], w2D[0, j])
                  cnt = 0
                  for e in range(NE):
                      aT = actT[e % 2]
                      for mp in range(6):
                          n = e * 6 + mp
                          w = w1s[n % 4]
                          for tb in range(nblk):
                              pg = pgu.at((cnt % 2) * 2)[:, (cnt % 2) * 2, :]
                              pu = pgu.at((cnt % 2) * 2 + 1)[:, (cnt % 2) * 2 + 1, :]
                              a1, u1 = a1s[cnt % 2], u1s[cnt % 2]
                              cnt += 1
                              for kc in range(16):
                                  kb.op("pe", "matmul", pg, w[:, kc, 0:128], hnT[:, kc, tb * 512:(tb + 1) * 512],
                                        start=(kc == 0), stop=(kc == 15))
                              for kc in range(16):
                                  kb.op("pe", "matmul", pu, w[:, kc, 128:256], hnT[:, kc, tb * 512:(tb + 1) * 512],
                                        start=(kc == 0), stop=(kc == 15))
                              kb.op("act", "activation", a1[:], pg, AF.Silu, bias=b1a[:, e, mp:mp + 1], scale=ALPHA)
                              kb.op("dve", "tensor_scalar", u1[:], pu, b1t[:, e, 6 + mp:7 + mp], LIMIT, ALU.add, ALU.min)
                              kb.op("dve", "tensor_scalar", u1[:], u1[:], -LIMIT, 1.0, ALU.max, ALU.add)
                              kb.op("dve", "scalar_tensor_tensor", aT[:, mp, tb * 512:(tb + 1) * 512], a1[:], float(F7),
                                    u1[:], ALU.min, ALU.mult)
                          n2 = n + 4
                          if n2 < NE * 6:
                              kb.dma("pool", w1s[n2 % 4][:], w1D[n2 // 6, n2 % 6])
                      for tt in range(ntile):
                          for hf in range(2):
                              pv = po.at(hf)
                              for dbh in range(2):
                                  db = hf * 2 + dbh
                                  for k in range(6):
                                      kb.op("pe", "matmul", pv[:, db * 512:(db + 1) * 512],
                                            aT[:, k, tt * 128:(tt + 1) * 128], w2s[k // 2][:, k % 2, db * 512:(db + 1) * 512],
                                            start=(k == 0), stop=(k == 5))
                              for dbh in range(2):
                                  sl = slice((hf * 2 + dbh) * 512, (hf * 2 + dbh + 1) * 512)
                                  kb.op("dve", "scalar_tensor_tensor", acc.at(tt)[:, tt, sl], pv[:, sl],
                                        gcol[:, tt, e:e + 1], acc.at(tt)[:, tt, sl], ALU.mult, ALU.add)
                      if e + 1 < NE:
                          for j in range(3):
                              kb.dma("pool", w2s[j][:], w2D[e + 1, j])
            with kb.scope():
                g2 = kb.sbuf("g2", [128, D], F32)
                load_bc(kb, g2, modD, modD.h[0:1, moff[2]:moff[2] + D])
                hts = [kb.sbuf(f"hr{i}", [128, D], F32) for i in range(2)]
                if fin is not None:
                    fmodD, fgD, outD = fin
                    fA = kb.sbuf("fA", [128, D], F32)
                    fsh = kb.sbuf("fsh", [128, D], F32)
                    tg = kb.sbuf("ftg", [128, D], F32)
                    load_bc(kb, fA, fmodD, fmodD.h[0:1, D:2 * D])
                    load_bc(kb, tg, fgD, fgD.h[0:1, :])
                    load_bc(kb, fsh, fmodD, fmodD.h[0:1, 0:D])
                    kb.op("dve", "scalar_tensor_tensor", fA[:], fA[:], 1.0, tg[:], ALU.add, ALU.mult)
                    fss = [kb.sbuf(f"fss{i}", [128, 4], F32) for i in range(2)]
                    fo = [kb.sbuf(f"fo{i}", [128, D], F32) for i in range(2)]
                for i in range(ntile):
                    gt = ps_ * ntile + i
                    ht = hts[i % 2]
                    kb.dma("sp", ht[:], hinD[gt * 128:(gt + 1) * 128, :])
                    kb.op("dve", "tensor_tensor", acc.at(i)[:, i, :], acc.at(i)[:, i, :], g2[:], ALU.mult)
                    kb.op("dve", "tensor_tensor", ht[:], ht[:], acc.at(i)[:, i, :], ALU.add)
                    if fin is None:
                        kb.dma("sp", houtD[gt * 128:(gt + 1) * 128, :], ht[:])
                    else:
                        emit_rmsnorm_tile(kb, ht, fA, fsh, fo[i % 2], fss[i % 2], tg)
                        kb.dma("sp", outD[gt * 128:(gt + 1) * 128, :], fo[i % 2][:])


TS = 512


def make_sel(kb, C):
    rowsel = kb.sbuf("rowsel", [128, 128, 128], BF16)
    colsel = kb.sbuf("colsel", [128, 128, 128], BF16)
    kb.op("dve", "tensor_copy", rowsel[:], V(C["identb"].h[:, :].unsqueeze(2).to_broadcast([128, 128, 128]), C["identb"].units))
    scr = kb.dram("identscr", [1, 128 * 128], BF16)
    kb.dma("sp", V(scr.h.rearrange("o (a b) -> (o a) b", a=128), scr.units), C["identb"][:])
    kb.dma("sp", V(colsel.h.rearrange("p a b -> p (a b)"), colsel.units), V(scr.h[0:1, :].partition_broadcast(128), scr.units))
    C["rowsel"], C["colsel"] = rowsel, colsel
    ones = kb.sbuf("ones32", [128, 128], F32)
    kb.op("dve", "memset", ones[:], 1.0)
    C["ones"] = ones


def emit_norm_segment(kb, C, hfullD, seg, modD, moff, ngD, hnT, ps):
    with kb.scope():
        A1 = kb.sbuf("A1", [128, D], F32)
        sh1 = kb.sbuf("sh1", [128, D], F32)
        tg = kb.sbuf("tg1", [128, D], F32)
        load_bc(kb, A1, modD, modD.h[0:1, moff[1]:moff[1] + D])
        load_bc(kb, tg, ngD, ngD.h[0:1, :])
        load_bc(kb, sh1, modD, modD.h[0:1, moff[0]:moff[0] + D])
        kb.op("dve", "scalar_tensor_tensor", A1[:], A1[:], 1.0, tg[:], ALU.add, ALU.mult)
        ht = kb.sbuf("ht_m", [128, D], F32)
        hn = kb.sbuf("hn_m", [128, D], F32)
        ss = kb.sbuf("ss_m", [128, 4], F32)
        for i in range(TS // 128):
            r0 = seg * TS + i * 128
            kb.dma("sp", ht[:], hfullD[r0:r0 + 128, :])
            emit_rmsnorm_tile(kb, ht, A1, sh1, hn, ss, tg)
            for bk in range(4):
                p = ps[bk % 2]
                for j in range(4):
                    kc = bk * 4 + j
                    kb.op("pe", "transpose", p[:, j * 128:(j + 1) * 128], hn[:, kc * 128:(kc + 1) * 128], C["ident"][:])
                kb.op("dve" if bk % 2 else "act", "tensor_copy" if bk % 2 else "copy",
                      hnT[:, bk * 4:(bk + 1) * 4, i * 128:(i + 1) * 128],
                      V(p.h[:, :].rearrange("p (kc t) -> p kc t", t=128), p.units))


def emit_proj_chunk(kb, wslot, wD_slab, hnT, p, mcols=128):
    src = wD_slab if mcols == 128 else V(wD_slab.ap[:, :, 0:mcols], wD_slab.units)
    kb.dma("pool", wslot[:, :, 0:mcols], src)
    for kc in range(16):
        kb.op("pe", "matmul", p[0:mcols, :], wslot[:, kc, 0:mcols], hnT[:, kc, :], start=(kc == 0), stop=(kc == 15))


GLA_PQ = "alt"
GLA_D1 = "dve"


def emit_gla(kb, C, decT, kT, qT, vhi, vlo, vrow0, nv, S, sbase, po, porow0, first, last, pbs, tmps, cnt):
    for v in range(nv):
        pb = pbs[cnt[0] % len(pbs)]
        d1, st, pq = tmps[cnt[0] % len(tmps)]
        cnt[0] += 1
        sel = C["rowsel"][:, vrow0 + v, :]
        kb.op("pe", "matmul", pb[:], sel, vhi, start=True, stop=False)
        kb.op("pe", "matmul", pb[:], sel, vlo, start=False, stop=True)
        if GLA_D1 == "dve":
            kb.op("dve", "tensor_tensor", d1[:], pb[:], kT, ALU.mult)
        else:
            kb.op("act", "copy", d1[:], pb[:])
            kb.op("pool", "tensor_tensor", d1[:], d1[:], kT, ALU.mult)
        sc = S.at(sbase + v)[:, sbase + v:sbase + v + 1]
        kb.op("dve", "tensor_tensor_scan", st[:], decT, d1[:], sc, ALU.mult, ALU.add)
        kb.op("act", "copy", sc, st[:, TS - 1:TS])
        pe_ = GLA_PQ if GLA_PQ in ("pool", "dve") else ("pool" if cnt[0] % 2 else "dve")
        kb.op(pe_, "tensor_tensor", pq[:], st[:], qT, ALU.mult)
        kb.op("pe", "matmul", po, C["colsel"][:, porow0 + v, :], pq[:],
              start=(first and v == 0), stop=(last and v == nv - 1))


def emit_hgrn_M(kb, C, L, lidx, hfullD, modD, ngD, wslabD, lbrawD, hggD, oTD):
    nseg = L // TS
    make_sel(kb, C)
    with kb.scope():
        lbr = kb.sbuf("lbr", [128, 4, 4], F32)
        lbe = kb.sbuf("lbe", [128, 4, 4], F32)
        lbs = kb.sbuf("lbs", [128, 4], F32)
        lb = kb.sbuf("lb", [128, 4], F32)
        oml = kb.sbuf("oml", [128, 4], F32)
        kb.dma("sp", lbr[:], lbrawD[:])
        kb.op("dve", "reduce_max", lbs[:], lbr[:], AX.X)
        kb.op("dve", "tensor_tensor", lbe[:], lbr[:], bc(lbs, (slice(None), slice(None)), [128, 4]) if False else
              V(lbs.h[:, :].unsqueeze(2).to_broadcast([128, 4, 4]), lbs.units), ALU.subtract)
        kb.op("act", "activation", lbe[:], lbe[:], AF.Exp)
        kb.op("dve", "reduce_sum", lbs[:], lbe[:], AX.X)
        kb.op("dve", "reciprocal", lbs[:], lbs[:])
        kb.op("dve", "reduce_sum", lb[:], lbe[:, :, 1:lidx + 1], AX.X)
        kb.op("dve", "tensor_tensor", lb[:], lb[:], lbs[:], ALU.mult)
        kb.op("dve", "tensor_scalar", oml[:], lb[:], -1.0, 1.0, ALU.mult, ALU.add)
        hgg = kb.sbuf("hgg", [128, 1], F32)
        kb.dma("sp", hgg[:], hggD[:])
        S = kb.sbuf("Sst", [128, 512], F32, nunits=512)
        kb.op("dve", "memset", S.all(), 0.0)
        hnT = kb.sbuf("hnT_m", [128, 16, TS], BF16)
        pss = [kb.psum(f"psA{i}", [128, 512], F32) for i in range(2)]
        pbs = [kb.psum(f"psB{i}", [128, 512], F32) for i in range(4)]
        po = kb.psum("psO", [128, 512], F32)
        pn = kb.psum("psN", [128, 512], F32)
        cnt = [0]
        pc = 0
        for seg in range(nseg):
            emit_norm_segment(kb, C, hfullD, seg, modD, (0, D, 2 * D), ngD, hnT, pss)
            with kb.scope():
                wsl = [kb.sbuf(f"wsl{i}", [128, 16, 128], BF16) for i in range(4)]
                decT = kb.sbuf("decT", [128, 4, TS], F32)
                kkT = kb.sbuf("kkT", [128, 4, TS], F32)
                qT = kb.sbuf("qT", [128, 4, TS], F32)
                ogs = kb.sbuf("ogs", [128, 4, TS], F32)
                ihi = kb.sbuf("ihi", [128, 4, TS], BF16, nunits=4)
                ilo = kb.sbuf("ilo", [128, 4, TS], BF16, nunits=4)
                tmps = [(kb.sbuf(f"d1_{i}", [128, TS], F32), kb.sbuf(f"st_{i}", [128, TS], F32),
                         kb.sbuf(f"pq_{i}", [128, TS], BF16)) for i in range(4)]
                sq = kb.sbuf("sq", [128, TS], F32)
                osb = kb.sbuf("osb", [128, TS], F32)
                rs = kb.sbuf("rs", [128, TS], F32)
                res = kb.sbuf("res", [128, TS], F32)
                for hh in range(4):
                    for ty in range(4):
                        p = pss[pc % 2]
                        w = wsl[pc % 4]
                        pc += 1
                        emit_proj_chunk(kb, w, wslabD[hh * 4 + ty], hnT, p)
                        if ty == 0:
                            kb.op("act", "activation", qT[:, hh, :], p[:], AF.Silu)
                        elif ty == 1:
                            kb.op("act", "activation", decT[:, hh, :], p[:], AF.Sigmoid)
                            kb.op("dve", "tensor_scalar", decT[:, hh, :], decT[:, hh, :], oml[:, hh:hh + 1], lb[:, hh:hh + 1],
                                  ALU.mult, ALU.add)
                            kb.op("dve", "tensor_scalar", kkT[:, hh, :], decT[:, hh, :], -1.0, 1.0, ALU.mult, ALU.add)
                        elif ty == 2:
                            kb.op("act", "copy", ihi.at(hh)[:, hh, :], p[:])
                            kb.op("dve", "tensor_tensor", ilo.at(hh)[:, hh, :], p[:], ihi.at(hh)[:, hh, :], ALU.subtract)
                        else:
                            kb.op("act", "activation", ogs[:, hh, :], p[:], AF.Silu)
                for hh in range(4):
                    emit_gla(kb, C, decT[:, hh, :], kkT[:, hh, :], qT[:, hh, :], ihi.at(hh)[:, hh, :], ilo.at(hh)[:, hh, :],
                             0, 128, S, hh * 128, po[:], 0, True, True, pbs, tmps, cnt)
                    kb.op("act", "activation", sq[:], po[:], AF.Square)
                    kb.op("dve", "tensor_copy", osb[:], po[:])
                    kb.op("pe", "matmul", pn[:], C["ones"][:], sq[:], start=True, stop=True)
                    kb.op("dve", "tensor_scalar", rs[:], pn[:], 1.0 / 128, RMS_EPS, ALU.mult, ALU.add)
                    kb.op("act", "activation", rs[:], rs[:], AF.Sqrt)
                    kb.op("dve", "reciprocal", rs[:], rs[:])
                    kb.op("dve", "tensor_tensor", res[:], osb[:], rs[:], ALU.mult)
                    kb.op("dve", "scalar_tensor_tensor", res[:], res[:], hgg[:, 0:1], ogs[:, hh, :], ALU.mult, ALU.mult)
                    kb.dma("sp", oTD[hh * 128:(hh + 1) * 128, seg * TS:(seg + 1) * TS], res[:])


def emit_outproj(kb, NT, hinD, hmidD, modD, g1off, srcs, woutD, nkc):
    with kb.scope():
        woutb = kb.sbuf("woutb", [128, nkc, D], BF16)
        wv = woutD.h.rearrange("(kc p) n -> p kc n", p=128)
        for k0 in range(0, nkc, 8):
            kb.dma("pool", woutb[:, k0:k0 + 8, :], V(wv[:, k0:k0 + 8, :], woutD.units))
        g1 = kb.sbuf("g1t", [128, D], F32)
        load_bc(kb, g1, modD, modD.h[0:1, g1off:g1off + D])
        mixs = [kb.sbuf(f"mixT{i}", [128, nkc, 512], BF16) for i in range(2 if nkc <= 16 else 1)]
        hts = [kb.sbuf(f"hto{i}", [128, D], F32) for i in range(2)]
        tmp = kb.sbuf("tmpo", [128, 512], F32)
        pss = [kb.psum(f"pso{i}", [128, 4, 512], F32, nunits=4) for i in range(2)]
        it = 0
        for tb in range(NT // 512):
            mx = mixs[tb % len(mixs)]
            k0 = 0
            for (sD, nch) in srcs:
                sv = sD.h.rearrange("(kc p) t -> p kc t", p=128)
                kb.dma("pool" if sD.h.dtype == F32 else "sp", mx[:, k0:k0 + nch, :], V(sv[:, :, tb * 512:(tb + 1) * 512], sD.units))
                k0 += nch
            for tt in range(4):
                gt = tb * 4 + tt
                ps = pss[it % 2]
                ht = hts[it % 2]
                it += 1
                kb.dma("sp", ht[:], hinD[gt * 128:(gt + 1) * 128, :])
                for db in range(4):
                    for kc in range(nkc):
                        kb.op("pe", "matmul", ps.at(db)[:, db, :], mx[:, kc, tt * 128:(tt + 1) * 128],
                              woutb[:, kc, db * 512:(db + 1) * 512], start=(kc == 0), stop=(kc == nkc - 1))
                for db in range(4):
                    sl = slice(db * 512, (db + 1) * 512)
                    kb.op("dve", "tensor_tensor", tmp[:], ps.at(db)[:, db, :], g1[:, sl], ALU.mult)
                    kb.op("dve", "tensor_tensor", ht[:, sl], ht[:, sl], tmp[:], ALU.add)
                kb.dma("sp", hmidD[gt * 128:(gt + 1) * 128, :], ht[:])


TWO_PI = 6.28318


def emit_ab_M(kb, C, L, hfullD, modD, ngD, wslabD, P, yaTD, ybTD):
    nseg = L // TS
    make_sel(kb, C)
    tabD = kb.dram("s5tab", [16, 128, 2, TS], F32)
    with kb.scope():
        ident = C["ident"]
        hnT = kb.sbuf("hnT_m", [128, 16, TS], BF16)
        S = kb.sbuf("Sst", [128, 512], F32, nunits=512)
        kb.op("dve", "memset", S.all(), 0.0)
        xpre = kb.sbuf("xpre", [128, 6, TS + 3], F32)
        kb.op("dve", "memset", xpre[:], 0.0)
        sprev = kb.sbuf("sprev", [128, 16, 2], F32)
        kb.op("dve", "memset", sprev[:], 0.0)
        convw = kb.sbuf("convw", [128, 6, 4], F32)
        convb = kb.sbuf("convb", [128, 6], F32)
        dtb = kb.sbuf("dtb", [8, 1], F32)
        negA = kb.sbuf("negA", [8, 1], F32)
        ssdD = kb.sbuf("ssdD", [128, 4], F32)
        normg = kb.sbuf("normg", [128, 4], F32)
        s5d = kb.sbuf("s5d", [32, 16], F32)
        for t_, n_ in ((convw, "convw"), (convb, "convb"), (dtb, "dtb"), (negA, "alog"), (ssdD, "ssdD"), (normg, "normg"), (s5d, "s5d")):
            kb.dma("sp", t_[:], P[n_][:])
        kb.op("act", "activation", negA[:], negA[:], AF.Exp)
        kb.op("dve", "tensor_scalar", negA[:], negA[:], -1.0, None, ALU.mult)
        sel8 = kb.sbuf("sel8", [8, 8, 128], F32)
        sel8b = kb.sbuf("sel8b", [8, 4, 128], F32)
        kb.op("dve", "tensor_copy", sel8[:], V(ident.h[0:8, 0:8].unsqueeze(2).to_broadcast([8, 8, 128]), ident.units))
        for c in range(4):
            kb.op("dve", "tensor_copy", sel8b[:, c, 0:64], sel8[:, 2 * c, 0:64])
            kb.op("dve", "tensor_copy", sel8b[:, c, 64:128], sel8[:, 2 * c + 1, 64:128])
        wz = kb.sbuf("wz", [128, 16, 512], BF16)
        wdt = kb.sbuf("wdt", [128, 16, 8], BF16)
        kb.dma("pool", wz[:], P["wz"][:])
        kb.dma("pool", wdt[:], P["wdt"][:])
        dtbB = kb.sbuf("dtbB", [128, 8], F32)
        negAB = kb.sbuf("negAB", [128, 8], F32)
        DB = kb.sbuf("DB", [128, 512], F32)
        NB = kb.sbuf("NB", [128, 512], F32)
        load_bc(kb, dtbB, P["dtbr"], P["dtbr"].h[0:1, :])
        load_bc(kb, negAB, P["alogr"], P["alogr"].h[0:1, :])
        load_bc(kb, DB, P["ssdDr"], P["ssdDr"].h[0:1, :])
        load_bc(kb, NB, P["normgr"], P["normgr"].h[0:1, :])
        kb.op("act", "activation", negAB[:], negAB[:], AF.Exp)
        kb.op("dve", "tensor_scalar", negAB[:], negAB[:], -1.0, None, ALU.mult)
        mle = kb.sbuf("mle", [128, 128], F32)
        ugt = kb.sbuf("ugt", [128, 128], F32)
        nmask = kb.sbuf("nmask", [128, 128], F32)
        kb.op("dve", "tensor_single_scalar", mle[:], C["jmp"][:], 0.0, ALU.is_ge)
        kb.op("dve", "tensor_single_scalar", ugt[:], C["jmp"][:], 0.0, ALU.is_lt)
        kb.op("dve", "tensor_scalar", nmask[:], ugt[:], -1.0e4, None, ALU.mult)
        Sf = kb.sbuf("Sf", [128, 512], F32)
        Sb = kb.sbuf("Sb", [128, 512], BF16)
        kb.op("dve", "memset", Sf[:], 0.0)
        kb.op("dve", "memset", Sb[:], 0.0)
        BbTr = kb.sbuf("BbTr", [32, 16, 128], BF16)
        BbTi = kb.sbuf("BbTi", [32, 16, 128], BF16)
        Creb = kb.sbuf("Creb", [128, 16, 32], BF16)
        nCimb = kb.sbuf("nCimb", [128, 16, 32], BF16)
        rho = kb.sbuf("rho", [128, 16], F32)
        cth = kb.sbuf("cth", [128, 16], F32)
        sth = kb.sbuf("sth", [128, 16], F32)
        pss = [kb.psum(f"psA{i}", [128, 512], F32) for i in range(2)]
        pbs = [kb.psum(f"psB{i}", [128, 512], F32) for i in range(2)]
        po = kb.psum("psO", [128, 512], F32)
        pn = kb.psum("psN", [128, 512], F32)
        with kb.scope():
            lre = kb.sbuf("lre", [128, 16], F32)
            lim = kb.sbuf("lim", [128, 16], F32)
            stp = kb.sbuf("stp", [128, 16], F32)
            thr = kb.sbuf("thr", [128, 16], F32)
            kb.dma("sp", lre[:], P["lre"][:])
            kb.dma("sp", lim[:], P["lim"][:])
            kb.dma("sp", stp[:], P["lstep"][:])
            kb.op("act", "activation", stp[:], stp[:], AF.Exp)
            kb.op("dve", "tensor_tensor", rho[:], lre[:], stp[:], ALU.mult)
            kb.op("act", "activation", rho[:], rho[:], AF.Exp)
            kb.op("dve", "tensor_tensor", thr[:], lim[:], stp[:], ALU.mult)
            kb.op("dve", "tensor_scalar", thr[:], thr[:], float(1.0 / (2 * np.pi)), None, ALU.mult)
            ioi = kb.sbuf("ioi", [128, TS], I32)
            iot = kb.sbuf("iot", [128, TS], F32)
            kb.op("pool", "iota", ioi[:], [[1, TS]], base=0, channel_multiplier=0)
            kb.op("dve", "tensor_copy", iot[:], ioi[:])
            ur = kb.sbuf("ur", [128, TS], F32)
            ki = kb.sbuf("ki", [128, TS], I32)
            kf = kb.sbuf("kf", [128, TS], F32)
            fr = kb.sbuf("fr", [128, TS], F32)
            mk = kb.sbuf("mk", [128, TS], F32)
            tabs = [kb.sbuf(f"tab{i}", [128, 2, TS], F32) for i in range(2)]
            for s in range(16):
                tb_ = tabs[s % 2]
                kb.op("dve", "tensor_scalar", ur[:], iot[:], thr[:, s:s + 1], None, ALU.mult)
                kb.op("dve", "tensor_copy", ki[:], ur[:])
                kb.op("dve", "tensor_copy", kf[:], ki[:])
                kb.op("dve", "tensor_tensor", fr[:], ur[:], kf[:], ALU.subtract)
                kb.op("act", "activation", tb_[:, 1, :], fr[:], AF.Sin, scale=TWO_PI)
                kb.op("dve", "tensor_scalar", fr[:], fr[:], 0.25, None, ALU.add)
                kb.op("dve", "tensor_single_scalar", mk[:], fr[:], 0.5, ALU.is_gt)
                kb.op("dve", "tensor_tensor", fr[:], fr[:], mk[:], ALU.subtract)
                kb.op("act", "activation", tb_[:, 0, :], fr[:], AF.Sin, scale=TWO_PI)
                kb.op("dve", "tensor_copy", cth[:, s:s + 1], tb_[:, 0, 1:2])
                kb.op("dve", "tensor_copy", sth[:, s:s + 1], tb_[:, 1, 1:2])
                kb.dma("sp", tabD[s], tb_[:])
            lbr = kb.sbuf("lbr_", [128, 16], F32)
            lbi = kb.sbuf("lbi_", [128, 16], F32)
            den = kb.sbuf("den", [128, 16], F32)
            t1 = kb.sbuf("t1_", [128, 16], F32)
            gre = kb.sbuf("gre", [128, 16], F32)
            gim = kb.sbuf("gim", [128, 16], F32)
            kb.op("dve", "tensor_tensor", lbr[:], rho[:], cth[:], ALU.mult)
            kb.op("dve", "tensor_scalar", lbr[:], lbr[:], -1.0, None, ALU.add)
            kb.op("dve", "tensor_tensor", lbi[:], rho[:], sth[:], ALU.mult)
            kb.op("dve", "tensor_tensor", den[:], lre[:], lre[:], ALU.mult)
            kb.op("dve", "tensor_tensor", t1[:], lim[:], lim[:], ALU.mult)
            kb.op("dve", "tensor_tensor", den[:], den[:], t1[:], ALU.add)
            kb.op("dve", "reciprocal", den[:], den[:])
            kb.op("dve", "tensor_tensor", gre[:], lbr[:], lre[:], ALU.mult)
            kb.op("dve", "tensor_tensor", t1[:], lbi[:], lim[:], ALU.mult)
            kb.op("dve", "tensor_tensor", gre[:], gre[:], t1[:], ALU.add)
            kb.op("dve", "tensor_tensor", gre[:], gre[:], den[:], ALU.mult)
            kb.op("dve", "tensor_tensor", gim[:], lbi[:], lre[:], ALU.mult)
            kb.op("dve", "tensor_tensor", t1[:], lbr[:], lim[:], ALU.mult)
            kb.op("dve", "tensor_tensor", gim[:], gim[:], t1[:], ALU.subtract)
            kb.op("dve", "tensor_tensor", gim[:], gim[:], den[:], ALU.mult)
            bre = kb.sbuf("bre", [128, 16, 32], F32)
            bim = kb.sbuf("bim", [128, 16, 32], F32)
            bbr = kb.sbuf("bbr", [128, 16, 32], F32)
            bbi = kb.sbuf("bbi", [128, 16, 32], F32)
            tt_ = kb.sbuf("tt_", [128, 16, 32], F32)
            kb.dma("sp", bre[:], P["bre"][:])
            kb.dma("sp", bim[:], P["bim"][:])
            greb = V(gre.h[:, :].unsqueeze(2).to_broadcast([128, 16, 32]), gre.units)
            gimb = V(gim.h[:, :].unsqueeze(2).to_broadcast([128, 16, 32]), gim.units)
            kb.op("dve", "tensor_tensor", bbr[:], bre[:], greb, ALU.mult)
            kb.op("dve", "tensor_tensor", tt_[:], bim[:], gimb, ALU.mult)
            kb.op("dve", "tensor_tensor", bbr[:], bbr[:], tt_[:], ALU.subtract)
            kb.op("dve", "tensor_tensor", bbi[:], bim[:], greb, ALU.mult)
            kb.op("dve", "tensor_tensor", tt_[:], bre[:], gimb, ALU.mult)
            kb.op("dve", "tensor_tensor", bbi[:], bbi[:], tt_[:], ALU.add)
            for s in range(16):
                for (src, dst) in ((bbr, BbTr), (bbi, BbTi)):
                    p = pss[s % 2]
                    kb.op("pe", "transpose", p[0:32, 0:128], src[:, s, :], ident[:])
                    kb.op("act", "copy", dst[:, s, :], p[0:32, 0:128])
            kb.dma("sp", bre[:], P["cre"][:])
            kb.dma("sp", bim[:], P["cim"][:])
            kb.op("dve", "tensor_copy", Creb[:], bre[:])
            kb.op("dve", "tensor_scalar", nCimb[:], bim[:], -1.0, None, ALU.mult)
        cnt = [0]
        pc = 0
        for seg in range(nseg):
            cs = slice(seg * TS, (seg + 1) * TS)
            emit_norm_segment(kb, C, hfullD, seg, modD, (0, D, 2 * D), ngD, hnT, pss)
            with kb.scope():
                wsl = [kb.sbuf(f"wsl{i}", [128, 16, 128], BF16) for i in range(2)]
                xc = kb.sbuf("xc", [128, 6, TS], F32)
                xcb = kb.sbuf("xcb", [128, 2, TS], BF16)
                ycv = kb.sbuf("ycv", [128, TS], F32)
                zs = kb.sbuf("zs", [128, 512], F32)
                dtt = kb.sbuf("dtt", [128, 8], F32)
                aa_ = kb.sbuf("a_", [128, 8], F32)
                acum = kb.sbuf("acum", [128, 8], F32)
                eacum = kb.sbuf("eacum", [128, 8], F32)
                wend = kb.sbuf("wend", [128, 8], F32)
                eatot = kb.sbuf("eatot", [128, 8], F32)
                xtm = kb.sbuf("xtm", [128, 8, 64], F32)
                xdtb = kb.sbuf("xdtb", [128, 8, 64], BF16)
                xwb = kb.sbuf("xwb", [128, 8, 64], BF16)
                btm = kb.sbuf("btm", [128, 128], BF16)
                Gs = kb.sbuf("Gs", [128, 128], F32)
                am = kb.sbuf("am", [128, 8, 128], F32)
                Ee = kb.sbuf("Ee", [128, 8, 128], F32)
                Wb = kb.sbuf("Wb", [128, 8, 128], BF16)
                yt = kb.sbuf("yt", [128, 8, 64], F32)
                vv = kb.sbuf("vv", [128, 512], F32)
                ssn = kb.sbuf("ssn", [128, 4], F32)
                ob = [kb.sbuf(f"ob{i}", [128, 512], F32) for i in range(2)]
                for c in range(6):
                    p = pss[pc % 2]; w = wsl[pc % 2]; pc += 1
                    emit_proj_chunk(kb, w, wslabD[4 + c], hnT, p)
                    kb.op("act", "copy", xpre[:, c, 3:TS + 3], p[:])
                    kb.op("dve", "tensor_scalar", ycv[:], xpre[:, c, 0:TS], convw[:, c, 0:1], None, ALU.mult)
                    for k in range(1, 4):
                        kb.op("dve", "scalar_tensor_tensor", ycv[:], xpre[:, c, k:k + TS], convw[:, c, k:k + 1], ycv[:], ALU.mult, ALU.add)
                    kb.op("act", "activation", xc[:, c, :], ycv[:], AF.Silu, bias=convb[:, c:c + 1])
                    kb.op("dve", "tensor_copy", xpre[:, c, 0:3], xpre[:, c, TS:TS + 3])
                kb.op("act", "copy", xcb[:], xc[:, 4:6, :])
                psm, pxy, pbg, pS_ = pn, po, pbs[0], pbs[1]
                pdf = kb.psum("pdf", [128, 8, 128], F32)
                for ck in range(4):
                    cc_ = slice(ck * 128, (ck + 1) * 128)
                    r0 = seg * TS + ck * 128
                    p = pss[pc % 2]; pc += 1
                    for kc in range(16):
                        kb.op("pe", "matmul", p[:], hnT[:, kc, cc_], wz[:, kc, :], start=(kc == 0), stop=(kc == 15))
                    kb.op("act", "activation", zs[:], p[:], AF.Silu)
                    for kc in range(16):
                        kb.op("pe", "matmul", psm[:, 0:8], hnT[:, kc, cc_], wdt[:, kc, :], start=(kc == 0), stop=(kc == 15))
                    kb.op("dve", "tensor_tensor", dtt[:], psm[:, 0:8], dtbB[:], ALU.add)
                    kb.op("act", "activation", dtt[:], dtt[:], AF.Exp)
                    kb.op("act", "activation", dtt[:], dtt[:], AF.Ln, bias=1.0)
                    kb.op("dve", "tensor_tensor", aa_[:], dtt[:], negAB[:], ALU.mult)
                    kb.op("pe", "matmul", psm[:, 8:16], mle[:], aa_[:], start=True, stop=True)
                    kb.op("pe", "matmul", psm[:, 16:24], C["ones"][:], aa_[:], start=True, stop=True)
                    kb.op("dve", "tensor_copy", acum[:], psm[:, 8:16])
                    kb.op("act", "activation", eacum[:], acum[:], AF.Exp)
                    kb.op("dve", "tensor_tensor", wend[:], psm[:, 16:24], acum[:], ALU.subtract)
                    kb.op("act", "activation", wend[:], wend[:], AF.Exp)
                    kb.op("act", "activation", eatot[:], psm[:, 16:24], AF.Exp)
                    kb.op("dve", "tensor_tensor", wend[:], wend[:], dtt[:], ALU.mult)
                    for c in range(4):
                        kb.op("pe", "transpose", pxy[:, c * 128:(c + 1) * 128], xc[:, c, cc_], ident[:])
                    kb.op("act", "copy", V(xtm.h.rearrange("p a b -> p (a b)"), xtm.units), pxy[:])
                    kb.op("dve", "tensor_tensor", xdtb[:], xtm[:], V(dtt.h[:, :].unsqueeze(2).to_broadcast([128, 8, 64]), dtt.units), ALU.mult)
                    kb.op("dve", "tensor_tensor", xwb[:], xtm[:], V(wend.h[:, :].unsqueeze(2).to_broadcast([128, 8, 64]), wend.units), ALU.mult)
                    kb.op("pe", "transpose", pbg[:, 0:128], xc[:, 4, cc_], ident[:])
                    kb.op("act", "copy", btm[:], pbg[:, 0:128])
                    kb.op("pe", "matmul", pbg[:, 128:256], xcb[:, 0, cc_], xcb[:, 1, cc_], start=True, stop=True)
                    kb.op("act", "copy", Gs[:], pbg[:, 128:256])
                    kb.op("dve", "tensor_tensor", am[:], V(aa_.h[:, :].unsqueeze(2).to_broadcast([128, 8, 128]), aa_.units),
                          V(mle.h[:, :].unsqueeze(1).to_broadcast([128, 8, 128]), mle.units), ALU.mult)
                    for hb in range(2):
                        kb.op("pe", "matmul", V(pdf.h[:, hb * 4:(hb + 1) * 4, :], pdf.units), ugt[:], am[:, hb * 4:(hb + 1) * 4, :], start=True, stop=True)
                    for hb in range(2):
                        kb.op("dve", "tensor_tensor", Ee[:, hb * 4:(hb + 1) * 4, :], V(pdf.h[:, hb * 4:(hb + 1) * 4, :], pdf.units),
                              V(nmask.h[:, :].unsqueeze(1).to_broadcast([128, 4, 128]), nmask.units), ALU.add)
                    kb.op("act", "activation", Ee[:], Ee[:], AF.Exp)
                    kb.op("dve", "tensor_tensor", Wb[:], Ee[:], V(Gs.h[:, :].unsqueeze(1).to_broadcast([128, 8, 128]), Gs.units), ALU.mult)
                    for hh in range(8):
                        kb.op("pe", "matmul", pxy[:, hh * 64:(hh + 1) * 64], Wb[:, hh, :], xdtb[:, hh, :], start=True, stop=True)
                    kb.op("pe", "matmul", pbg[:], xcb[:, 1, cc_], Sb[:], start=True, stop=True)
                    kb.op("pe", "matmul", pS_[:], btm[:], V(xwb.h.rearrange("p a b -> p (a b)"), xwb.units), start=True, stop=True)
                    kb.op("dve", "tensor_tensor", yt[:], V(pbg.h[:, :].rearrange("p (a b) -> p a b", b=64), pbg.units),
                          V(eacum.h[:, :].unsqueeze(2).to_broadcast([128, 8, 64]), eacum.units), ALU.mult)
                    ytf = V(yt.h.rearrange("p a b -> p (a b)"), yt.units)
                    kb.op("dve", "tensor_tensor", ytf, ytf, pxy[:], ALU.add)
                    xtf = V(xtm.h.rearrange("p a b -> p (a b)"), xtm.units)
                    kb.op("dve", "tensor_tensor", vv[:], xtf, DB[:], ALU.mult)
                    kb.op("dve", "tensor_tensor", vv[:], vv[:], ytf, ALU.add)
                    S3 = V(Sf.h[:, :].rearrange("p (a b) -> p a b", b=64), Sf.units)
                    kb.op("dve", "tensor_tensor", S3, S3, V(eatot.h[:, :].unsqueeze(2).to_broadcast([128, 8, 64]), eatot.units), ALU.mult)
                    kb.op("dve", "tensor_tensor", Sf[:], Sf[:], pS_[:], ALU.add)
                    kb.op("act", "copy", Sb[:], Sf[:])
                    kb.op("dve", "tensor_tensor", vv[:], vv[:], zs[:], ALU.mult)
                    o_ = ob[ck % 2]
                    kb.op("act", "activation", o_[:], vv[:], AF.Square, accum_out=ssn[:, 0:1])
                    kb.op("dve", "tensor_scalar", ssn[:, 1:2], ssn[:, 0:1], 1.0 / 512, 1e-5, ALU.mult, ALU.add)
                    kb.op("act", "activation", ssn[:, 2:3], ssn[:, 1:2], AF.Sqrt)
                    kb.op("dve", "reciprocal", ssn[:, 3:4], ssn[:, 2:3])
                    kb.op("dve", "scalar_tensor_tensor", o_[:], vv[:], ssn[:, 3:4], NB[:], ALU.mult, ALU.mult)
                    kb.dma("sp", yaTD[r0:r0 + 128, :], o_[:])
            with kb.scope():
                wsl = [kb.sbuf(f"wsl{i}", [128, 16, 128], BF16) for i in range(4)]
                tabs = [kb.sbuf(f"tabl{i}", [128, 2, TS], F32) for i in range(2)]
                uf = [kb.sbuf(f"uf{i}", [32, TS], F32) for i in range(2)]
                ub = [kb.sbuf(f"ub{i}", [32, TS], BF16) for i in range(2)]
                bur = kb.sbuf("bur", [128, TS], F32)
                bui = kb.sbuf("bui", [128, TS], F32)
                m1 = kb.sbuf("m1", [128, TS], F32)
                m2 = kb.sbuf("m2", [128, TS], F32)
                aa = kb.sbuf("aa", [128, TS], F32)
                bb = kb.sbuf("bb", [128, TS], F32)
                rhoB = kb.sbuf("rhoB", [128, TS], F32)
                onesT = kb.sbuf("onesT", [128, TS], F32)
                kb.op("pool", "memset", onesT[:], 1.0)
                wre = kb.sbuf("wre", [128, TS], F32)
                wim = kb.sbuf("wim", [128, TS], F32)
                sre = kb.sbuf("sre", [128, TS], F32)
                sim = kb.sbuf("sim", [128, TS], F32)
                sreb = kb.sbuf("sreb", [128, TS], BF16)
                simb = kb.sbuf("simb", [128, TS], BF16)
                ini = kb.sbuf("ini", [128, 4], F32)
                yb = [kb.sbuf(f"yb{i}", [32, TS], F32) for i in range(2)]
                for s in range(16):
                    p = pss[pc % 2]; w = wsl[pc % 4]; pc += 1
                    tb_ = tabs[s % 2]
                    ct_, st_ = tb_[:, 0, :], tb_[:, 1, :]
                    kb.dma("sp", tb_[:], tabD[s])
                    emit_proj_chunk(kb, w, wslabD[11 + s], hnT, p, mcols=32)
                    kb.op("act", "copy", uf[s % 2][:], p[0:32, :])
                    kb.op("dve", "tensor_copy", ub[s % 2][:], p[0:32, :])
                    kb.op("pe", "matmul", pbs[0][:], BbTr[:, s, :], ub[s % 2][:], start=True, stop=True)
                    kb.op("pe", "matmul", pbs[1][:], BbTi[:, s, :], ub[s % 2][:], start=True, stop=True)
                    kb.op("act", "copy", bur[:], pbs[0][:])
                    kb.op("act", "copy", bui[:], pbs[1][:])
                    kb.op("pool", "tensor_tensor", m1[:], bur[:], ct_, ALU.mult)
                    kb.op("pool", "tensor_tensor", m2[:], bui[:], st_, ALU.mult)
                    kb.op("pool", "tensor_tensor", aa[:], m1[:], m2[:], ALU.add)
                    kb.op("pool", "tensor_tensor", m1[:], bui[:], ct_, ALU.mult)
                    kb.op("pool", "tensor_tensor", m2[:], bur[:], st_, ALU.mult)
                    kb.op("pool", "tensor_tensor", bb[:], m1[:], m2[:], ALU.subtract)
                    kb.op("pool", "tensor_scalar", rhoB[:], onesT[:], rho[:, s:s + 1], None, ALU.mult)
                    kb.op("dve", "tensor_tensor", ini[:, 2:3], sprev[:, s, 1:2], sth[:, s:s + 1], ALU.mult)
                    kb.op("dve", "scalar_tensor_tensor", ini[:, 0:1], sprev[:, s, 0:1], cth[:, s:s + 1], ini[:, 2:3], ALU.mult, ALU.subtract)
                    kb.op("dve", "tensor_tensor", ini[:, 3:4], sprev[:, s, 1:2], cth[:, s:s + 1], ALU.mult)
                    kb.op("dve", "scalar_tensor_tensor", ini[:, 1:2], sprev[:, s, 0:1], sth[:, s:s + 1], ini[:, 3:4], ALU.mult, ALU.add)
                    kb.op("dve", "tensor_tensor_scan", wre[:], rhoB[:], aa[:], ini[:, 0:1], ALU.mult, ALU.add)
                    kb.op("dve", "tensor_tensor_scan", wim[:], rhoB[:], bb[:], ini[:, 1:2], ALU.mult, ALU.add)
                    kb.op("pool", "tensor_tensor", m1[:], wre[:], ct_, ALU.mult)
                    kb.op("pool", "tensor_tensor", m2[:], wim[:], st_, ALU.mult)
                    kb.op("pool", "tensor_tensor", sre[:], m1[:], m2[:], ALU.subtract)
                    kb.op("pool", "tensor_tensor", m1[:], wre[:], st_, ALU.mult)
                    kb.op("pool", "tensor_tensor", m2[:], wim[:], ct_, ALU.mult)
                    kb.op("pool", "tensor_tensor", sim[:], m1[:], m2[:], ALU.add)
                    kb.op("act", "copy", sprev[:, s, 0:1], sre[:, TS - 1:TS])
                    kb.op("act", "copy", sprev[:, s, 1:2], sim[:, TS - 1:TS])
                    kb.op("act", "copy", sreb[:], sre[:])
                    kb.op("act", "copy", simb[:], sim[:])
                    kb.op("pe", "matmul", po[0:32, :], Creb[:, s, :], sreb[:], start=True, stop=False)
                    kb.op("pe", "matmul", po[0:32, :], nCimb[:, s, :], simb[:], start=False, stop=True)
                    kb.op("dve", "scalar_tensor_tensor", yb[s % 2][:], uf[s % 2][:], s5d[:, s:s + 1], po[0:32, :], ALU.mult, ALU.add)
                    kb.dma("sp", ybTD[s * 32:(s + 1) * 32, cs], yb[s % 2][:])


def emit_glu(kb, NT, ybTD, gluwD, glubD, ybfD):
    with kb.scope():
        gw = kb.sbuf("gluw", [128, 16, D], BF16)
        wv = gluwD.h.rearrange("(kc p) n -> p kc n", p=128)
        for k0 in range(0, 16, 8):
            kb.dma("pool", gw[:, k0:k0 + 8, :], V(wv[:, k0:k0 + 8, :], gluwD.units))
        gb = kb.sbuf("glub", [128, 16], F32)
        kb.dma("sp", gb[:], glubD[:])
        yb = kb.sbuf("ybl", [128, 16, 512], F32)
        glT = kb.sbuf("glT", [128, 16, 512], BF16)
        obf = kb.sbuf("obf", [128, 16, 512], BF16)
        t1 = [kb.sbuf(f"gt1_{i}", [128, 512], F32) for i in range(2)]
        t2 = [kb.sbuf(f"gt2_{i}", [128, 512], F32) for i in range(2)]
        pss = [kb.psum(f"psg{i}", [128, 512], F32) for i in range(2)]
        yv = ybTD.h.rearrange("(kc p) t -> p kc t", p=128)
        ov = ybfD.h.rearrange("(kc p) t -> p kc t", p=128)
        for tb in range(NT // 512):
            cs = slice(tb * 512, (tb + 1) * 512)
            kb.dma("sp", yb[:], V(yv[:, :, cs], ybTD.units))
            for kc in range(16):
                a, b = t1[kc % 2], t2[kc % 2]
                kb.op("act", "activation", a[:], yb[:, kc, :], AF.Square)
                kb.op("dve", "tensor_scalar", a[:], a[:], 0.044715, 1.0, ALU.mult, ALU.add)
                kb.op("pool", "tensor_tensor", a[:], a[:], yb[:, kc, :], ALU.mult)
                kb.op("act", "activation", b[:], a[:], AF.Sigmoid, scale=1.5957691216057308)
                kb.op("pool", "tensor_tensor", glT[:, kc, :], b[:], yb[:, kc, :], ALU.mult)
            for m in range(16):
                p = pss[m % 2]
                a = t1[m % 2]
                for kc in range(16):
                    kb.op("pe", "matmul", p[:], gw[:, kc, m * 128:(m + 1) * 128], glT[:, kc, :], start=(kc == 0), stop=(kc == 15))
                kb.op("act", "activation", a[:], p[:], AF.Sigmoid, bias=gb[:, m:m + 1])
                kb.op("dve", "tensor_tensor", obf[:, m, :], a[:], yb[:, m, :], ALU.mult)
            kb.dma("sp", V(ov[:, :, cs], ybfD.units), obf[:])


def emit_hgrn_M2(kb, C, L, lidx, hfullD, modD, ngD, wslabD, wiD, wogD, lbrawD, hggrD, oD):
    nseg = L // TS
    ident, identb = C["ident"], C["identb"]
    with kb.scope():
        lbr = kb.sbuf("lbr", [128, 4, 4], F32)
        lbe = kb.sbuf("lbe", [128, 4, 4], F32)
        lbs = kb.sbuf("lbs", [128, 4], F32)
        lb = kb.sbuf("lb", [128, 4], F32)
        oml = kb.sbuf("oml", [128, 4], F32)
        kb.dma("sp", lbr[:], lbrawD[:])
        kb.op("dve", "reduce_max", lbs[:], lbr[:], AX.X)
        kb.op("dve", "tensor_tensor", lbe[:], lbr[:], V(lbs.h[:, :].unsqueeze(2).to_broadcast([128, 4, 4]), lbs.units), ALU.subtract)
        kb.op("act", "activation", lbe[:], lbe[:], AF.Exp)
        kb.op("dve", "reduce_sum", lbs[:], lbe[:], AX.X)
        kb.op("dve", "reciprocal", lbs[:], lbs[:])
        kb.op("dve", "reduce_sum", lb[:], lbe[:, :, 1:lidx + 1], AX.X)
        kb.op("dve", "tensor_tensor", lb[:], lb[:], lbs[:], ALU.mult)
        kb.op("dve", "tensor_scalar", oml[:], lb[:], -1.0, 1.0, ALU.mult, ALU.add)
        hgB = kb.sbuf("hgB", [128, 128], F32)
        load_bc(kb, hgB, hggrD, hggrD.h[0:1, :])
        wi = kb.sbuf("wi", [128, 16, 512], BF16)
        wog = kb.sbuf("wog", [128, 16, 512], BF16)
        kb.dma("pool", wi[:], wiD[:])
        kb.dma("pool", wog[:], wogD[:])
        Sf = kb.sbuf("Sf", [128, 4, 128], F32, nunits=4)
        Sb = kb.sbuf("Sb", [128, 4, 128], BF16, nunits=4)
        kb.op("dve", "memset", Sf.all(), 0.0)
        kb.op("dve", "memset", Sb.all(), 0.0)
        m64 = kb.sbuf("m64", [128, 128], F32)
        kb.op("dve", "tensor_single_scalar", m64[:], C["jmp"][:], 0.0, ALU.is_ge)
        kb.op("dve", "memset", m64[0:64, 64:128], 0.0)
        rmask = kb.sbuf("rmask", [128, 8, 64], F32)
        kb.op("dve", "memset", rmask[:], 1.0)
        kb.op("dve", "memset", rmask[:, :, 0:1], 0.0)
        rmf = V(rmask.h.rearrange("p a b -> p (a b)"), rmask.units)
        hnT = kb.sbuf("hnT_m", [128, 16, TS], BF16)
        pss = [kb.psum(f"psA{i}", [128, 512], F32) for i in range(2)]
        psc = [kb.psum(f"psC{i}", [128, 512], F32) for i in range(2)]
        pos = [kb.psum(f"psO{i}", [128, 512], F32) for i in range(2)]
        pkv = [kb.psum(f"psK{i}", [128, 512], F32) for i in range(2)]
        pc = 0
        it = 0
        for seg in range(nseg):
            emit_norm_segment(kb, C, hfullD, seg, modD, (0, D, 2 * D), ngD, hnT, pss)
            with kb.scope():
                wsl = [kb.sbuf(f"wsl{i}", [128, 16, 128], BF16) for i in range(2)]
                fg = kb.sbuf("fg", [128, 4, TS], F32)
                kkT = kb.sbuf("kkT", [128, 4, TS], F32)
                qT = kb.sbuf("qT", [128, 4, TS], F32)
                cum = kb.sbuf("cum", [128, 4, TS], F32)
                ex = kb.sbuf("exq", [128, 4, TS], F32)
                qdec = kb.sbuf("qdec", [128, 4, TS], BF16)
                kinv = kb.sbuf("kinv", [128, 4, TS], BF16)
                kend = kb.sbuf("kend", [128, 4, TS], BF16)
                elast = kb.sbuf("elast", [128, 4, 8], F32)
                vtm = kb.sbuf("vtm", [128, 4, 512], BF16)
                ogs = kb.sbuf("ogs", [128, 4, 512], F32)
                kendT = kb.sbuf("kendT", [128, 4, 512], BF16)
                STs = [kb.sbuf(f"ST{i}", [128, 128], BF16) for i in range(2)]
                osb = [kb.sbuf(f"osb{i}", [128, 128], F32) for i in range(2)]
                junk = kb.sbuf("junk", [128, 128], F32)
                ssn = [kb.sbuf(f"ssn{i}", [128, 4], F32) for i in range(2)]
                otile = [kb.sbuf(f"otile{i}", [128, 512], F32) for i in range(2)]
                for hh in range(4):
                    for ty in range(2):
                        p = pss[pc % 2]; w = wsl[pc % 2]; pc += 1
                        emit_proj_chunk(kb, w, wslabD[hh * 2 + ty], hnT, p)
                        if ty == 0:
                            kb.op("act", "activation", qT[:, hh, :], p[:], AF.Silu)
                        else:
                            kb.op("act", "activation", fg[:, hh, :], p[:], AF.Sigmoid)
                            kb.op("dve", "tensor_scalar", fg[:, hh, :], fg[:, hh, :], oml[:, hh:hh + 1], lb[:, hh:hh + 1], ALU.mult, ALU.add)
                            kb.op("dve", "tensor_scalar", kkT[:, hh, :], fg[:, hh, :], -1.0, 1.0, ALU.mult, ALU.add)
                            kb.op("act", "activation", fg[:, hh, :], fg[:, hh, :], AF.Ln)
                            kb.op("dve", "tensor_tensor_scan", cum[:, hh, :], rmf, fg[:, hh, :], 0.0, ALU.mult, ALU.add)
                    c3 = V(cum.h[:, hh, :].rearrange("p (a b) -> p a b", b=64), cum.units)
                    e3 = V(ex.h[:, hh, :].rearrange("p (a b) -> p a b", b=64), ex.units)
                    lastb = V(cum.h[:, hh, :].rearrange("p (a b) -> p a b", b=64)[:, :, 63:64].to_broadcast([128, 8, 64]), cum.units)
                    kb.op("act", "activation", ex[:, hh, :], cum[:, hh, :], AF.Exp)
                    kb.op("dve", "tensor_tensor", qdec[:, hh, :], qT[:, hh, :], ex[:, hh, :], ALU.mult)
                    kb.op("dve", "tensor_tensor", e3, lastb, c3, ALU.subtract)
                    kb.op("act", "activation", ex[:, hh, :], ex[:, hh, :], AF.Exp)
                    kb.op("dve", "tensor_tensor", kend[:, hh, :], kkT[:, hh, :], ex[:, hh, :], ALU.mult)
                    kb.op("dve", "tensor_scalar", ex[:, hh, :], cum[:, hh, :], -1.0, 80.0, ALU.mult, ALU.min)
                    kb.op("act", "activation", ex[:, hh, :], ex[:, hh, :], AF.Exp)
                    kb.op("dve", "tensor_tensor", kinv[:, hh, :], kkT[:, hh, :], ex[:, hh, :], ALU.mult)
                    kb.op("act", "activation", elast[:, hh, :], V(cum.h[:, hh, :].rearrange("p (a b) -> p a b", b=64)[:, :, 63], cum.units), AF.Exp)
                for tt in range(4):
                    cc_ = slice(tt * 128, (tt + 1) * 128)
                    p = pss[pc % 2]; pc += 1
                    for kc in range(16):
                        kb.op("pe", "matmul", p[:], hnT[:, kc, cc_], wi[:, kc, :], start=(kc == 0), stop=(kc == 15))
                    kb.op("act", "copy", vtm[:, tt, :], p[:])
                    p = pss[pc % 2]; pc += 1
                    for kc in range(16):
                        kb.op("pe", "matmul", p[:], hnT[:, kc, cc_], wog[:, kc, :], start=(kc == 0), stop=(kc == 15))
                    kb.op("act", "activation", ogs[:, tt, :], p[:], AF.Silu)
                    p = pss[pc % 2]; pc += 1
                    pT = V(p.h[:, 0:256].bitcast(BF16), p.units)
                    for hh in range(4):
                        kb.op("pe", "transpose", V(p.h[:, 0:256].bitcast(BF16)[:, hh * 128:(hh + 1) * 128], p.units),
                              kend[:, hh, cc_], identb[:])
                    kb.op("dve", "tensor_copy", kendT[:, tt, :], pT)
                for tt in range(4):
                    cc_ = slice(tt * 128, (tt + 1) * 128)
                    r0 = seg * TS + tt * 128
                    ot = otile[tt % 2]
                    for hh in range(4):
                        hs = slice(hh * 128, (hh + 1) * 128)
                        sc_, po_, ST = psc[it % 2], pos[it % 2], STs[it % 2]
                        ob_, ss_ = osb[it % 2], ssn[it % 2]
                        it += 1
                        kb.op("pe", "matmul", sc_[:, 0:128], kinv[:, hh, cc_], qdec[:, hh, cc_], start=True, stop=True)
                        kb.op("dve", "tensor_tensor", ST[:], sc_[:, 0:128], m64[:], ALU.mult)
                        kb.op("pe", "matmul", po_[:, 0:128], ST[:], vtm[:, tt, hs], start=True, stop=False)
                        for bk in range(2):
                            rows = slice(bk * 64, (bk + 1) * 64)
                            cb = slice(tt * 128 + bk * 64, tt * 128 + (bk + 1) * 64)
                            kb.op("pe", "matmul", po_[rows, 0:128], qdec[:, hh, cb], Sb.at(hh)[:, hh, :], start=False, stop=(bk == 1))
                            pk = pkv[(it + bk) % 2]
                            kb.op("pe", "matmul", pk[:, 0:128], kendT[rows, tt, hs], vtm[rows, tt, hs], start=True, stop=True)
                            blk = tt * 2 + bk
                            kb.op("dve", "scalar_tensor_tensor", Sf.at(hh)[:, hh, :], Sf.at(hh)[:, hh, :], elast[:, hh, blk:blk + 1],
                                  pk[:, 0:128], ALU.mult, ALU.add)
                            kb.op("act", "copy", Sb.at(hh)[:, hh, :], Sf.at(hh)[:, hh, :])
                        kb.op("act", "activation", junk[:], po_[:, 0:128], AF.Square, accum_out=ss_[:, 0:1])
                        kb.op("dve", "tensor_copy", ob_[:], po_[:, 0:128])
                        kb.op("dve", "tensor_scalar", ss_[:, 1:2], ss_[:, 0:1], 1.0 / 128, RMS_EPS, ALU.mult, ALU.add)
                        kb.op("act", "activation", ss_[:, 2:3], ss_[:, 1:2], AF.Sqrt)
                        kb.op("dve", "reciprocal", ss_[:, 3:4], ss_[:, 2:3])
                        kb.op("dve", "scalar_tensor_tensor", ob_[:], ob_[:], ss_[:, 3:4], hgB[:], ALU.mult, ALU.mult)
                        kb.op("dve", "tensor_tensor", ot[:, hs], ob_[:], ogs[:, tt, hs], ALU.mult)
                    kb.dma("sp", oD[r0:r0 + 128, :], ot[:])


def lay_c(c_b):
    return np.ascontiguousarray(c_b.reshape(16, 128).T)
def lay_w1(w1_l):
    NE = w1_l.shape[0]
    g = w1_l[:, :, 0::2].reshape(NE, 16, 128, 6, 128)
    u = w1_l[:, :, 1::2].reshape(NE, 16, 128, 6, 128)
    gu = np.concatenate([g, u], axis=-1)
    return np.ascontiguousarray(gu.transpose(0, 3, 2, 1, 4))
def lay_b1(b1_l):
    NE = b1_l.shape[0]
    g = b1_l[:, 0::2].reshape(NE, 6, 128)
    u = b1_l[:, 1::2].reshape(NE, 6, 128)
    gu = np.concatenate([g, u], axis=1)
    return np.ascontiguousarray(gu.transpose(2, 0, 1))
def lay_w2(w2_l):
    NE = w2_l.shape[0]
    return np.ascontiguousarray(w2_l.reshape(NE, 3, 2, 128, 2048).transpose(0, 1, 3, 2, 4))
def lay_slabs(w, cols_list):
    out = np.zeros((len(cols_list), 128, 16, 128), np.float32)
    for i, cols in enumerate(cols_list):
        blk = w[:, cols].reshape(16, 128, len(cols))
        out[i, :, :, :len(cols)] = blk.transpose(1, 0, 2)
    return out
def hgrn_slabs(w_in, j):
    cl = []
    for hh in range(4):
        hd = 4 * j + hh
        for ty in range(4):
            cl.append(np.arange(ty * 2048 + hd * 128, ty * 2048 + (hd + 1) * 128))
    return lay_slabs(w_in, cl)
def hgrn_lbraw(lb_all, j):
    x = lb_all[:, j * 512:(j + 1) * 512].reshape(4, 4, 128)
    return np.ascontiguousarray(x.transpose(2, 1, 0))
def ab_slabs(w_in, g):
    cl = []
    for c in range(4): cl.append(g * 512 + c * 128 + np.arange(128))
    for c in range(4): cl.append(2048 + g * 512 + c * 128 + np.arange(128))
    cl.append(4096 + g * 128 + np.arange(128))
    cl.append(4608 + g * 128 + np.arange(128))
    cl.append(5120 + g * 8 + np.arange(8))
    for s in range(16): cl.append(5152 + g * 512 + s * 32 + np.arange(32))
    return lay_slabs(w_in, cl)
def ab_params(inp, i, g):
    P = {}
    cw = inp["ab_conv_w"][i]; cb = inp["ab_conv_b"][i]
    chs = [g * 512 + c * 128 + np.arange(128) for c in range(4)] + [2048 + g * 128 + np.arange(128), 2560 + g * 128 + np.arange(128)]
    P["convw"] = np.ascontiguousarray(np.stack([cw[:, ch].T for ch in chs], axis=1))
    P["convb"] = np.ascontiguousarray(np.stack([cb[ch] for ch in chs], axis=1))
    P["dtb"] = np.ascontiguousarray(inp["ssd_dt_bias"][i][g * 8:(g + 1) * 8, None])
    P["alog"] = np.ascontiguousarray(inp["ssd_a_log"][i][g * 8:(g + 1) * 8, None])
    heads = g * 8 + (np.arange(512) // 64)
    P["ssdD"] = np.ascontiguousarray(inp["ssd_d"][i][heads].reshape(4, 128).T)
    P["normg"] = np.ascontiguousarray(inp["ssd_norm_g"][i][g * 512:(g + 1) * 512].reshape(4, 128).T)
    G = (32 * g + np.arange(32)).reshape(16, 2)
    def st(a):
        return np.ascontiguousarray(a[G].transpose(1, 2, 0).reshape(128, 16))
    P["lre"] = st(inp["s5_lam_re"][i]); P["lim"] = st(inp["s5_lam_im"][i]); P["lstep"] = st(inp["s5_log_step"][i])
    def bd(a):
        out = np.zeros((2, 64, 16, 2, 16), np.float32)
        for gg in range(2):
            out[gg, :, :, gg, :] = a[:, gg].transpose(1, 0, 2)
        return out.reshape(128, 16, 32)
    P["bre"] = bd(inp["s5_b_re"][i][G]); P["bim"] = bd(inp["s5_b_im"][i][G])
    P["cre"] = bd(inp["s5_c_re"][i][G].transpose(0, 1, 3, 2)); P["cim"] = bd(inp["s5_c_im"][i][G].transpose(0, 1, 3, 2))
    P["s5d"] = np.ascontiguousarray(inp["s5_d"][i][g * 512:(g + 1) * 512].reshape(16, 32).T)
    w_in = inp["ab_w_in"][i]
    P["wz"] = np.ascontiguousarray(w_in[:, g * 512:(g + 1) * 512].reshape(16, 128, 512).transpose(1, 0, 2))
    P["wdt"] = np.ascontiguousarray(w_in[:, 5120 + g * 8:5120 + (g + 1) * 8].reshape(16, 128, 8).transpose(1, 0, 2))
    P["dtbr"] = np.ascontiguousarray(inp["ssd_dt_bias"][i][None, g * 8:(g + 1) * 8])
    P["alogr"] = np.ascontiguousarray(inp["ssd_a_log"][i][None, g * 8:(g + 1) * 8])
    P["ssdDr"] = np.ascontiguousarray(inp["ssd_d"][i][heads][None, :])
    P["normgr"] = np.ascontiguousarray(inp["ssd_norm_g"][i][None, g * 512:(g + 1) * 512])
    return P
AB_PSHAPES = dict(convw=[128, 6, 4], convb=[128, 6], dtb=[8, 1], alog=[8, 1], ssdD=[128, 4], normg=[128, 4],
                  lre=[128, 16], lim=[128, 16], lstep=[128, 16], bre=[128, 16, 32], bim=[128, 16, 32],
                  cre=[128, 16, 32], cim=[128, 16, 32], s5d=[32, 16],
                  wz=[128, 16, 512], wdt=[128, 16, 8], dtbr=[1, 8], alogr=[1, 8], ssdDr=[1, 512], normgr=[1, 512])
def hgrn2_lay(w_in, j):
    cl = []
    for hh in range(4):
        hd = 4 * j + hh
        for ty in range(2):
            cl.append(np.arange(ty * 2048 + hd * 128, ty * 2048 + (hd + 1) * 128))
    slabs = lay_slabs(w_in, cl)
    wi = np.ascontiguousarray(w_in[:, 2 * 2048 + j * 512:2 * 2048 + (j + 1) * 512].reshape(16, 128, 512).transpose(1, 0, 2))
    wog = np.ascontiguousarray(w_in[:, 3 * 2048 + j * 512:3 * 2048 + (j + 1) * 512].reshape(16, 128, 512).transpose(1, 0, 2))
    return slabs, wi, wog


NT_CORE = 2048
SEQ = 8192
_PROGS = {}


def _common(kb):
    ct = kb.dram("ct", [128, 16], F32, kind="ExternalInput")
    adaw = kb.dram("adaw", [D, 6 * D], F32, kind="ExternalInput")
    adab = kb.dram("adab", [1, 6 * D], F32, kind="ExternalInput")
    modD = kb.dram("modD", [1, 6 * D], F32)
    C = make_consts(kb)
    emit_mod(kb, ct, adaw, adab, modD, 6 * D)
    return C, modD, ct


def _build_M_hg(lidx):
    nc = bass.Bass("TRN2", target_bir_lowering=False)
    with ExitStack() as st:
        kb = KB(nc, st)
        hfull = kb.dram("hfull", [SEQ, D], F32, kind="ExternalInput")
        ng = kb.dram("ng", [1, D], F32, kind="ExternalInput")
        wsl = kb.dram("wsl", [8, 128, 16, 128], F32, kind="ExternalInput")
        wi = kb.dram("wi", [128, 16, 512], F32, kind="ExternalInput")
        wog = kb.dram("wog", [128, 16, 512], F32, kind="ExternalInput")
        lbraw = kb.dram("lbraw", [128, 4, 4], F32, kind="ExternalInput")
        hgg = kb.dram("hgg", [1, 128], F32, kind="ExternalInput")
        oT = kb.dram("oT", [SEQ, 512], F32, kind="ExternalOutput")
        C, modD, ct = _common(kb)
        emit_hgrn_M2(kb, C, SEQ, lidx, hfull, modD, ng, wsl, wi, wog, lbraw, hgg, oT)
        kb.finish()
    return nc


def _build_M_ab():
    nc = bass.Bass("TRN2", target_bir_lowering=False)
    with ExitStack() as st:
        kb = KB(nc, st)
        hfull = kb.dram("hfull", [SEQ, D], F32, kind="ExternalInput")
        ng = kb.dram("ng", [1, D], F32, kind="ExternalInput")
        wsl = kb.dram("wsl", [27, 128, 16, 128], F32, kind="ExternalInput")
        P = {k: kb.dram("p_" + k, shp, F32, kind="ExternalInput") for k, shp in AB_PSHAPES.items()}
        yaT = kb.dram("yaT", [SEQ, 512], F32, kind="ExternalOutput")
        ybT = kb.dram("ybT", [512, SEQ], F32, kind="ExternalOutput")
        C, modD, ct = _common(kb)
        emit_ab_M(kb, C, SEQ, hfull, modD, ng, wsl, P, yaT, ybT)
        kb.finish()
    return nc


def _build_P(kind, final):
    nc = bass.Bass("TRN2", target_bir_lowering=False)
    with ExitStack() as st:
        kb = KB(nc, st)
        NT = NT_CORE
        hin = kb.dram("hin", [NT, D], F32, kind="ExternalInput")
        ng = kb.dram("ng", [1, D], F32, kind="ExternalInput")
        rw = kb.dram("rw", [D, NE], F32, kind="ExternalInput")
        rb = kb.dram("rb", [1, NE], F32, kind="ExternalInput")
        w1 = kb.dram("w1", [NE, 6, 128, 16, 256], F32, kind="ExternalInput")
        b1 = kb.dram("b1", [128, NE, 12], F32, kind="ExternalInput")
        w2 = kb.dram("w2", [NE, 3, 128, 2, D], F32, kind="ExternalInput")
        b2 = kb.dram("b2", [NE, D], F32, kind="ExternalInput")
        hout = kb.dram("hout", [NT, D], F32, kind="ExternalOutput")
        hmid = kb.dram("hmid", [NT, D], F32)
        C, modD, ct = _common(kb)
        if kind == "ab":
            yaT = kb.dram("yaT", [D, NT], F32, kind="ExternalInput")
            ybT = kb.dram("ybT", [D, NT], F32, kind="ExternalInput")
            gluw = kb.dram("gluw", [D, D], F32, kind="ExternalInput")
            glub = kb.dram("glub", [128, 16], F32, kind="ExternalInput")
            wout = kb.dram("wout", [2 * D, D], F32, kind="ExternalInput")
            ybfD = kb.dram("ybfD", [D, NT], BF16)
            emit_glu(kb, NT, ybT, gluw, glub, ybfD)
            emit_outproj(kb, NT, hin, hmid, modD, 2 * D, [(yaT, 16), (ybfD, 16)], wout, 32)
        else:
            oT = kb.dram("oT", [D, NT], F32, kind="ExternalInput")
            wout = kb.dram("wout", [D, D], F32, kind="ExternalInput")
            emit_outproj(kb, NT, hin, hmid, modD, 2 * D, [(oT, 16)], wout, 16)
        fin = None
        if final:
            faw = kb.dram("faw", [D, 2 * D], F32, kind="ExternalInput")
            fab = kb.dram("fab", [1, 2 * D], F32, kind="ExternalInput")
            fg = kb.dram("fg", [1, D], F32, kind="ExternalInput")
            fmodD = kb.dram("fmodD", [1, 2 * D], F32)
            emit_mod(kb, ct, faw, fab, fmodD, 2 * D)
            fin = (fmodD, fg, hout)
        emit_moe(kb, C, NT, hmid, hout, modD, (3 * D, 4 * D, 5 * D), ng, rw, rb, w1, b1, w2, b2, fin=fin)
        kb.finish()
    return nc


def _prog(key, fn, *a):
    if key not in _PROGS:
        _PROGS[key] = fn(*a)
    return _PROGS[key]


def kernel(**inp):
    inp = {k: np.asarray(v) for k, v in inp.items()}
    x = inp["x"].astype(np.float32, copy=False)
    c = inp["c"].astype(np.float32, copy=False)
    B, L, _ = x.shape
    nq = L // NT_CORE
    ncore = B * nq
    A = np.ascontiguousarray
    h = [A(x[cc // nq, (cc % nq) * NT_CORE:(cc % nq + 1) * NT_CORE]) for cc in range(ncore)]
    cts = [lay_c(c[b]) for b in range(B)]
    depth = inp["ada_w"].shape[0]
    for l in range(depth):
        i = l // 2
        final = (l == depth - 1)
        adaw, adab = A(inp["ada_w"][l]), A(inp["ada_b"][l][None])
        hfull = [np.concatenate(h[b * nq:(b + 1) * nq], axis=0) for b in range(B)]
        if l % 2 == 0:
            nc = _prog("M_ab", _build_M_ab)
            in_maps = []
            for cc in range(ncore):
                b, g = cc // 4, cc % 4
                d = dict(hfull=hfull[b], ct=cts[b], adaw=adaw, adab=adab, ng=A(inp["norm1_g"][l][None]),
                         wsl=ab_slabs(inp["ab_w_in"][i], g))
                for k, v in ab_params(inp, i, g).items():
                    d["p_" + k] = v
                in_maps.append(d)
            res = run_bass_kernel_spmd(nc, in_maps, core_ids=list(range(ncore)))
            yaT = [np.concatenate([res.results[b * 4 + g]["yaT"].T for g in range(4)], axis=0) for b in range(B)]
            ybT = [np.concatenate([res.results[b * 4 + g]["ybT"] for g in range(4)], axis=0) for b in range(B)]
            extra = lambda b, q: dict(yaT=A(yaT[b][:, q * NT_CORE:(q + 1) * NT_CORE]), ybT=A(ybT[b][:, q * NT_CORE:(q + 1) * NT_CORE]),
                                      gluw=A(inp["s5_glu_w"][i]), glub=A(inp["s5_glu_b"][i].reshape(16, 128).T),
                                      wout=A(inp["ab_w_out"][i]))
            kind = "ab"
        else:
            nc = _prog(("M_hg", l), _build_M_hg, l)
            in_maps = []
            for cc in range(ncore):
                b, j = cc // 4, cc % 4
                sl_, wi_, wog_ = hgrn2_lay(inp["hg_w_in"][i], j)
                in_maps.append(dict(hfull=hfull[b], ct=cts[b], adaw=adaw, adab=adab, ng=A(inp["norm1_g"][l][None]),
                                    wsl=sl_, wi=wi_, wog=wog_, lbraw=hgrn_lbraw(inp["hg_lower_bounds"], j),
                                    hgg=A(inp["hg_norm_g"][i][None, :])))
            res = run_bass_kernel_spmd(nc, in_maps, core_ids=list(range(ncore)))
            oT = [np.concatenate([res.results[b * 4 + j]["oT"].T for j in range(4)], axis=0) for b in range(B)]
            extra = lambda b, q: dict(oT=A(oT[b][:, q * NT_CORE:(q + 1) * NT_CORE]), wout=A(inp["hg_w_out"][i]))
            kind = "hg"
        del res
        nc = _prog(("P", kind, final), _build_P, kind, final)
        shared = dict(adaw=adaw, adab=adab, ng=A(inp["norm2_g"][l][None]), rw=A(inp["moe_router_w"][l]),
                      rb=A(inp["moe_router_b"][l][None]), w1=lay_w1(inp["moe_w1"][l]), b1=lay_b1(inp["moe_b1"][l]),
                      w2=lay_w2(inp["moe_w2"][l]), b2=A(inp["moe_b2"][l]))
        if final:
            shared.update(faw=A(inp["final_ada_w"]), fab=A(inp["final_ada_b"][None]), fg=A(inp["final_norm_g"][None]))
        in_maps = []
        for cc in range(ncore):
            b, q = cc // nq, cc % nq
            d = dict(shared, hin=h[cc], ct=cts[b])
            d.update(extra(b, q))
            in_maps.append(d)
        res = run_bass_kernel_spmd(nc, in_maps, core_ids=list(range(ncore)))
        h = [np.asarray(res.results[cc]["hout"]) for cc in range(ncore)]
        del res
    out = np.stack([np.concatenate(h[b * nq:(b + 1) * nq], axis=0) for b in range(B)], axis=0)
    return out.astype(np.float32)
```

```python
from contextlib import ExitStack
from concourse.bass_utils import run_bass_kernel_spmd
import numpy as np
import concourse.bass as bass
import concourse.mybir as mybir

F32 = mybir.dt.float32
BF16 = mybir.dt.bfloat16
AF = mybir.ActivationFunctionType
ALU = mybir.AluOpType
AX = mybir.AxisListType


class Unit:
    __slots__ = ("last_write", "reads", "name", "excl")

    def __init__(self, name=""):
        self.excl = False
        self.last_write = None
        self.reads = []
        self.name = name


class V:
    __slots__ = ("ap", "units")

    def __init__(self, ap, units):
        self.ap = ap
        self.units = units


class T:
    def __init__(self, handle, name, nunits=1):
        self.h = handle
        self.name = name
        self.units = [Unit(f"{name}.{i}") for i in range(nunits)]

    def __getitem__(self, idx):
        return V(self.h[idx], [self.units[0]])

    def at(self, i):
        return _At(self, i)

    def all(self, idx=slice(None)):
        return V(self.h[idx], list(self.units))

    def ap(self):
        return self.h


class _At:
    def __init__(self, t, i):
        self.t, self.i = t, i

    def __getitem__(self, idx):
        return V(self.t.h[idx], [self.t.units[self.i]])


class Op:
    __slots__ = ("eng", "emit", "waits", "idx", "needed", "num", "sem", "is_dma")

    def __init__(self, eng, emit, is_dma=False):
        self.eng = eng
        self.emit = emit
        self.waits = []
        self.idx = -1
        self.needed = False
        self.num = -1
        self.sem = None
        self.is_dma = is_dma


class _Scope:
    def __init__(self, kb):
        self.kb = kb

    def __enter__(self):
        self.mark = (self.kb.sb_off, self.kb.ps_off)
        return self

    def __exit__(self, *a):
        self.kb.barrier()
        self.kb.sb_off, self.kb.ps_off = self.mark
        return False


class KB:
    CE = ("pe", "dve", "act", "pool", "sp")

    def __init__(self, nc, stack, n_dma_sems=24):
        self.nc = nc
        self.stack = stack
        self.eng = {"pe": nc.tensor, "dve": nc.vector, "act": nc.scalar,
                    "pool": nc.gpsimd, "sp": nc.sync}
        self.sem = {e: stack.enter_context(nc.semaphore(f"s_{e}")) for e in self.CE}
        self.dma_sems = [stack.enter_context(nc.semaphore(f"s_dma{i}")) for i in range(n_dma_sems)]
        self.dma_last = [None] * n_dma_sems
        self.dma_rr = 0
        self.ops = []
        self.stream_len = {e: 0 for e in self.CE}
        self.seen = {e: {} for e in self.CE}
        self.sb_big = None
        self.ps_big = None
        self.sb_off = 0
        self.ps_off = 0
        self.sb_peak = 0
        ses = True
        self.same_engine_sync = {"pe": False, "dve": ses, "act": ses, "pool": ses, "sp": True}

    SB_WORDS = 51200
    PS_WORDS = 4096

    def scope(self):
        return _Scope(self)

    def _carve(self, big, off, shape, dtype):
        n = 1
        for d in shape[1:]:
            n *= d
        esz = mybir.dt.size(dtype)
        words = (n * esz + 3) // 4
        words = (words + 7) // 8 * 8
        ap = big[0:shape[0], off:off + words]
        if dtype != F32:
            ap = ap.bitcast(dtype)
        ap = ap[:, 0:n]
        if len(shape) > 2:
            names = " ".join(f"d{i}" for i in range(1, len(shape)))
            kw = {f"d{i}": shape[i] for i in range(1, len(shape))}
            ap = ap.rearrange(f"p ({names}) -> p {names}", **kw)
        return ap, words

    def sbuf(self, name, shape, dtype, nunits=1):
        if self.sb_big is None:
            self.sb_big = self.stack.enter_context(self.nc.sbuf_tensor("sb_big", [128, self.SB_WORDS], F32))
        ap, words = self._carve(self.sb_big, self.sb_off, shape, dtype)
        self.sb_off += words
        self.sb_peak = max(self.sb_peak, self.sb_off)
        assert self.sb_off <= self.SB_WORDS, f"SBUF overflow allocating {name}: {self.sb_off * 4} B"
        return T(ap, name, nunits)

    def psum(self, name, shape, dtype, nunits=1):
        if self.ps_big is None:
            self.ps_big = self.stack.enter_context(self.nc.psum_tensor("ps_big", [128, self.PS_WORDS], F32))
        n = 1
        for d in shape[1:]:
            n *= d
        w = (n * mybir.dt.size(dtype) + 3) // 4
        if (self.ps_off % 512) + min(w, 512) > 512:
            self.ps_off = (self.ps_off + 511) // 512 * 512
        ap, words = self._carve(self.ps_big, self.ps_off, shape, dtype)
        self.ps_off += words
        assert self.ps_off <= self.PS_WORDS, f"PSUM overflow allocating {name}"
        t = T(ap, name, nunits)
        for u in t.units:
            u.excl = True
        return t

    def barrier(self):
        last = {}
        for o in self.ops:
            if o.emit is not None:
                last[self._stream_key(o)] = o
        for e in self.CE:
            b = Op(e, None)
            b.idx = self.stream_len[e]
            b.sem = self.sem[e]
            for o in last.values():
                if (not o.is_dma) and o.eng == e:
                    continue
                self._add_wait(b, o)
            self.ops.append(b)

    def dram(self, name, shape, dtype, kind="Internal", nunits=1):
        h = self.nc.dram_tensor(name, list(shape), dtype, kind=kind).ap()
        return T(h, name, nunits)

    def _stream_key(self, op):
        return ("dma", id(op.sem)) if op.is_dma else op.eng

    def _add_wait(self, op, prod):
        if prod is None or prod is op:
            return
        if (not prod.is_dma) and prod.eng == op.eng and not self.same_engine_sync[op.eng]:
            return
        key = self._stream_key(prod)
        pidx = prod.idx
        if self.seen[op.eng].get(key, -1) >= pidx:
            return
        self.seen[op.eng][key] = pidx
        prod.needed = True
        op.waits.append(prod)

    def _track(self, op, reads, writes):
        ex = [v for v in reads if any(u.excl for u in v.units)]
        if ex:
            reads = [v for v in reads if v not in ex]
            writes = list(writes) + ex
        for v in reads:
            for u in v.units:
                self._add_wait(op, u.last_write)
        for v in writes:
            for u in v.units:
                self._add_wait(op, u.last_write)
                for r in reversed(u.reads):
                    self._add_wait(op, r)
        for v in reads:
            for u in v.units:
                u.reads.append(op)
                if len(u.reads) > 64:
                    u.reads = u.reads[-48:]
        for v in writes:
            for u in v.units:
                u.last_write = op
                u.reads = []

    WRITE_KW = ("out", "accum_out", "out_max", "out_indices")

    def op(self, e, fn, *args, **kw):
        reads, writes = [], []
        for i, a in enumerate(args):
            if isinstance(a, V):
                (writes if i == 0 else reads).append(a)
        for k, a in kw.items():
            if isinstance(a, V):
                (writes if k in self.WRITE_KW else reads).append(a)
        extra_r = kw.pop("_reads", [])
        extra_w = kw.pop("_writes", [])
        reads += extra_r
        writes += extra_w
        rargs = [a.ap if isinstance(a, V) else a for a in args]
        rkw = {k: (a.ap if isinstance(a, V) else a) for k, a in kw.items()}
        engine = self.eng[e]

        def emit():
            return getattr(engine, fn)(*rargs, **rkw)

        o = Op(e, emit)
        o.idx = self.stream_len[e]
        self.stream_len[e] += 1
        o.sem = self.sem[e]
        self._track(o, reads, writes)
        self.ops.append(o)
        return o

    def dma(self, q, out, in_, **kw):
        engine = self.eng[q]
        si = self.dma_rr
        self.dma_rr = (self.dma_rr + 1) % len(self.dma_sems)
        oap, iap = out.ap, in_.ap

        def emit():
            return engine.dma_start(out=oap, in_=iap, **kw)

        o = Op(q, emit, is_dma=True)
        o.sem = self.dma_sems[si]
        prev = self.dma_last[si]
        o.idx = (prev.idx + 1) if prev is not None else 0
        self._add_wait(o, prev)
        self.dma_last[si] = o
        self._track(o, [in_], [out])
        o.needed = True
        self.ops.append(o)
        return o

    def collective(self, kind, op, groups, in_, out):
        engine = self.eng["pool"]
        si = self.dma_rr
        self.dma_rr = (self.dma_rr + 1) % len(self.dma_sems)
        oap, iap = out.ap, in_.ap

        def emit():
            return engine.collective_compute(kind, op, replica_groups=groups, ins=[iap], outs=[oap])

        o = Op("pool", emit, is_dma=True)
        o.sem = self.dma_sems[si]
        prev = self.dma_last[si]
        o.idx = (prev.idx + 1) if prev is not None else 0
        self._add_wait(o, prev)
        self.dma_last[si] = o
        self._track(o, [in_], [out])
        o.needed = True
        self.ops.append(o)
        return o

    def finish(self):
        fin = Op("sp", None)
        fin.idx = self.stream_len["sp"]
        last = {}
        for o in self.ops:
            if o.emit is not None:
                last[self._stream_key(o)] = o
        for o in last.values():
            if o.eng == "sp" and not o.is_dma:
                continue
            self._add_wait(fin, o)
        counters = {}
        for o in self.ops:
            if o.needed:
                key = self._stream_key(o)
                counters[key] = counters.get(key, 0) + 1
                o.num = counters[key]
        nwaits = 0
        for o in self.ops + [fin]:
            engine = self.eng[o.eng]
            for p in o.waits:
                val = p.num * (16 if p.is_dma else 1)
                engine.wait_ge(p.sem, val)
                nwaits += 1
            if o.emit is None:
                continue
            ins = o.emit()
            if o.needed:
                ins.then_inc(o.sem, 16 if o.is_dma else 1)
        self.stats = dict(nops=len(self.ops), nwaits=nwaits,
                          counters={str(k): v for k, v in counters.items()})
        return self.stats


I32 = mybir.dt.int32
D = 2048
NE = 32
FF = 768
ALPHA = 1.702
LIMIT = 7.0
F7 = ALPHA * LIMIT / (1.0 + np.exp(-ALPHA * LIMIT))
RMS_EPS = 1e-6


def bc(t, idx, shape):
    return V(t.h[idx].to_broadcast(list(shape)), t.units)


def make_consts(kb):
    C = {}
    io = kb.sbuf("c_io", [128, 128], I32)
    iof = kb.sbuf("c_iof", [128, 128], F32)
    C["ident"] = kb.sbuf("c_ident", [128, 128], F32)
    C["identb"] = kb.sbuf("c_identb", [128, 128], BF16)
    kb.op("pool", "iota", io[:], [[1, 128]], base=0, channel_multiplier=-1)
    kb.op("dve", "tensor_copy", iof[:], io[:])
    kb.op("dve", "tensor_single_scalar", C["ident"][:], iof[:], 0.0, ALU.is_equal)
    kb.op("dve", "tensor_copy", C["identb"][:], C["ident"][:])
    C["jmp"] = iof
    return C


def emit_mod(kb, ctD, wD, bD, modD, ncols, c0=0):
    with kb.scope():
        cs = kb.sbuf("cs", [128, 16], F32)
        kb.dma("sp", cs[:], ctD[:])
        kb.op("act", "activation", cs[:], cs[:], AF.Silu)
        ws = [kb.sbuf(f"adaw{i}", [128, 16, 512], F32) for i in range(2)]
        brow = kb.sbuf("adab", [1, ncols], F32)
        orow = kb.sbuf("modrow", [1, ncols], F32)
        kb.dma("sp", brow[:], bD[0:1, 0:ncols])
        ps = [kb.psum(f"modps{i}", [1, 512], F32) for i in range(2)]
        wv = wD.h.rearrange("(kc p) n -> p kc n", p=128)
        for cb in range(c0 // 512, ncols // 512):
            w = ws[cb % 2]
            kb.dma("sp", w[:], V(wv[:, :, cb * 512:(cb + 1) * 512], wD.units))
            p = ps[cb % 2]
            for kc in range(16):
                kb.op("pe", "matmul", p[:], cs[:, kc:kc + 1], w[:, kc, :], start=(kc == 0), stop=(kc == 15))
            kb.op("dve", "tensor_tensor", orow[:, cb * 512:(cb + 1) * 512], p[:], brow[:, cb * 512:(cb + 1) * 512], ALU.add)
        kb.dma("sp", modD[0:1, c0:ncols], orow[0:1, c0:ncols])


def load_bc(kb, t, dramT, row_ap):
    kb.dma("sp", t[:], V(row_ap.partition_broadcast(t.h.shape[0]), dramT.units))


def emit_rmsnorm_tile(kb, ht, A, sh, hn, ss, junk):
    kb.op("act", "activation", junk[:], ht[:], AF.Square, accum_out=ss[:, 0:1])
    kb.op("dve", "tensor_scalar", ss[:, 1:2], ss[:, 0:1], 1.0 / D, RMS_EPS, ALU.mult, ALU.add)
    kb.op("act", "activation", ss[:, 2:3], ss[:, 1:2], AF.Sqrt)
    kb.op("dve", "reciprocal", ss[:, 3:4], ss[:, 2:3])
    kb.op("dve", "scalar_tensor_tensor", junk[:], ht[:], ss[:, 3:4], A[:], ALU.mult, ALU.mult)
    kb.op("dve", "tensor_tensor", hn[:], junk[:], sh[:], ALU.add)


STOP = ""


def emit_moe(kb, C, NT, hinD, houtD, modD, moff, ngD, rwD, rbD, w1D, b1D, w2D, b2D, fin=None):
    SP = 1024
    npass = NT // SP
    ntile = SP // 128
    with kb.scope():
        hnT = kb.sbuf("hnT", [128, 16, SP], BF16)
        acc = kb.sbuf("acc", [128, ntile, D], F32, nunits=ntile)
        gcol = kb.sbuf("gcol", [128, ntile, NE], F32)
        b1t = kb.sbuf("b1t", [128, NE, 12], F32)
        b1a = kb.sbuf("b1a", [128, NE, 6], F32)
        kb.dma("sp", b1t[:], b1D[:])
        kb.op("dve", "tensor_scalar", b1a[:], b1t[:, :, 0:6], ALPHA, None, ALU.mult)
        po = kb.psum("po", [128, 2048], F32, nunits=2)
        pgu = kb.psum("pgu", [128, 4, 512], F32, nunits=4)
        for ps_ in range(npass):
            with kb.scope():
                A2 = kb.sbuf("A2", [128, D], F32)
                sh2 = kb.sbuf("sh2", [128, D], F32)
                tmpg = kb.sbuf("tmpg", [128, D], F32)
                load_bc(kb, A2, modD, modD.h[0:1, moff[1]:moff[1] + D])
                load_bc(kb, tmpg, ngD, ngD.h[0:1, :])
                load_bc(kb, sh2, modD, modD.h[0:1, moff[0]:moff[0] + D])
                kb.op("dve", "scalar_tensor_tensor", A2[:], A2[:], 1.0, tmpg[:], ALU.add, ALU.mult)
                rw = kb.sbuf("rw", [128, 16, NE], F32)
                kb.dma("sp", rw[:], V(rwD.h.rearrange("(kc p) e -> p kc e", p=128), rwD.units))
                rb = kb.sbuf("rb", [128, NE], F32)
                load_bc(kb, rb, rbD, rbD.h[0:1, :])
                b2s = kb.sbuf("b2s", [NE, D], F32)
                kb.dma("sp", b2s[:], b2D[:])
                hts = [kb.sbuf(f"ht{i}", [128, D], F32) for i in range(2)]
                hns = [kb.sbuf(f"hn{i}", [128, D], F32) for i in range(2)]
                hT32 = kb.sbuf("hT32", [128, D], F32)
                sss = [kb.sbuf(f"ss{i}", [128, 4], F32) for i in range(2)]
                lgs = kb.sbuf("lgs", [128, NE], F32)
                m8 = kb.sbuf("m8", [128, 8], F32)
                msk = kb.sbuf("msk", [128, NE], F32)
                ex = kb.sbuf("ex", [128, NE], F32)
                sm = kb.sbuf("sm", [128, 4], F32)
                comb = kb.sbuf("comb", [128, NE], F32)
                combT = kb.sbuf("combT", [NE, 128], F32)
                lg = V(pgu.h[:, 0, 0:NE], [pgu.units[0]])
                cT = V(pgu.h[0:NE, 1, 0:128], [pgu.units[1]])
                for i in range(ntile):
                    gt = ps_ * ntile + i
                    ht, hn, ss = hts[i % 2], hns[i % 2], sss[i % 2]
                    kb.dma("sp", ht[:], hinD[gt * 128:(gt + 1) * 128, :])
                    emit_rmsnorm_tile(kb, ht, A2, sh2, hn, ss, tmpg)
                    tp = po.all()
                    for kc in range(16):
                        kb.op("pe", "transpose", V(po.h[:, kc * 128:(kc + 1) * 128], po.units),
                              hn[:, kc * 128:(kc + 1) * 128], C["ident"][:])
                    for bk in range(4):
                        kb.op("act", "copy", hT32[:, bk * 512:(bk + 1) * 512], V(po.h[:, bk * 512:(bk + 1) * 512], po.units))
                        kb.op("dve", "tensor_copy", hnT[:, bk * 4:(bk + 1) * 4, i * 128:(i + 1) * 128],
                              V(po.h[:, bk * 512:(bk + 1) * 512].rearrange("p (kc t) -> p kc t", t=128), po.units))
                    for kc in range(16):
                        kb.op("pe", "matmul", lg, hT32[:, kc * 128:(kc + 1) * 128], rw[:, kc, :],
                              start=(kc == 0), stop=(kc == 15))
                    kb.op("dve", "tensor_tensor", lgs[:], lg, rb[:], ALU.add)
                    kb.op("dve", "max", m8[:], lgs[:])
                    kb.op("dve", "tensor_scalar", msk[:], lgs[:], m8[:, 3:4], None, ALU.is_ge)
                    kb.op("dve", "tensor_scalar", sm[:, 0:1], m8[:, 0:1], -1.0, None, ALU.mult)
                    kb.op("act", "activation", ex[:], lgs[:], AF.Exp, bias=sm[:, 0:1])
                    kb.op("dve", "tensor_tensor", ex[:], ex[:], msk[:], ALU.mult)
                    kb.op("dve", "reduce_sum", sm[:, 1:2], ex[:], AX.X)
                    kb.op("dve", "reciprocal", sm[:, 2:3], sm[:, 1:2])
                    kb.op("dve", "tensor_scalar", comb[:], ex[:], sm[:, 2:3], None, ALU.mult)
                    kb.op("dve", "tensor_scalar", gcol[:, i, :], comb[:], 1.0 / ALPHA, None, ALU.mult)
                    kb.op("pe", "transpose", cT, comb[:], C["ident"][:])
                    kb.op("act", "copy", combT[:], cT)
                    for db in range(4):
                        kb.op("pe", "matmul", V(po.h[:, db * 512:(db + 1) * 512], po.units),
                              combT[:], b2s[:, db * 512:(db + 1) * 512], start=True, stop=True)
                    for bk in range(4):
                        kb.op("act", "copy", acc.at(i)[:, i, bk * 512:(bk + 1) * 512], V(po.h[:, bk * 512:(bk + 1) * 512], po.units))
            with kb.scope():
              if STOP != "stage0":
                  actT = [kb.sbuf(f"actT{i}", [128, 6, SP], BF16) for i in range(2)]
                  w1s = [kb.sbuf(f"w1s{i}", [128, 16, 256], BF16) for i in range(4)]
                  w2s = [kb.sbuf(f"w2s{i}", [128, 2, D], BF16) for i in range(3)]
                  a1s = [kb.sbuf(f"a1_{i}", [128, 512], F32) for i in range(2)]
                  u1s = [kb.sbuf(f"u1_{i}", [128, 512], F32) for i in range(2)]
                  nblk = SP // 512
                  for j in range(4):
                      kb.dma("pool", w1s[j][:], w1D[0, j])
                  for j in range(3):
                      kb.dma("pool", w2s[j][:], w2D[0, j])
                  cnt = 0
                  for e in range(NE):
                      aT = actT[e % 2]
                      for mp in range(6):
                          n = e * 6 + mp
                          w = w1s[n % 4]
                          for tb in range(nblk):
                              pg = pgu.at((cnt % 2) * 2)[:, (cnt % 2) * 2, :]
                              pu = pgu.at((cnt % 2) * 2 + 1)[:, (cnt % 2) * 2 + 1, :]
                              a1, u1 = a1s[cnt % 2], u1s[cnt % 2]
                              cnt += 1
                              for kc in range(16):
                                  kb.op("pe", "matmul", pg, w[:, kc, 0:128], hnT[:, kc, tb * 512:(tb + 1) * 512],
                                        start=(kc == 0), stop=(kc == 15))
                              for kc in range(16):
                                  kb.op("pe", "matmul", pu, w[:, kc, 128:256], hnT[:, kc, tb * 512:(tb + 1) * 512],
                                        start=(kc == 0), stop=(kc == 15))
                              kb.op("act", "activation", a1[:], pg, AF.Silu, bias=b1a[:, e, mp:mp + 1], scale=ALPHA)
                              kb.op("dve", "tensor_scalar", u1[:], pu, b1t[:, e, 6 + mp:7 + mp], LIMIT, ALU.add, ALU.min)
                              kb.op("dve", "tensor_scalar", u1[:], u1[:], -LIMIT, 1.0, ALU.max, ALU.add)
                              kb.op("dve", "scalar_tensor_tensor", aT[:, mp, tb * 512:(tb + 1) * 512], a1[:], float(F7),
                                    u1[:], ALU.min, ALU.mult)
                          n2 = n + 4
                          if n2 < NE * 6:
                              kb.dma("pool", w1s[n2 % 4][:], w1D[n2 // 6, n2 % 6])
                      for tt in range(ntile):
                          for hf in range(2):
                              pv = po.at(hf)
                              for dbh in range(2):
                                  db = hf * 2 + dbh
                                  for k in range(6):
                                      kb.op("pe", "matmul", pv[:, db * 512:(db + 1) * 512],
                                            aT[:, k, tt * 128:(tt + 1) * 128], w2s[k // 2][:, k % 2, db * 512:(db + 1) * 512],
                                            start=(k == 0), stop=(k == 5))
                              for dbh in range(2):
                                  sl = slice((hf * 2 + dbh) * 512, (hf * 2 + dbh + 1) * 512)
                                  kb.op("dve", "scalar_tensor_tensor", acc.at(tt)[:, tt, sl], pv[:, sl],
                                        gcol[:, tt, e:e + 1], acc.at(tt)[:, tt, sl], ALU.mult, ALU.add)
                      if e + 1 < NE:
                          for j in range(3):
                              kb.dma("pool", w2s[j][:], w2D[e + 1, j])
            with kb.scope():
                g2 = kb.sbuf("g2", [128, D], F32)
                load_bc(kb, g2, modD, modD.h[0:1, moff[2]:moff[2] + D])
                hts = [kb.sbuf(f"hr{i}", [128, D], F32) for i in range(2)]
                if fin is not None:
                    fmodD, fgD, outD = fin
                    fA = kb.sbuf("fA", [128, D], F32)
                    fsh = kb.sbuf("fsh", [128, D], F32)
                    tg = kb.sbuf("ftg", [128, D], F32)
                    load_bc(kb, fA, fmodD, fmodD.h[0:1, D:2 * D])
                    load_bc(kb, tg, fgD, fgD.h[0:1, :])
                    load_bc(kb, fsh, fmodD, fmodD.h[0:1, 0:D])
                    kb.op("dve", "scalar_tensor_tensor", fA[:], fA[:], 1.0, tg[:], ALU.add, ALU.mult)
                    fss = [kb.sbuf(f"fss{i}", [128, 4], F32) for i in range(2)]
                    fo = [kb.sbuf(f"fo{i}", [128, D], F32) for i in range(2)]
                for i in range(ntile):
                    gt = ps_ * ntile + i
                    ht = hts[i % 2]
                    kb.dma("sp", ht[:], hinD[gt * 128:(gt + 1) * 128, :])
                    kb.op("dve", "tensor_tensor", acc.at(i)[:, i, :], acc.at(i)[:, i, :], g2[:], ALU.mult)
                    kb.op("dve", "tensor_tensor", ht[:], ht[:], acc.at(i)[:, i, :], ALU.add)
                    if fin is None:
                        kb.dma("sp", houtD[gt * 128:(gt + 1) * 128, :], ht[:])
                    else:
                        emit_rmsnorm_tile(kb, ht, fA, fsh, fo[i % 2], fss[i % 2], tg)
                        kb.dma("sp", outD[gt * 128:(gt + 1) * 128, :], fo[i % 2][:])


TS = 512


def make_sel(kb, C):
    rowsel = kb.sbuf("rowsel", [128, 128, 128], BF16)
    colsel = kb.sbuf("colsel", [128, 128, 128], BF16)
    kb.op("dve", "tensor_copy", rowsel[:], V(C["identb"].h[:, :].unsqueeze(2).to_broadcast([128, 128, 128]), C["identb"].units))
    scr = kb.dram("identscr", [1, 128 * 128], BF16)
    kb.dma("sp", V(scr.h.rearrange("o (a b) -> (o a) b", a=128), scr.units), C["identb"][:])
    kb.dma("sp", V(colsel.h.rearrange("p a b -> p (a b)"), colsel.units), V(scr.h[0:1, :].partition_broadcast(128), scr.units))
    C["rowsel"], C["colsel"] = rowsel, colsel
    ones = kb.sbuf("ones32", [128, 128], F32)
    kb.op("dve", "memset", ones[:], 1.0)
    C["ones"] = ones


def emit_norm_segment(kb, C, hfullD, seg, modD, moff, ngD, hnT, ps):
    with kb.scope():
        A1 = kb.sbuf("A1", [128, D], F32)
        sh1 = kb.sbuf("sh1", [128, D], F32)
        tg = kb.sbuf("tg1", [128, D], F32)
        load_bc(kb, A1, modD, modD.h[0:1, moff[1]:moff[1] + D])
        load_bc(kb, tg, ngD, ngD.h[0:1, :])
        load_bc(kb, sh1, modD, modD.h[0:1, moff[0]:moff[0] + D])
        kb.op("dve", "scalar_tensor_tensor", A1[:], A1[:], 1.0, tg[:], ALU.add, ALU.mult)
        ht = kb.sbuf("ht_m", [128, D], F32)
        hn = kb.sbuf("hn_m", [128, D], F32)
        ss = kb.sbuf("ss_m", [128, 4], F32)
        for i in range(TS // 128):
            r0 = seg * TS + i * 128
            kb.dma("sp", ht[:], hfullD[r0:r0 + 128, :])
            emit_rmsnorm_tile(kb, ht, A1, sh1, hn, ss, tg)
            for bk in range(4):
                p = ps[bk % 2]
                for j in range(4):
                    kc = bk * 4 + j
                    kb.op("pe", "transpose", p[:, j * 128:(j + 1) * 128], hn[:, kc * 128:(kc + 1) * 128], C["ident"][:])
                kb.op("dve" if bk % 2 else "act", "tensor_copy" if bk % 2 else "copy",
                      hnT[:, bk * 4:(bk + 1) * 4, i * 128:(i + 1) * 128],
                      V(p.h[:, :].rearrange("p (kc t) -> p kc t", t=128), p.units))


def emit_proj_chunk(kb, wslot, wD_slab, hnT, p, mcols=128):
    src = wD_slab if mcols == 128 else V(wD_slab.ap[:, :, 0:mcols], wD_slab.units)
    kb.dma("pool", wslot[:, :, 0:mcols], src)
    for kc in range(16):
        kb.op("pe", "matmul", p[0:mcols, :], wslot[:, kc, 0:mcols], hnT[:, kc, :], start=(kc == 0), stop=(kc == 15))


GLA_PQ = "alt"
GLA_D1 = "dve"


def emit_gla(kb, C, decT, kT, qT, vhi, vlo, vrow0, nv, S, sbase, po, porow0, first, last, pbs, tmps, cnt):
    for v in range(nv):
        pb = pbs[cnt[0] % len(pbs)]
        d1, st, pq = tmps[cnt[0] % len(tmps)]
        cnt[0] += 1
        sel = C["rowsel"][:, vrow0 + v, :]
        kb.op("pe", "matmul", pb[:], sel, vhi, start=True, stop=False)
        kb.op("pe", "matmul", pb[:], sel, vlo, start=False, stop=True)
        if GLA_D1 == "dve":
            kb.op("dve", "tensor_tensor", d1[:], pb[:], kT, ALU.mult)
        else:
            kb.op("act", "copy", d1[:], pb[:])
            kb.op("pool", "tensor_tensor", d1[:], d1[:], kT, ALU.mult)
        sc = S.at(sbase + v)[:, sbase + v:sbase + v + 1]
        kb.op("dve", "tensor_tensor_scan", st[:], decT, d1[:], sc, ALU.mult, ALU.add)
        kb.op("act", "copy", sc, st[:, TS - 1:TS])
        pe_ = GLA_PQ if GLA_PQ in ("pool", "dve") else ("pool" if cnt[0] % 2 else "dve")
        kb.op(pe_, "tensor_tensor", pq[:], st[:], qT, ALU.mult)
        kb.op("pe", "matmul", po, C["colsel"][:, porow0 + v, :], pq[:],
              start=(first and v == 0), stop=(last and v == nv - 1))


def emit_hgrn_M(kb, C, L, lidx, hfullD, modD, ngD, wslabD, lbrawD, hggD, oTD):
    nseg = L // TS
    make_sel(kb, C)
    with kb.scope():
        lbr = kb.sbuf("lbr", [128, 4, 4], F32)
        lbe = kb.sbuf("lbe", [128, 4, 4], F32)
        lbs = kb.sbuf("lbs", [128, 4], F32)
        lb = kb.sbuf("lb", [128, 4], F32)
        oml = kb.sbuf("oml", [128, 4], F32)
        kb.dma("sp", lbr[:], lbrawD[:])
        kb.op("dve", "reduce_max", lbs[:], lbr[:], AX.X)
        kb.op("dve", "tensor_tensor", lbe[:], lbr[:], bc(lbs, (slice(None), slice(None)), [128, 4]) if False else
              V(lbs.h[:, :].unsqueeze(2).to_broadcast([128, 4, 4]), lbs.units), ALU.subtract)
        kb.op("act", "activation", lbe[:], lbe[:], AF.Exp)
        kb.op("dve", "reduce_sum", lbs[:], lbe[:], AX.X)
        kb.op("dve", "reciprocal", lbs[:], lbs[:])
        kb.op("dve", "reduce_sum", lb[:], lbe[:, :, 1:lidx + 1], AX.X)
        kb.op("dve", "tensor_tensor", lb[:], lb[:], lbs[:], ALU.mult)
        kb.op("dve", "tensor_scalar", oml[:], lb[:], -1.0, 1.0, ALU.mult, ALU.add)
        hgg = kb.sbuf("hgg", [128, 1], F32)
        kb.dma("sp", hgg[:], hggD[:])
        S = kb.sbuf("Sst", [128, 512], F32, nunits=512)
        kb.op("dve", "memset", S.all(), 0.0)
        hnT = kb.sbuf("hnT_m", [128, 16, TS], BF16)
        pss = [kb.psum(f"psA{i}", [128, 512], F32) for i in range(2)]
        pbs = [kb.psum(f"psB{i}", [128, 512], F32) for i in range(4)]
        po = kb.psum("psO", [128, 512], F32)
        pn = kb.psum("psN", [128, 512], F32)
        cnt = [0]
        pc = 0
        for seg in range(nseg):
            emit_norm_segment(kb, C, hfullD, seg, modD, (0, D, 2 * D), ngD, hnT, pss)
            with kb.scope():
                wsl = [kb.sbuf(f"wsl{i}", [128, 16, 128], BF16) for i in range(4)]
                decT = kb.sbuf("decT", [128, 4, TS], F32)
                kkT = kb.sbuf("kkT", [128, 4, TS], F32)
                qT = kb.sbuf("qT", [128, 4, TS], F32)
                ogs = kb.sbuf("ogs", [128, 4, TS], F32)
                ihi = kb.sbuf("ihi", [128, 4, TS], BF16, nunits=4)
                ilo = kb.sbuf("ilo", [128, 4, TS], BF16, nunits=4)
                tmps = [(kb.sbuf(f"d1_{i}", [128, TS], F32), kb.sbuf(f"st_{i}", [128, TS], F32),
                         kb.sbuf(f"pq_{i}", [128, TS], BF16)) for i in range(4)]
                sq = kb.sbuf("sq", [128, TS], F32)
                osb = kb.sbuf("osb", [128, TS], F32)
                rs = kb.sbuf("rs", [128, TS], F32)
                res = kb.sbuf("res", [128, TS], F32)
                for hh in range(4):
                    for ty in range(4):
                        p = pss[pc % 2]
                        w = wsl[pc % 4]
                        pc += 1
                        emit_proj_chunk(kb, w, wslabD[hh * 4 + ty], hnT, p)
                        if ty == 0:
                            kb.op("act", "activation", qT[:, hh, :], p[:], AF.Silu)
                        elif ty == 1:
                            kb.op("act", "activation", decT[:, hh, :], p[:], AF.Sigmoid)
                            kb.op("dve", "tensor_scalar", decT[:, hh, :], decT[:, hh, :], oml[:, hh:hh + 1], lb[:, hh:hh + 1],
                                  ALU.mult, ALU.add)
                            kb.op("dve", "tensor_scalar", kkT[:, hh, :], decT[:, hh, :], -1.0, 1.0, ALU.mult, ALU.add)
                        elif ty == 2:
                            kb.op("act", "copy", ihi.at(hh)[:, hh, :], p[:])
                            kb.op("dve", "tensor_tensor", ilo.at(hh)[:, hh, :], p[:], ihi.at(hh)[:, hh, :], ALU.subtract)
                        else:
                            kb.op("act", "activation", ogs[:, hh, :], p[:], AF.Silu)
                for hh in range(4):
                    emit_gla(kb, C, decT[:, hh, :], kkT[:, hh, :], qT[:, hh, :], ihi.at(hh)[:, hh, :], ilo.at(hh)[:, hh, :],
                             0, 128, S, hh * 128, po[:], 0, True, True, pbs, tmps, cnt)
                    kb.op("act", "activation", sq[:], po[:], AF.Square)
                    kb.op("dve", "tensor_copy", osb[:], po[:])
                    kb.op("pe", "matmul", pn[:], C["ones"][:], sq[:], start=True, stop=True)
                    kb.op("dve", "tensor_scalar", rs[:], pn[:], 1.0 / 128, RMS_EPS, ALU.mult, ALU.add)
                    kb.op("act", "activation", rs[:], rs[:], AF.Sqrt)
                    kb.op("dve", "reciprocal", rs[:], rs[:])
                    kb.op("dve", "tensor_tensor", res[:], osb[:], rs[:], ALU.mult)
                    kb.op("dve", "scalar_tensor_tensor", res[:], res[:], hgg[:, 0:1], ogs[:, hh, :], ALU.mult, ALU.mult)
                    kb.dma("sp", oTD[hh * 128:(hh + 1) * 128, seg * TS:(seg + 1) * TS], res[:])


def emit_outproj(kb, NT, hinD, hmidD, modD, g1off, srcs, woutD, nkc):
    with kb.scope():
        woutb = kb.sbuf("woutb", [128, nkc, D], BF16)
        wv = woutD.h.rearrange("(kc p) n -> p kc n", p=128)
        for k0 in range(0, nkc, 8):
            kb.dma("pool", woutb[:, k0:k0 + 8, :], V(wv[:, k0:k0 + 8, :], woutD.units))
        g1 = kb.sbuf("g1t", [128, D], F32)
        load_bc(kb, g1, modD, modD.h[0:1, g1off:g1off + D])
        mixs = [kb.sbuf(f"mixT{i}", [128, nkc, 512], BF16) for i in range(2 if nkc <= 16 else 1)]
        hts = [kb.sbuf(f"hto{i}", [128, D], F32) for i in range(2)]
        tmp = kb.sbuf("tmpo", [128, 512], F32)
        pss = [kb.psum(f"pso{i}", [128, 4, 512], F32, nunits=4) for i in range(2)]
        it = 0
        for tb in range(NT // 512):
            mx = mixs[tb % len(mixs)]
            k0 = 0
            for (sD, nch) in srcs:
                sv = sD.h.rearrange("(kc p) t -> p kc t", p=128)
                kb.dma("pool" if sD.h.dtype == F32 else "sp", mx[:, k0:k0 + nch, :], V(sv[:, :, tb * 512:(tb + 1) * 512], sD.units))
                k0 += nch
            for tt in range(4):
                gt = tb * 4 + tt
                ps = pss[it % 2]
                ht = hts[it % 2]
                it += 1
                kb.dma("sp", ht[:], hinD[gt * 128:(gt + 1) * 128, :])
                for db in range(4):
                    for kc in range(nkc):
                        kb.op("pe", "matmul", ps.at(db)[:, db, :], mx[:, kc, tt * 128:(tt + 1) * 128],
                              woutb[:, kc, db * 512:(db + 1) * 512], start=(kc == 0), stop=(kc == nkc - 1))
                for db in range(4):
                    sl = slice(db * 512, (db + 1) * 512)
                    kb.op("dve", "tensor_tensor", tmp[:], ps.at(db)[:, db, :], g1[:, sl], ALU.mult)
                    kb.op("dve", "tensor_tensor", ht[:, sl], ht[:, sl], tmp[:], ALU.add)
                kb.dma("sp", hmidD[gt * 128:(gt + 1) * 128, :], ht[:])


TWO_PI = 6.28318


def emit_ab_M(kb, C, L, hfullD, modD, ngD, wslabD, P, yaTD, ybTD):
    nseg = L // TS
    make_sel(kb, C)
    tabD = kb.dram("s5tab", [16, 128, 2, TS], F32)
    with kb.scope():
        ident = C["ident"]
        hnT = kb.sbuf("hnT_m", [128, 16, TS], BF16)
        S = kb.sbuf("Sst", [128, 512], F32, nunits=512)
        kb.op("dve", "memset", S.all(), 0.0)
        xpre = kb.sbuf("xpre", [128, 6, TS + 3], F32)
        kb.op("dve", "memset", xpre[:], 0.0)
        sprev = kb.sbuf("sprev", [128, 16, 2], F32)
        kb.op("dve", "memset", sprev[:], 0.0)
        convw = kb.sbuf("convw", [128, 6, 4], F32)
        convb = kb.sbuf("convb", [128, 6], F32)
        dtb = kb.sbuf("dtb", [8, 1], F32)
        negA = kb.sbuf("negA", [8, 1], F32)
        ssdD = kb.sbuf("ssdD", [128, 4], F32)
        normg = kb.sbuf("normg", [128, 4], F32)
        s5d = kb.sbuf("s5d", [32, 16], F32)
        for t_, n_ in ((convw, "convw"), (convb, "convb"), (dtb, "dtb"), (negA, "alog"), (ssdD, "ssdD"), (normg, "normg"), (s5d, "s5d")):
            kb.dma("sp", t_[:], P[n_][:])
        kb.op("act", "activation", negA[:], negA[:], AF.Exp)
        kb.op("dve", "tensor_scalar", negA[:], negA[:], -1.0, None, ALU.mult)
        sel8 = kb.sbuf("sel8", [8, 8, 128], F32)
        sel8b = kb.sbuf("sel8b", [8, 4, 128], F32)
        kb.op("dve", "tensor_copy", sel8[:], V(ident.h[0:8, 0:8].unsqueeze(2).to_broadcast([8, 8, 128]), ident.units))
        for c in range(4):
            kb.op("dve", "tensor_copy", sel8b[:, c, 0:64], sel8[:, 2 * c, 0:64])
            kb.op("dve", "tensor_copy", sel8b[:, c, 64:128], sel8[:, 2 * c + 1, 64:128])
        wz = kb.sbuf("wz", [128, 16, 512], BF16)
        wdt = kb.sbuf("wdt", [128, 16, 8], BF16)
        kb.dma("pool", wz[:], P["wz"][:])
        kb.dma("pool", wdt[:], P["wdt"][:])
        dtbB = kb.sbuf("dtbB", [128, 8], F32)
        negAB = kb.sbuf("negAB", [128, 8], F32)
        DB = kb.sbuf("DB", [128, 512], F32)
        NB = kb.sbuf("NB", [128, 512], F32)
        load_bc(kb, dtbB, P["dtbr"], P["dtbr"].h[0:1, :])
        load_bc(kb, negAB, P["alogr"], P["alogr"].h[0:1, :])
        load_bc(kb, DB, P["ssdDr"], P["ssdDr"].h[0:1, :])
        load_bc(kb, NB, P["normgr"], P["normgr"].h[0:1, :])
        kb.op("act", "activation", negAB[:], negAB[:], AF.Exp)
        kb.op("dve", "tensor_scalar", negAB[:], negAB[:], -1.0, None, ALU.mult)
        mle = kb.sbuf("mle", [128, 128], F32)
        ugt = kb.sbuf("ugt", [128, 128], F32)
        nmask = kb.sbuf("nmask", [128, 128], F32)
        kb.op("dve", "tensor_single_scalar", mle[:], C["jmp"][:], 0.0, ALU.is_ge)
        kb.op("dve", "tensor_single_scalar", ugt[:], C["jmp"][:], 0.0, ALU.is_lt)
        kb.op("dve", "tensor_scalar", nmask[:], ugt[:], -1.0e4, None, ALU.mult)
        Sf = kb.sbuf("Sf", [128, 512], F32)
        Sb = kb.sbuf("Sb", [128, 512], BF16)
        kb.op("dve", "memset", Sf[:], 0.0)
        kb.op("dve", "memset", Sb[:], 0.0)
        BbTr = kb.sbuf("BbTr", [32, 16, 128], BF16)
        BbTi = kb.sbuf("BbTi", [32, 16, 128], BF16)
        Creb = kb.sbuf("Creb", [128, 16, 32], BF16)
        nCimb = kb.sbuf("nCimb", [128, 16, 32], BF16)
        rho = kb.sbuf("rho", [128, 16], F32)
        cth = kb.sbuf("cth", [128, 16], F32)
        sth = kb.sbuf("sth", [128, 16], F32)
        pss = [kb.psum(f"psA{i}", [128, 512], F32) for i in range(2)]
        pbs = [kb.psum(f"psB{i}", [128, 512], F32) for i in range(2)]
        po = kb.psum("psO", [128, 512], F32)
        pn = kb.psum("psN", [128, 512], F32)
        with kb.scope():
            lre = kb.sbuf("lre", [128, 16], F32)
            lim = kb.sbuf("lim", [128, 16], F32)
            stp = kb.sbuf("stp", [128, 16], F32)
            thr = kb.sbuf("thr", [128, 16], F32)
            kb.dma("sp", lre[:], P["lre"][:])
            kb.dma("sp", lim[:], P["lim"][:])
            kb.dma("sp", stp[:], P["lstep"][:])
            kb.op("act", "activation", stp[:], stp[:], AF.Exp)
            kb.op("dve", "tensor_tensor", rho[:], lre[:], stp[:], ALU.mult)
            kb.op("act", "activation", rho[:], rho[:], AF.Exp)
            kb.op("dve", "tensor_tensor", thr[:], lim[:], stp[:], ALU.mult)
            kb.op("dve", "tensor_scalar", thr[:], thr[:], float(1.0 / (2 * np.pi)), None, ALU.mult)
            ioi = kb.sbuf("ioi", [128, TS], I32)
            iot = kb.sbuf("iot", [128, TS], F32)
            kb.op("pool", "iota", ioi[:], [[1, TS]], base=0, channel_multiplier=0)
            kb.op("dve", "tensor_copy", iot[:], ioi[:])
            ur = kb.sbuf("ur", [128, TS], F32)
            ki = kb.sbuf("ki", [128, TS], I32)
            kf = kb.sbuf("kf", [128, TS], F32)
            fr = kb.sbuf("fr", [128, TS], F32)
            mk = kb.sbuf("mk", [128, TS], F32)
            tabs = [kb.sbuf(f"tab{i}", [128, 2, TS], F32) for i in range(2)]
            for s in range(16):
                tb_ = tabs[s % 2]
                kb.op("dve", "tensor_scalar", ur[:], iot[:], thr[:, s:s + 1], None, ALU.mult)
                kb.op("dve", "tensor_copy", ki[:], ur[:])
                kb.op("dve", "tensor_copy", kf[:], ki[:])
                kb.op("dve", "tensor_tensor", fr[:], ur[:], kf[:], ALU.subtract)
                kb.op("act", "activation", tb_[:, 1, :], fr[:], AF.Sin, scale=TWO_PI)
                kb.op("dve", "tensor_scalar", fr[:], fr[:], 0.25, None, ALU.add)
                kb.op("dve", "tensor_single_scalar", mk[:], fr[:], 0.5, ALU.is_gt)
                kb.op("dve", "tensor_tensor", fr[:], fr[:], mk[:], ALU.subtract)
                kb.op("act", "activation", tb_[:, 0, :], fr[:], AF.Sin, scale=TWO_PI)
                kb.op("dve", "tensor_copy", cth[:, s:s + 1], tb_[:, 0, 1:2])
                kb.op("dve", "tensor_copy", sth[:, s:s + 1], tb_[:, 1, 1:2])
                kb.dma("sp", tabD[s], tb_[:])
            lbr = kb.sbuf("lbr_", [128, 16], F32)
            lbi = kb.sbuf("lbi_", [128, 16], F32)
            den = kb.sbuf("den", [128, 16], F32)
            t1 = kb.sbuf("t1_", [128, 16], F32)
            gre = kb.sbuf("gre", [128, 16], F32)
            gim = kb.sbuf("gim", [128, 16], F32)
            kb.op("dve", "tensor_tensor", lbr[:], rho[:], cth[:], ALU.mult)
            kb.op("dve", "tensor_scalar", lbr[:], lbr[:], -1.0, None, ALU.add)
            kb.op("dve", "tensor_tensor", lbi[:], rho[:], sth[:], ALU.mult)
            kb.op("dve", "tensor_tensor", den[:], lre[:], lre[:], ALU.mult)
            kb.op("dve", "tensor_tensor", t1[:], lim[:], lim[:], ALU.mult)
            kb.op("dve", "tensor_tensor", den[:], den[:], t1[:], ALU.add)
            kb.op("dve", "reciprocal", den[:], den[:])
            kb.op("dve", "tensor_tensor", gre[:], lbr[:], lre[:], ALU.mult)
            kb.op("dve", "tensor_tensor", t1[:], lbi[:], lim[:], ALU.mult)
            kb.op("dve", "tensor_tensor", gre[:], gre[:], t1[:], ALU.add)
            kb.op("dve", "tensor_tensor", gre[:], gre[:], den[:], ALU.mult)
            kb.op("dve", "tensor_tensor", gim[:], lbi[:], lre[:], ALU.mult)
            kb.op("dve", "tensor_tensor", t1[:], lbr[:], lim[:], ALU.mult)
            kb.op("dve", "tensor_tensor", gim[:], gim[:], t1[:], ALU.subtract)
            kb.op("dve", "tensor_tensor", gim[:], gim[:], den[:], ALU.mult)
            bre = kb.sbuf("bre", [128, 16, 32], F32)
            bim = kb.sbuf("bim", [128, 16, 32], F32)
            bbr = kb.sbuf("bbr", [128, 16, 32], F32)
            bbi = kb.sbuf("bbi", [128, 16, 32], F32)
            tt_ = kb.sbuf("tt_", [128, 16, 32], F32)
            kb.dma("sp", bre[:], P["bre"][:])
            kb.dma("sp", bim[:], P["bim"][:])
            greb = V(gre.h[:, :].unsqueeze(2).to_broadcast([128, 16, 32]), gre.units)
            gimb = V(gim.h[:, :].unsqueeze(2).to_broadcast([128, 16, 32]), gim.units)
            kb.op("dve", "tensor_tensor", bbr[:], bre[:], greb, ALU.mult)
            kb.op("dve", "tensor_tensor", tt_[:], bim[:], gimb, ALU.mult)
            kb.op("dve", "tensor_tensor", bbr[:], bbr[:], tt_[:], ALU.subtract)
            kb.op("dve", "tensor_tensor", bbi[:], bim[:], greb, ALU.mult)
            kb.op("dve", "tensor_tensor", tt_[:], bre[:], gimb, ALU.mult)
            kb.op("dve", "tensor_tensor", bbi[:], bbi[:], tt_[:], ALU.add)
            for s in range(16):
                for (src, dst) in ((bbr, BbTr), (bbi, BbTi)):
                    p = pss[s % 2]
                    kb.op("pe", "transpose", p[0:32, 0:128], src[:, s, :], ident[:])
                    kb.op("act", "copy", dst[:, s, :], p[0:32, 0:128])
            kb.dma("sp", bre[:], P["cre"][:])
            kb.dma("sp", bim[:], P["cim"][:])
            kb.op("dve", "tensor_copy", Creb[:], bre[:])
            kb.op("dve", "tensor_scalar", nCimb[:], bim[:], -1.0, None, ALU.mult)
        cnt = [0]
        pc = 0
        for seg in range(nseg):
            cs = slice(seg * TS, (seg + 1) * TS)
            emit_norm_segment(kb, C, hfullD, seg, modD, (0, D, 2 * D), ngD, hnT, pss)
            with kb.scope():
                wsl = [kb.sbuf(f"wsl{i}", [128, 16, 128], BF16) for i in range(2)]
                xc = kb.sbuf("xc", [128, 6, TS], F32)
                xcb = kb.sbuf("xcb", [128, 2, TS], BF16)
                ycv = kb.sbuf("ycv", [128, TS], F32)
                zs = kb.sbuf("zs", [128, 512], F32)
                dtt = kb.sbuf("dtt", [128, 8], F32)
                aa_ = kb.sbuf("a_", [128, 8], F32)
                acum = kb.sbuf("acum", [128, 8], F32)
                eacum = kb.sbuf("eacum", [128, 8], F32)
                wend = kb.sbuf("wend", [128, 8], F32)
                eatot = kb.sbuf("eatot", [128, 8], F32)
                xtm = kb.sbuf("xtm", [128, 8, 64], F32)
                xdtb = kb.sbuf("xdtb", [128, 8, 64], BF16)
                xwb = kb.sbuf("xwb", [128, 8, 64], BF16)
                btm = kb.sbuf("btm", [128, 128], BF16)
                Gs = kb.sbuf("Gs", [128, 128], F32)
                am = kb.sbuf("am", [128, 8, 128], F32)
                Ee = kb.sbuf("Ee", [128, 8, 128], F32)
                Wb = kb.sbuf("Wb", [128, 8, 128], BF16)
                yt = kb.sbuf("yt", [128, 8, 64], F32)
                vv = kb.sbuf("vv", [128, 512], F32)
                ssn = kb.sbuf("ssn", [128, 4], F32)
                ob = [kb.sbuf(f"ob{i}", [128, 512], F32) for i in range(2)]
                for c in range(6):
                    p = pss[pc % 2]; w = wsl[pc % 2]; pc += 1
                    emit_proj_chunk(kb, w, wslabD[4 + c], hnT, p)
                    kb.op("act", "copy", xpre[:, c, 3:TS + 3], p[:])
                    kb.op("dve", "tensor_scalar", ycv[:], xpre[:, c, 0:TS], convw[:, c, 0:1], None, ALU.mult)
                    for k in range(1, 4):
                        kb.op("dve", "scalar_tensor_tensor", ycv[:], xpre[:, c, k:k + TS], convw[:, c, k:k + 1], ycv[:], ALU.mult, ALU.add)
                    kb.op("act", "activation", xc[:, c, :], ycv[:], AF.Silu, bias=convb[:, c:c + 1])
                    kb.op("dve", "tensor_copy", xpre[:, c, 0:3], xpre[:, c, TS:TS + 3])
                kb.op("act", "copy", xcb[:], xc[:, 4:6, :])
                psm, pxy, pbg, pS_ = pn, po, pbs[0], pbs[1]
                pdf = kb.psum("pdf", [128, 8, 128], F32)
                for ck in range(4):
                    cc_ = slice(ck * 128, (ck + 1) * 128)
                    r0 = seg * TS + ck * 128
                    p = pss[pc % 2]; pc += 1
                    for kc in range(16):
                        kb.op("pe", "matmul", p[:], hnT[:, kc, cc_], wz[:, kc, :], start=(kc == 0), stop=(kc == 15))
                    kb.op("act", "activation", zs[:], p[:], AF.Silu)
                    for kc in range(16):
                        kb.op("pe", "matmul", psm[:, 0:8], hnT[:, kc, cc_], wdt[:, kc, :], start=(kc == 0), stop=(kc == 15))
                    kb.op("dve", "tensor_tensor", dtt[:], psm[:, 0:8], dtbB[:], ALU.add)
                    kb.op("act", "activation", dtt[:], dtt[:], AF.Exp)
                    kb.op("act", "activation", dtt[:], dtt[:], AF.Ln, bias=1.0)
                    kb.op("dve", "tensor_tensor", aa_[:], dtt[:], negAB[:], ALU.mult)
                    kb.op("pe", "matmul", psm[:, 8:16], mle[:], aa_[:], start=True, stop=True)
                    kb.op("pe", "matmul", psm[:, 16:24], C["ones"][:], aa_[:], start=True, stop=True)
                    kb.op("dve", "tensor_copy", acum[:], psm[:, 8:16])
                    kb.op("act", "activation", eacum[:], acum[:], AF.Exp)
                    kb.op("dve", "tensor_tensor", wend[:], psm[:, 16:24], acum[:], ALU.subtract)
                    kb.op("act", "activation", wend[:], wend[:], AF.Exp)
                    kb.op("act", "activation", eatot[:], psm[:, 16:24], AF.Exp)
                    kb.op("dve", "tensor_tensor", wend[:], wend[:], dtt[:], ALU.mult)
                    for c in range(4):
                        kb.op("pe", "transpose", pxy[:, c * 128:(c + 1) * 128], xc[:, c, cc_], ident[:])
                    kb.op("act", "copy", V(xtm.h.rearrange("p a b -> p (a b)"), xtm.units), pxy[:])
                    kb.op("dve", "tensor_tensor", xdtb[:], xtm[:], V(dtt.h[:, :].unsqueeze(2).to_broadcast([128, 8, 64]), dtt.units), ALU.mult)
                    kb.op("dve", "tensor_tensor", xwb[:], xtm[:], V(wend.h[:, :].unsqueeze(2).to_broadcast([128, 8, 64]), wend.units), ALU.mult)
                    kb.op("pe", "transpose", pbg[:, 0:128], xc[:, 4, cc_], ident[:])
                    kb.op("act", "copy", btm[:], pbg[:, 0:128])
                    kb.op("pe", "matmul", pbg[:, 128:256], xcb[:, 0, cc_], xcb[:, 1, cc_], start=True, stop=True)
                    kb.op("act", "copy", Gs[:], pbg[:, 128:256])
                    kb.op("dve", "tensor_tensor", am[:], V(aa_.h[:, :].unsqueeze(2).to_broadcast([128, 8, 128]), aa_.units),
                          V(mle.h[:, :].unsqueeze(1).to_broadcast([128, 8, 128]), mle.units), ALU.mult)
                    for hb in range(2):
                        kb.op("pe", "matmul", V(pdf.h[:, hb * 4:(hb + 1) * 4, :], pdf.units), ugt[:], am[:, hb * 4:(hb + 1) * 4, :], start=True, stop=True)
                    for hb in range(2):
                        kb.op("dve", "tensor_tensor", Ee[:, hb * 4:(hb + 1) * 4, :], V(pdf.h[:, hb * 4:(hb + 1) * 4, :], pdf.units),
                              V(nmask.h[:, :].unsqueeze(1).to_broadcast([128, 4, 128]), nmask.units), ALU.add)
                    kb.op("act", "activation", Ee[:], Ee[:], AF.Exp)
                    kb.op("dve", "tensor_tensor", Wb[:], Ee[:], V(Gs.h[:, :].unsqueeze(1).to_broadcast([128, 8, 128]), Gs.units), ALU.mult)
                    for hh in range(8):
                        kb.op("pe", "matmul", pxy[:, hh * 64:(hh + 1) * 64], Wb[:, hh, :], xdtb[:, hh, :], start=True, stop=True)
                    kb.op("pe", "matmul", pbg[:], xcb[:, 1, cc_], Sb[:], start=True, stop=True)
                    kb.op("pe", "matmul", pS_[:], btm[:], V(xwb.h.rearrange("p a b -> p (a b)"), xwb.units), start=True, stop=True)
                    kb.op("dve", "tensor_tensor", yt[:], V(pbg.h[:, :].rearrange("p (a b) -> p a b", b=64), pbg.units),
                          V(eacum.h[:, :].unsqueeze(2).to_broadcast([128, 8, 64]), eacum.units), ALU.mult)
                    ytf = V(yt.h.rearrange("p a b -> p (a b)"), yt.units)
                    kb.op("dve", "tensor_tensor", ytf, ytf, pxy[:], ALU.add)
                    xtf = V(xtm.h.rearrange("p a b -> p (a b)"), xtm.units)
                    kb.op("dve", "tensor_tensor", vv[:], xtf, DB[:], ALU.mult)
                    kb.op("dve", "tensor_tensor", vv[:], vv[:], ytf, ALU.add)
                    S3 = V(Sf.h[:, :].rearrange("p (a b) -> p a b", b=64), Sf.units)
                    kb.op("dve", "tensor_tensor", S3, S3, V(eatot.h[:, :].unsqueeze(2).to_broadcast([128, 8, 64]), eatot.units), ALU.mult)
                    kb.op("dve", "tensor_tensor", Sf[:], Sf[:], pS_[:], ALU.add)
                    kb.op("act", "copy", Sb[:], Sf[:])
                    kb.op("dve", "tensor_tensor", vv[:], vv[:], zs[:], ALU.mult)
                    o_ = ob[ck % 2]
                    kb.op("act", "activation", o_[:], vv[:], AF.Square, accum_out=ssn[:, 0:1])
                    kb.op("dve", "tensor_scalar", ssn[:, 1:2], ssn[:, 0:1], 1.0 / 512, 1e-5, ALU.mult, ALU.add)
                    kb.op("act", "activation", ssn[:, 2:3], ssn[:, 1:2], AF.Sqrt)
                    kb.op("dve", "reciprocal", ssn[:, 3:4], ssn[:, 2:3])
                    kb.op("dve", "scalar_tensor_tensor", o_[:], vv[:], ssn[:, 3:4], NB[:], ALU.mult, ALU.mult)
                    kb.dma("sp", yaTD[r0:r0 + 128, :], o_[:])
            with kb.scope():
                wsl = [kb.sbuf(f"wsl{i}", [128, 16, 128], BF16) for i in range(4)]
                tabs = [kb.sbuf(f"tabl{i}", [128, 2, TS], F32) for i in range(2)]
                uf = [kb.sbuf(f"uf{i}", [32, TS], F32) for i in range(2)]
                ub = [kb.sbuf(f"ub{i}", [32, TS], BF16) for i in range(2)]
                bur = kb.sbuf("bur", [128, TS], F32)
                bui = kb.sbuf("bui", [128, TS], F32)
                m1 = kb.sbuf("m1", [128, TS], F32)
                m2 = kb.sbuf("m2", [128, TS], F32)
                aa = kb.sbuf("aa", [128, TS], F32)
                bb = kb.sbuf("bb", [128, TS], F32)
                rhoB = kb.sbuf("rhoB", [128, TS], F32)
                onesT = kb.sbuf("onesT", [128, TS], F32)
                kb.op("pool", "memset", onesT[:], 1.0)
                wre = kb.sbuf("wre", [128, TS], F32)
                wim = kb.sbuf("wim", [128, TS], F32)
                sre = kb.sbuf("sre", [128, TS], F32)
                sim = kb.sbuf("sim", [128, TS], F32)
                sreb = kb.sbuf("sreb", [128, TS], BF16)
                simb = kb.sbuf("simb", [128, TS], BF16)
                ini = kb.sbuf("ini", [128, 4], F32)
                yb = [kb.sbuf(f"yb{i}", [32, TS], F32) for i in range(2)]
                for s in range(16):
                    p = pss[pc % 2]; w = wsl[pc % 4]; pc += 1
                    tb_ = tabs[s % 2]
                    ct_, st_ = tb_[:, 0, :], tb_[:, 1, :]
                    kb.dma("sp", tb_[:], tabD[s])
                    emit_proj_chunk(kb, w, wslabD[11 + s], hnT, p, mcols=32)
                    kb.op("act", "copy", uf[s % 2][:], p[0:32, :])
                    kb.op("dve", "tensor_copy", ub[s % 2][:], p[0:32, :])
                    kb.op("pe", "matmul", pbs[0][:], BbTr[:, s, :], ub[s % 2][:], start=True, stop=True)
                    kb.op("pe", "matmul", pbs[1][:], BbTi[:, s, :], ub[s % 2][:], start=True, stop=True)
                    kb.op("dve", "tensor_tensor", bur[:], pbs[0][:], ct_, ALU.mult)
                    kb.op("dve", "tensor_tensor", bui[:], pbs[1][:], st_, ALU.mult)
                    kb.op("dve", "tensor_tensor", aa[:], bur[:], bui[:], ALU.add)
                    kb.op("dve", "tensor_tensor", bur[:], pbs[1][:], ct_, ALU.mult)
                    kb.op("dve", "tensor_tensor", bui[:], pbs[0][:], st_, ALU.mult)
                    kb.op("dve", "tensor_tensor", bb[:], bur[:], bui[:], ALU.subtract)
                    kb.op("pool", "tensor_scalar", rhoB[:], onesT[:], rho[:, s:s + 1], None, ALU.mult)
                    kb.op("dve", "tensor_tensor", ini[:, 2:3], sprev[:, s, 1:2], sth[:, s:s + 1], ALU.mult)
                    kb.op("dve", "scalar_tensor_tensor", ini[:, 0:1], sprev[:, s, 0:1], cth[:, s:s + 1], ini[:, 2:3], ALU.mult, ALU.subtract)
                    kb.op("dve", "tensor_tensor", ini[:, 3:4], sprev[:, s, 1:2], cth[:, s:s + 1], ALU.mult)
                    kb.op("dve", "scalar_tensor_tensor", ini[:, 1:2], sprev[:, s, 0:1], sth[:, s:s + 1], ini[:, 3:4], ALU.mult, ALU.add)
                    kb.op("dve", "tensor_tensor_scan", wre[:], rhoB[:], aa[:], ini[:, 0:1], ALU.mult, ALU.add)
                    kb.op("dve", "tensor_tensor_scan", wim[:], rhoB[:], bb[:], ini[:, 1:2], ALU.mult, ALU.add)
                    kb.op("pool", "tensor_tensor", m1[:], wre[:], ct_, ALU.mult)
                    kb.op("pool", "tensor_tensor", m2[:], wim[:], st_, ALU.mult)
                    kb.op("pool", "tensor_tensor", sre[:], m1[:], m2[:], ALU.subtract)
                    kb.op("pool", "tensor_tensor", m1[:], wre[:], st_, ALU.mult)
                    kb.op("pool", "tensor_tensor", m2[:], wim[:], ct_, ALU.mult)
                    kb.op("pool", "tensor_tensor", sim[:], m1[:], m2[:], ALU.add)
                    kb.op("act", "copy", sprev[:, s, 0:1], sre[:, TS - 1:TS])
                    kb.op("act", "copy", sprev[:, s, 1:2], sim[:, TS - 1:TS])
                    kb.op("act", "copy", sreb[:], sre[:])
                    kb.op("act", "copy", simb[:], sim[:])
                    kb.op("pe", "matmul", po[0:32, :], Creb[:, s, :], sreb[:], start=True, stop=False)
                    kb.op("pe", "matmul", po[0:32, :], nCimb[:, s, :], simb[:], start=False, stop=True)
                    kb.op("dve", "scalar_tensor_tensor", yb[s % 2][:], uf[s % 2][:], s5d[:, s:s + 1], po[0:32, :], ALU.mult, ALU.add)
                    kb.dma("sp", ybTD[s * 32:(s + 1) * 32, cs], yb[s % 2][:])


def emit_glu(kb, NT, ybTD, gluwD, glubD, ybfD):
    with kb.scope():
        gw = kb.sbuf("gluw", [128, 16, D], BF16)
        wv = gluwD.h.rearrange("(kc p) n -> p kc n", p=128)
        for k0 in range(0, 16, 8):
            kb.dma("pool", gw[:, k0:k0 + 8, :], V(wv[:, k0:k0 + 8, :], gluwD.units))
        gb = kb.sbuf("glub", [128, 16], F32)
        kb.dma("sp", gb[:], glubD[:])
        yb = kb.sbuf("ybl", [128, 16, 512], F32)
        glT = kb.sbuf("glT", [128, 16, 512], BF16)
        obf = kb.sbuf("obf", [128, 16, 512], BF16)
        t1 = [kb.sbuf(f"gt1_{i}", [128, 512], F32) for i in range(2)]
        t2 = [kb.sbuf(f"gt2_{i}", [128, 512], F32) for i in range(2)]
        pss = [kb.psum(f"psg{i}", [128, 512], F32) for i in range(2)]
        yv = ybTD.h.rearrange("(kc p) t -> p kc t", p=128)
        ov = ybfD.h.rearrange("(kc p) t -> p kc t", p=128)
        for tb in range(NT // 512):
            cs = slice(tb * 512, (tb + 1) * 512)
            kb.dma("sp", yb[:], V(yv[:, :, cs], ybTD.units))
            for kc in range(16):
                a, b = t1[kc % 2], t2[kc % 2]
                kb.op("act", "activation", a[:], yb[:, kc, :], AF.Square)
                kb.op("dve", "tensor_scalar", a[:], a[:], 0.044715, 1.0, ALU.mult, ALU.add)
                kb.op("pool", "tensor_tensor", a[:], a[:], yb[:, kc, :], ALU.mult)
                kb.op("act", "activation", b[:], a[:], AF.Sigmoid, scale=1.5957691216057308)
                kb.op("pool", "tensor_tensor", glT[:, kc, :], b[:], yb[:, kc, :], ALU.mult)
            for m in range(16):
                p = pss[m % 2]
                a = t1[m % 2]
                for kc in range(16):
                    kb.op("pe", "matmul", p[:], gw[:, kc, m * 128:(m + 1) * 128], glT[:, kc, :], start=(kc == 0), stop=(kc == 15))
                kb.op("act", "activation", a[:], p[:], AF.Sigmoid, bias=gb[:, m:m + 1])
                kb.op("dve", "tensor_tensor", obf[:, m, :], a[:], yb[:, m, :], ALU.mult)
            kb.dma("sp", V(ov[:, :, cs], ybfD.units), obf[:])


def emit_hgrn_M2(kb, C, L, lidx, hfullD, modD, ngD, wslabD, wiD, wogD, lbrawD, hggrD, oD):
    nseg = L // TS
    ident, identb = C["ident"], C["identb"]
    with kb.scope():
        lbr = kb.sbuf("lbr", [128, 4, 4], F32)
        lbe = kb.sbuf("lbe", [128, 4, 4], F32)
        lbs = kb.sbuf("lbs", [128, 4], F32)
        lb = kb.sbuf("lb", [128, 4], F32)
        oml = kb.sbuf("oml", [128, 4], F32)
        kb.dma("sp", lbr[:], lbrawD[:])
        kb.op("dve", "reduce_max", lbs[:], lbr[:], AX.X)
        kb.op("dve", "tensor_tensor", lbe[:], lbr[:], V(lbs.h[:, :].unsqueeze(2).to_broadcast([128, 4, 4]), lbs.units), ALU.subtract)
        kb.op("act", "activation", lbe[:], lbe[:], AF.Exp)
        kb.op("dve", "reduce_sum", lbs[:], lbe[:], AX.X)
        kb.op("dve", "reciprocal", lbs[:], lbs[:])
        kb.op("dve", "reduce_sum", lb[:], lbe[:, :, 1:lidx + 1], AX.X)
        kb.op("dve", "tensor_tensor", lb[:], lb[:], lbs[:], ALU.mult)
        kb.op("dve", "tensor_scalar", oml[:], lb[:], -1.0, 1.0, ALU.mult, ALU.add)
        hgB = kb.sbuf("hgB", [128, 128], F32)
        load_bc(kb, hgB, hggrD, hggrD.h[0:1, :])
        wi = kb.sbuf("wi", [128, 16, 512], BF16)
        wog = kb.sbuf("wog", [128, 16, 512], BF16)
        kb.dma("pool", wi[:], wiD[:])
        kb.dma("pool", wog[:], wogD[:])
        Sf = kb.sbuf("Sf", [128, 4, 128], F32, nunits=4)
        Sb = kb.sbuf("Sb", [128, 4, 128], BF16, nunits=4)
        kb.op("dve", "memset", Sf.all(), 0.0)
        kb.op("dve", "memset", Sb.all(), 0.0)
        m64 = kb.sbuf("m64", [128, 128], F32)
        kb.op("dve", "tensor_single_scalar", m64[:], C["jmp"][:], 0.0, ALU.is_ge)
        kb.op("dve", "memset", m64[0:64, 64:128], 0.0)
        rmask = kb.sbuf("rmask", [128, 8, 64], F32)
        kb.op("dve", "memset", rmask[:], 1.0)
        kb.op("dve", "memset", rmask[:, :, 0:1], 0.0)
        rmf = V(rmask.h.rearrange("p a b -> p (a b)"), rmask.units)
        hnT = kb.sbuf("hnT_m", [128, 16, TS], BF16)
        pss = [kb.psum(f"psA{i}", [128, 512], F32) for i in range(2)]
        psc = [kb.psum(f"psC{i}", [128, 512], F32) for i in range(2)]
        pos = [kb.psum(f"psO{i}", [128, 512], F32) for i in range(2)]
        pkv = [kb.psum(f"psK{i}", [128, 512], F32) for i in range(2)]
        pc = 0
        it = 0
        for seg in range(nseg):
            emit_norm_segment(kb, C, hfullD, seg, modD, (0, D, 2 * D), ngD, hnT, pss)
            with kb.scope():
                wsl = [kb.sbuf(f"wsl{i}", [128, 16, 128], BF16) for i in range(2)]
                fg = kb.sbuf("fg", [128, 4, TS], F32)
                kkT = kb.sbuf("kkT", [128, 4, TS], F32)
                qT = kb.sbuf("qT", [128, 4, TS], F32)
                cum = kb.sbuf("cum", [128, 4, TS], F32)
                ex = kb.sbuf("exq", [128, 4, TS], F32)
                qdec = kb.sbuf("qdec", [128, 4, TS], BF16)
                kinv = kb.sbuf("kinv", [128, 4, TS], BF16)
                kend = kb.sbuf("kend", [128, 4, TS], BF16)
                elast = kb.sbuf("elast", [128, 4, 8], F32)
                vtm = kb.sbuf("vtm", [128, 4, 512], BF16)
                ogs = kb.sbuf("ogs", [128, 4, 512], F32)
                kendT = kb.sbuf("kendT", [128, 4, 512], BF16)
                STs = [kb.sbuf(f"ST{i}", [128, 128], BF16) for i in range(2)]
                osb = [kb.sbuf(f"osb{i}", [128, 128], F32) for i in range(2)]
                junk = kb.sbuf("junk", [128, 128], F32)
                ssn = [kb.sbuf(f"ssn{i}", [128, 4], F32) for i in range(2)]
                otile = [kb.sbuf(f"otile{i}", [128, 512], F32) for i in range(2)]
                for hh in range(4):
                    for ty in range(2):
                        p = pss[pc % 2]; w = wsl[pc % 2]; pc += 1
                        emit_proj_chunk(kb, w, wslabD[hh * 2 + ty], hnT, p)
                        if ty == 0:
                            kb.op("act", "activation", qT[:, hh, :], p[:], AF.Silu)
                        else:
                            kb.op("act", "activation", fg[:, hh, :], p[:], AF.Sigmoid)
                            kb.op("dve", "tensor_scalar", fg[:, hh, :], fg[:, hh, :], oml[:, hh:hh + 1], lb[:, hh:hh + 1], ALU.mult, ALU.add)
                            kb.op("dve", "tensor_scalar", kkT[:, hh, :], fg[:, hh, :], -1.0, 1.0, ALU.mult, ALU.add)
                            kb.op("act", "activation", fg[:, hh, :], fg[:, hh, :], AF.Ln)
                            kb.op("dve", "tensor_tensor_scan", cum[:, hh, :], rmf, fg[:, hh, :], 0.0, ALU.mult, ALU.add)
                    c3 = V(cum.h[:, hh, :].rearrange("p (a b) -> p a b", b=64), cum.units)
                    e3 = V(ex.h[:, hh, :].rearrange("p (a b) -> p a b", b=64), ex.units)
                    lastb = V(cum.h[:, hh, :].rearrange("p (a b) -> p a b", b=64)[:, :, 63:64].to_broadcast([128, 8, 64]), cum.units)
                    kb.op("act", "activation", ex[:, hh, :], cum[:, hh, :], AF.Exp)
                    kb.op("dve", "tensor_tensor", qdec[:, hh, :], qT[:, hh, :], ex[:, hh, :], ALU.mult)
                    kb.op("dve", "tensor_tensor", e3, lastb, c3, ALU.subtract)
                    kb.op("act", "activation", ex[:, hh, :], ex[:, hh, :], AF.Exp)
                    kb.op("dve", "tensor_tensor", kend[:, hh, :], kkT[:, hh, :], ex[:, hh, :], ALU.mult)
                    kb.op("dve", "tensor_scalar", ex[:, hh, :], cum[:, hh, :], -1.0, 80.0, ALU.mult, ALU.min)
                    kb.op("act", "activation", ex[:, hh, :], ex[:, hh, :], AF.Exp)
                    kb.op("dve", "tensor_tensor", kinv[:, hh, :], kkT[:, hh, :], ex[:, hh, :], ALU.mult)
                    kb.op("act", "activation", elast[:, hh, :], V(cum.h[:, hh, :].rearrange("p (a b) -> p a b", b=64)[:, :, 63], cum.units), AF.Exp)
                for tt in range(4):
                    cc_ = slice(tt * 128, (tt + 1) * 128)
                    p = pss[pc % 2]; pc += 1
                    for kc in range(16):
                        kb.op("pe", "matmul", p[:], hnT[:, kc, cc_], wi[:, kc, :], start=(kc == 0), stop=(kc == 15))
                    kb.op("act", "copy", vtm[:, tt, :], p[:])
                    p = pss[pc % 2]; pc += 1
                    for kc in range(16):
                        kb.op("pe", "matmul", p[:], hnT[:, kc, cc_], wog[:, kc, :], start=(kc == 0), stop=(kc == 15))
                    kb.op("act", "activation", ogs[:, tt, :], p[:], AF.Silu)
                    p = pss[pc % 2]; pc += 1
                    pT = V(p.h[:, 0:256].bitcast(BF16), p.units)
                    for hh in range(4):
                        kb.op("pe", "transpose", V(p.h[:, 0:256].bitcast(BF16)[:, hh * 128:(hh + 1) * 128], p.units),
                              kend[:, hh, cc_], identb[:])
                    kb.op("dve", "tensor_copy", kendT[:, tt, :], pT)
                for tt in range(4):
                    cc_ = slice(tt * 128, (tt + 1) * 128)
                    r0 = seg * TS + tt * 128
                    ot = otile[tt % 2]
                    for hh in range(4):
                        hs = slice(hh * 128, (hh + 1) * 128)
                        sc_, po_, ST = psc[it % 2], pos[it % 2], STs[it % 2]
                        ob_, ss_ = osb[it % 2], ssn[it % 2]
                        it += 1
                        kb.op("pe", "matmul", sc_[:, 0:128], kinv[:, hh, cc_], qdec[:, hh, cc_], start=True, stop=True)
                        kb.op("dve", "tensor_tensor", ST[:], sc_[:, 0:128], m64[:], ALU.mult)
                        kb.op("pe", "matmul", po_[:, 0:128], ST[:], vtm[:, tt, hs], start=True, stop=False)
                        for bk in range(2):
                            rows = slice(bk * 64, (bk + 1) * 64)
                            cb = slice(tt * 128 + bk * 64, tt * 128 + (bk + 1) * 64)
                            kb.op("pe", "matmul", po_[rows, 0:128], qdec[:, hh, cb], Sb.at(hh)[:, hh, :], start=False, stop=(bk == 1))
                            pk = pkv[(it + bk) % 2]
                            kb.op("pe", "matmul", pk[:, 0:128], kendT[rows, tt, hs], vtm[rows, tt, hs], start=True, stop=True)
                            blk = tt * 2 + bk
                            kb.op("dve", "scalar_tensor_tensor", Sf.at(hh)[:, hh, :], Sf.at(hh)[:, hh, :], elast[:, hh, blk:blk + 1],
                                  pk[:, 0:128], ALU.mult, ALU.add)
                            kb.op("act", "copy", Sb.at(hh)[:, hh, :], Sf.at(hh)[:, hh, :])
                        kb.op("act", "activation", junk[:], po_[:, 0:128], AF.Square, accum_out=ss_[:, 0:1])
                        kb.op("dve", "tensor_copy", ob_[:], po_[:, 0:128])
                        kb.op("dve", "tensor_scalar", ss_[:, 1:2], ss_[:, 0:1], 1.0 / 128, RMS_EPS, ALU.mult, ALU.add)
                        kb.op("act", "activation", ss_[:, 2:3], ss_[:, 1:2], AF.Sqrt)
                        kb.op("dve", "reciprocal", ss_[:, 3:4], ss_[:, 2:3])
                        kb.op("dve", "scalar_tensor_tensor", ob_[:], ob_[:], ss_[:, 3:4], hgB[:], ALU.mult, ALU.mult)
                        kb.op("dve", "tensor_tensor", ot[:, hs], ob_[:], ogs[:, tt, hs], ALU.mult)
                    kb.dma("sp", oD[r0:r0 + 128, :], ot[:])


def lay_c(c_b):
    return np.ascontiguousarray(c_b.reshape(16, 128).T)
def lay_w1(w1_l):
    NE = w1_l.shape[0]
    g = w1_l[:, :, 0::2].reshape(NE, 16, 128, 6, 128)
    u = w1_l[:, :, 1::2].reshape(NE, 16, 128, 6, 128)
    gu = np.concatenate([g, u], axis=-1)
    return np.ascontiguousarray(gu.transpose(0, 3, 2, 1, 4))
def lay_b1(b1_l):
    NE = b1_l.shape[0]
    g = b1_l[:, 0::2].reshape(NE, 6, 128)
    u = b1_l[:, 1::2].reshape(NE, 6, 128)
    gu = np.concatenate([g, u], axis=1)
    return np.ascontiguousarray(gu.transpose(2, 0, 1))
def lay_w2(w2_l):
    NE = w2_l.shape[0]
    return np.ascontiguousarray(w2_l.reshape(NE, 3, 2, 128, 2048).transpose(0, 1, 3, 2, 4))
def lay_slabs(w, cols_list):
    out = np.zeros((len(cols_list), 128, 16, 128), np.float32)
    for i, cols in enumerate(cols_list):
        blk = w[:, cols].reshape(16, 128, len(cols))
        out[i, :, :, :len(cols)] = blk.transpose(1, 0, 2)
    return out
def hgrn_slabs(w_in, j):
    cl = []
    for hh in range(4):
        hd = 4 * j + hh
        for ty in range(4):
            cl.append(np.arange(ty * 2048 + hd * 128, ty * 2048 + (hd + 1) * 128))
    return lay_slabs(w_in, cl)
def hgrn_lbraw(lb_all, j):
    x = lb_all[:, j * 512:(j + 1) * 512].reshape(4, 4, 128)
    return np.ascontiguousarray(x.transpose(2, 1, 0))
def ab_slabs(w_in, g):
    cl = []
    for c in range(4): cl.append(g * 512 + c * 128 + np.arange(128))
    for c in range(4): cl.append(2048 + g * 512 + c * 128 + np.arange(128))
    cl.append(4096 + g * 128 + np.arange(128))
    cl.append(4608 + g * 128 + np.arange(128))
    cl.append(5120 + g * 8 + np.arange(8))
    for s in range(16): cl.append(5152 + g * 512 + s * 32 + np.arange(32))
    return lay_slabs(w_in, cl)
def ab_params(inp, i, g):
    P = {}
    cw = inp["ab_conv_w"][i]; cb = inp["ab_conv_b"][i]
    chs = [g * 512 + c * 128 + np.arange(128) for c in range(4)] + [2048 + g * 128 + np.arange(128), 2560 + g * 128 + np.arange(128)]
    P["convw"] = np.ascontiguousarray(np.stack([cw[:, ch].T for ch in chs], axis=1))
    P["convb"] = np.ascontiguousarray(np.stack([cb[ch] for ch in chs], axis=1))
    P["dtb"] = np.ascontiguousarray(inp["ssd_dt_bias"][i][g * 8:(g + 1) * 8, None])
    P["alog"] = np.ascontiguousarray(inp["ssd_a_log"][i][g * 8:(g + 1) * 8, None])
    heads = g * 8 + (np.arange(512) // 64)
    P["ssdD"] = np.ascontiguousarray(inp["ssd_d"][i][heads].reshape(4, 128).T)
    P["normg"] = np.ascontiguousarray(inp["ssd_norm_g"][i][g * 512:(g + 1) * 512].reshape(4, 128).T)
    G = (32 * g + np.arange(32)).reshape(16, 2)
    def st(a):
        return np.ascontiguousarray(a[G].transpose(1, 2, 0).reshape(128, 16))
    P["lre"] = st(inp["s5_lam_re"][i]); P["lim"] = st(inp["s5_lam_im"][i]); P["lstep"] = st(inp["s5_log_step"][i])
    def bd(a):
        out = np.zeros((2, 64, 16, 2, 16), np.float32)
        for gg in range(2):
            out[gg, :, :, gg, :] = a[:, gg].transpose(1, 0, 2)
        return out.reshape(128, 16, 32)
    P["bre"] = bd(inp["s5_b_re"][i][G]); P["bim"] = bd(inp["s5_b_im"][i][G])
    P["cre"] = bd(inp["s5_c_re"][i][G].transpose(0, 1, 3, 2)); P["cim"] = bd(inp["s5_c_im"][i][G].transpose(0, 1, 3, 2))
    P["s5d"] = np.ascontiguousarray(inp["s5_d"][i][g * 512:(g + 1) * 512].reshape(16, 32).T)
    w_in = inp["ab_w_in"][i]
    P["wz"] = np.ascontiguousarray(w_in[:, g * 512:(g + 1) * 512].reshape(16, 128, 512).transpose(1, 0, 2))
    P["wdt"] = np.ascontiguousarray(w_in[:, 5120 + g * 8:5120 + (g + 1) * 8].reshape(16, 128, 8).transpose(1, 0, 2))
    P["dtbr"] = np.ascontiguousarray(inp["ssd_dt_bias"][i][None, g * 8:(g + 1) * 8])
    P["alogr"] = np.ascontiguousarray(inp["ssd_a_log"][i][None, g * 8:(g + 1) * 8])
    P["ssdDr"] = np.ascontiguousarray(inp["ssd_d"][i][heads][None, :])
    P["normgr"] = np.ascontiguousarray(inp["ssd_norm_g"][i][None, g * 512:(g + 1) * 512])
    return P
AB_PSHAPES = dict(convw=[128, 6, 4], convb=[128, 6], dtb=[8, 1], alog=[8, 1], ssdD=[128, 4], normg=[128, 4],
                  lre=[128, 16], lim=[128, 16], lstep=[128, 16], bre=[128, 16, 32], bim=[128, 16, 32],
                  cre=[128, 16, 32], cim=[128, 16, 32], s5d=[32, 16],
                  wz=[128, 16, 512], wdt=[128, 16, 8], dtbr=[1, 8], alogr=[1, 8], ssdDr=[1, 512], normgr=[1, 512])
def hgrn2_lay(w_in, j):
    cl = []
    for hh in range(4):
        hd = 4 * j + hh
        for ty in range(2):
            cl.append(np.arange(ty * 2048 + hd * 128, ty * 2048 + (hd + 1) * 128))
    slabs = lay_slabs(w_in, cl)
    wi = np.ascontiguousarray(w_in[:, 2 * 2048 + j * 512:2 * 2048 + (j + 1) * 512].reshape(16, 128, 512).transpose(1, 0, 2))
    wog = np.ascontiguousarray(w_in[:, 3 * 2048 + j * 512:3 * 2048 + (j + 1) * 512].reshape(16, 128, 512).transpose(1, 0, 2))
    return slabs, wi, wog


NT_CORE = 2048
SEQ = 8192
_PROGS = {}


def _common(kb, c0=0, ncols=6 * D):
    ct = kb.dram("ct", [128, 16], F32, kind="ExternalInput")
    adaw = kb.dram("adaw", [D, 6 * D], F32, kind="ExternalInput")
    adab = kb.dram("adab", [1, 6 * D], F32, kind="ExternalInput")
    modD = kb.dram("modD", [1, 6 * D], F32)
    C = make_consts(kb)
    emit_mod(kb, ct, adaw, adab, modD, ncols, c0)
    return C, modD, ct


def _build_M_hg(lidx):
    nc = bass.Bass("TRN2", target_bir_lowering=False)
    with ExitStack() as st:
        kb = KB(nc, st)
        hfull = kb.dram("hfull", [SEQ, D], F32, kind="ExternalInput")
        ng = kb.dram("ng", [1, D], F32, kind="ExternalInput")
        wsl = kb.dram("wsl", [8, 128, 16, 128], F32, kind="ExternalInput")
        wi = kb.dram("wi", [128, 16, 512], F32, kind="ExternalInput")
        wog = kb.dram("wog", [128, 16, 512], F32, kind="ExternalInput")
        lbraw = kb.dram("lbraw", [128, 4, 4], F32, kind="ExternalInput")
        hgg = kb.dram("hgg", [1, 128], F32, kind="ExternalInput")
        oT = kb.dram("oT", [SEQ, 512], F32, kind="ExternalOutput")
        C, modD, ct = _common(kb, 0, 2 * D)
        emit_hgrn_M2(kb, C, SEQ, lidx, hfull, modD, ng, wsl, wi, wog, lbraw, hgg, oT)
        kb.finish()
    return nc


def _build_M_ab():
    nc = bass.Bass("TRN2", target_bir_lowering=False)
    with ExitStack() as st:
        kb = KB(nc, st)
        hfull = kb.dram("hfull", [SEQ, D], F32, kind="ExternalInput")
        ng = kb.dram("ng", [1, D], F32, kind="ExternalInput")
        wsl = kb.dram("wsl", [27, 128, 16, 128], F32, kind="ExternalInput")
        P = {k: kb.dram("p_" + k, shp, F32, kind="ExternalInput") for k, shp in AB_PSHAPES.items()}
        yaT = kb.dram("yaT", [SEQ, 512], F32, kind="ExternalOutput")
        ybT = kb.dram("ybT", [512, SEQ], F32, kind="ExternalOutput")
        C, modD, ct = _common(kb, 0, 2 * D)
        emit_ab_M(kb, C, SEQ, hfull, modD, ng, wsl, P, yaT, ybT)
        kb.finish()
    return nc


def _build_P(kind, final):
    nc = bass.Bass("TRN2", target_bir_lowering=False)
    with ExitStack() as st:
        kb = KB(nc, st)
        NT = NT_CORE
        hin = kb.dram("hin", [NT, D], F32, kind="ExternalInput")
        ng = kb.dram("ng", [1, D], F32, kind="ExternalInput")
        rw = kb.dram("rw", [D, NE], F32, kind="ExternalInput")
        rb = kb.dram("rb", [1, NE], F32, kind="ExternalInput")
        w1 = kb.dram("w1", [NE, 6, 128, 16, 256], F32, kind="ExternalInput")
        b1 = kb.dram("b1", [128, NE, 12], F32, kind="ExternalInput")
        w2 = kb.dram("w2", [NE, 3, 128, 2, D], F32, kind="ExternalInput")
        b2 = kb.dram("b2", [NE, D], F32, kind="ExternalInput")
        hout = kb.dram("hout", [NT, D], F32, kind="ExternalOutput")
        hmid = kb.dram("hmid", [NT, D], F32)
        C, modD, ct = _common(kb, 2 * D, 6 * D)
        if kind == "ab":
            yaT = kb.dram("yaT", [D, NT], F32, kind="ExternalInput")
            ybT = kb.dram("ybT", [D, NT], F32, kind="ExternalInput")
            gluw = kb.dram("gluw", [D, D], F32, kind="ExternalInput")
            glub = kb.dram("glub", [128, 16], F32, kind="ExternalInput")
            wout = kb.dram("wout", [2 * D, D], F32, kind="ExternalInput")
            ybfD = kb.dram("ybfD", [D, NT], BF16)
            emit_glu(kb, NT, ybT, gluw, glub, ybfD)
            emit_outproj(kb, NT, hin, hmid, modD, 2 * D, [(yaT, 16), (ybfD, 16)], wout, 32)
        else:
            oT = kb.dram("oT", [D, NT], F32, kind="ExternalInput")
            wout = kb.dram("wout", [D, D], F32, kind="ExternalInput")
            emit_outproj(kb, NT, hin, hmid, modD, 2 * D, [(oT, 16)], wout, 16)
        fin = None
        if final:
            faw = kb.dram("faw", [D, 2 * D], F32, kind="ExternalInput")
            fab = kb.dram("fab", [1, 2 * D], F32, kind="ExternalInput")
            fg = kb.dram("fg", [1, D], F32, kind="ExternalInput")
            fmodD = kb.dram("fmodD", [1, 2 * D], F32)
            emit_mod(kb, ct, faw, fab, fmodD, 2 * D)
            fin = (fmodD, fg, hout)
        emit_moe(kb, C, NT, hmid, hout, modD, (3 * D, 4 * D, 5 * D), ng, rw, rb, w1, b1, w2, b2, fin=fin)
        kb.finish()
    return nc


def _prog(key, fn, *a):
    if key not in _PROGS:
        _PROGS[key] = fn(*a)
    return _PROGS[key]


def kernel(**inp):
    inp = {k: np.asarray(v) for k, v in inp.items()}
    x = inp["x"].astype(np.float32, copy=False)
    c = inp["c"].astype(np.float32, copy=False)
    B, L, _ = x.shape
    nq = L // NT_CORE
    ncore = B * nq
    A = np.ascontiguousarray
    h = [A(x[cc // nq, (cc % nq) * NT_CORE:(cc % nq + 1) * NT_CORE]) for cc in range(ncore)]
    cts = [lay_c(c[b]) for b in range(B)]
    depth = inp["ada_w"].shape[0]
    for l in range(depth):
        i = l // 2
        final = (l == depth - 1)
        adaw, adab = A(inp["ada_w"][l]), A(inp["ada_b"][l][None])
        hfull = [np.concatenate(h[b * nq:(b + 1) * nq], axis=0) for b in range(B)]
        if l % 2 == 0:
            nc = _prog("M_ab", _build_M_ab)
            in_maps = []
            for cc in range(ncore):
                b, g = cc // 4, cc % 4
                d = dict(hfull=hfull[b], ct=cts[b], adaw=adaw, adab=adab, ng=A(inp["norm1_g"][l][None]),
                         wsl=ab_slabs(inp["ab_w_in"][i], g))
                for k, v in ab_params(inp, i, g).items():
                    d["p_" + k] = v
                in_maps.append(d)
            res = run_bass_kernel_spmd(nc, in_maps, core_ids=list(range(ncore)))
            yaT = [np.concatenate([res.results[b * 4 + g]["yaT"].T for g in range(4)], axis=0) for b in range(B)]
            ybT = [np.concatenate([res.results[b * 4 + g]["ybT"] for g in range(4)], axis=0) for b in range(B)]
            extra = lambda b, q: dict(yaT=A(yaT[b][:, q * NT_CORE:(q + 1) * NT_CORE]), ybT=A(ybT[b][:, q * NT_CORE:(q + 1) * NT_CORE]),
                                      gluw=A(inp["s5_glu_w"][i]), glub=A(inp["s5_glu_b"][i].reshape(16, 128).T),
                                      wout=A(inp["ab_w_out"][i]))
            kind = "ab"
        else:
            nc = _prog(("M_hg", l), _build_M_hg, l)
            in_maps = []
            for cc in range(ncore):
                b, j = cc // 4, cc % 4
                sl_, wi_, wog_ = hgrn2_lay(inp["hg_w_in"][i], j)
                in_maps.append(dict(hfull=hfull[b], ct=cts[b], adaw=adaw, adab=adab, ng=A(inp["norm1_g"][l][None]),
                                    wsl=sl_, wi=wi_, wog=wog_, lbraw=hgrn_lbraw(inp["hg_lower_bounds"], j),
                                    hgg=A(inp["hg_norm_g"][i][None, :])))
            res = run_bass_kernel_spmd(nc, in_maps, core_ids=list(range(ncore)))
            oT = [np.concatenate([res.results[b * 4 + j]["oT"].T for j in range(4)], axis=0) for b in range(B)]
            extra = lambda b, q: dict(oT=A(oT[b][:, q * NT_CORE:(q + 1) * NT_CORE]), wout=A(inp["hg_w_out"][i]))
            kind = "hg"
        del res
        nc = _prog(("P", kind, final), _build_P, kind, final)
        shared = dict(adaw=adaw, adab=adab, ng=A(inp["norm2_g"][l][None]), rw=A(inp["moe_router_w"][l]),
                      rb=A(inp["moe_router_b"][l][None]), w1=lay_w1(inp["moe_w1"][l]), b1=lay_b1(inp["moe_b1"][l]),
                      w2=lay_w2(inp["moe_w2"][l]), b2=A(inp["moe_b2"][l]))
        if final:
            shared.update(faw=A(inp["final_ada_w"]), fab=A(inp["final_ada_b"][None]), fg=A(inp["final_norm_g"][None]))
        in_maps = []
        for cc in range(ncore):
            b, q = cc // nq, cc % nq
            d = dict(shared, hin=h[cc], ct=cts[b])
            d.update(extra(b, q))
            in_maps.append(d)
        res = run_bass_kernel_spmd(nc, in_maps, core_ids=list(range(ncore)))
        h = [np.asarray(res.results[cc]["hout"]) for cc in range(ncore)]
        del res
    out = np.stack([np.concatenate(h[b * nq:(b + 1) * nq], axis=0) for b in range(B)], axis=0)
    return out.astype(np.float32)
```
